# Optimizing a Trainium2 kernel written in Bass

```python
import math
import jax, jax.numpy as jnp
from jax import lax
import numpy as np

D_MODEL = 1024
BATCH = 8
SEQ = 2048
DEPTH = 4

HEAD_DIM = 64
MOBA_HEADS = 4
MOBA_BLOCK = 256
MOBA_TOPK = 3
MOBA_Q_CHUNK = 64
DIFF_HEADS = 4
DIFF_QK_DIM = 32
DIFF_V_DIM = 2 * DIFF_QK_DIM
DIFF_Q_BLOCK = 128
SWA_Q_HEADS = 8
SWA_KV_HEADS = 2
SWA_WINDOW = 128
SWA_BLOCK = 128
D_FF = 2816

N_ALIBI = MOBA_HEADS + DIFF_HEADS + SWA_Q_HEADS
RMS_EPS = 1e-6
NEG = -1e30

A_Q = MOBA_HEADS * HEAD_DIM
A_K = MOBA_HEADS * HEAD_DIM
A_V = MOBA_HEADS * HEAD_DIM
B_Q = DIFF_HEADS * 2 * DIFF_QK_DIM
B_K = DIFF_HEADS * 2 * DIFF_QK_DIM
B_V = DIFF_HEADS * DIFF_V_DIM
C_Q = SWA_Q_HEADS * HEAD_DIM
C_KV = SWA_KV_HEADS * HEAD_DIM
PROJ_WIDTH = A_Q + A_K + A_V + B_Q + B_K + B_V + C_Q + 2 * C_KV
MIX_WIDTH = A_V + B_V + C_Q

kernel_name = "hybrid_moba_diff_swa_macaron"


def _split_points():
    widths = [A_Q, A_K, A_V, B_Q, B_K, B_V, C_Q, C_KV, C_KV]
    return [int(v) for v in np.cumsum(widths)[:-1]]


def _alibi_slopes():
    n = N_ALIBI
    return jnp.asarray(2.0 ** (-8.0 * (np.arange(n, dtype=np.float32) + 1.0) / n), dtype=jnp.float32)


def rmsnorm(x, g):
    xf = x.astype(jnp.float32)
    y = xf * lax.rsqrt(jnp.mean(xf * xf, axis=-1, keepdims=True) + RMS_EPS)
    return (y * g.astype(jnp.float32)).astype(x.dtype)


def swiglu(h, w_gate, w_up, w_down):
    return (jax.nn.silu(h @ w_gate) * (h @ w_up)) @ w_down


def moba_attention(q, k, v, slopes):
    B, S, H, Dh = q.shape
    L = MOBA_BLOCK
    nb = -(-S // L)
    Sp = nb * L
    padw = ((0, 0), (0, Sp - S), (0, 0), (0, 0))
    qf = jnp.pad(q.astype(jnp.float32), padw).transpose(0, 2, 1, 3)
    kf = jnp.pad(k.astype(jnp.float32), padw).transpose(0, 2, 1, 3)
    vf = jnp.pad(v.astype(jnp.float32), padw).transpose(0, 2, 1, 3)
    kb = kf.reshape(B, H, nb, L, Dh)
    vb = vf.reshape(B, H, nb, L, Dh)
    kmean = jnp.mean(kb, axis=3)
    gate = jnp.einsum('bhtd,bhnd->bhtn', qf, kmean)
    qblk = jnp.arange(Sp) // L
    past = jnp.arange(nb)[None, :] < qblk[:, None]
    gate = jnp.where(past[None, None], gate, NEG)
    n_sel = min(MOBA_TOPK, nb)
    _, idx = lax.top_k(gate, n_sel)
    valid = idx < qblk[None, None, :, None]

    QC = MOBA_Q_CHUNK
    nq = Sp // QC
    q_c = qf.reshape(B, H, nq, QC, Dh).transpose(2, 0, 1, 3, 4)
    idx_c = idx.reshape(B, H, nq, QC, n_sel).transpose(2, 0, 1, 3, 4)
    val_c = valid.reshape(B, H, nq, QC, n_sel).transpose(2, 0, 1, 3, 4)
    starts = jnp.arange(nq, dtype=jnp.int32) * QC
    gather = jax.vmap(jax.vmap(lambda blocks, ii: blocks[ii]))
    scale = Dh ** -0.5
    kpos_in = jnp.arange(L)

    def chunk(args):
        qc, ic, vc, t0 = args
        tq = t0 + jnp.arange(QC)
        own = t0 // L
        k_own = lax.dynamic_index_in_dim(kb, own, axis=2, keepdims=False)
        v_own = lax.dynamic_index_in_dim(vb, own, axis=2, keepdims=False)
        ks = gather(kb, ic)
        vs = gather(vb, ic)
        qs = qc * scale
        s_sel = jnp.einsum('bhqd,bhqjld->bhqjl', qs, ks)
        pos_sel = ic[..., None] * L + kpos_in
        dist_sel = (tq[None, None, :, None, None] - pos_sel).astype(jnp.float32)
        s_sel = s_sel - slopes[None, :, None, None, None] * dist_sel
        s_sel = jnp.where(vc[..., None], s_sel, NEG)
        s_own = jnp.einsum('bhqd,bhld->bhql', qs, k_own)
        dist_own = (tq[:, None] - (own * L + kpos_in)[None, :]).astype(jnp.float32)
        s_own = jnp.where(dist_own >= 0, s_own - slopes[None, :, None, None] * dist_own, NEG)
        s = jnp.concatenate([s_sel.reshape(B, H, QC, n_sel * L), s_own], axis=-1)
        p = jax.nn.softmax(s, axis=-1)
        p_sel = p[..., :n_sel * L].reshape(B, H, QC, n_sel, L)
        p_own = p[..., n_sel * L:]
        return (jnp.einsum('bhqjl,bhqjld->bhqd', p_sel, vs)
                + jnp.einsum('bhql,bhld->bhqd', p_own, v_own))

    out = lax.map(chunk, (q_c, idx_c, val_c, starts))
    out = out.transpose(1, 0, 3, 2, 4).reshape(B, Sp, H, Dh)
    return out[:, :S]


def diff_attention(q, k, v, slopes, lam, subln_g, lam_init):
    B, S, H, _, dq = q.shape
    dv = v.shape[-1]
    QB = DIFF_Q_BLOCK
    nq = S // QB
    qf = q.astype(jnp.float32) * (dq ** -0.5)
    kf = k.astype(jnp.float32)
    vf = v.astype(jnp.float32)
    q_c = qf.reshape(B, nq, QB, H, 2, dq).transpose(1, 0, 2, 3, 4, 5)
    starts = jnp.arange(nq, dtype=jnp.int32) * QB
    kpos = jnp.arange(S)

    def blk(args):
        qc, t0 = args
        tq = t0 + jnp.arange(QB)
        dist = (tq[:, None] - kpos[None, :]).astype(jnp.float32)
        bias = -slopes[:, None, None] * dist
        s = jnp.einsum('bqhmd,bshmd->bhmqs', qc, kf) + bias[None, :, None]
        s = jnp.where(dist >= 0, s, NEG)
        p = jax.nn.softmax(s, axis=-1)
        w = p[:, :, 0] - lam * p[:, :, 1]
        return jnp.einsum('bhqs,bshd->bqhd', w, vf)

    o = lax.map(blk, (q_c, starts))
    o = o.transpose(1, 0, 2, 3, 4).reshape(B, S, H, dv)
    return rmsnorm(o, subln_g) * (1.0 - lam_init)


def swa_attention(q, k, v, slopes, sinks):
    B, S, Hq, Dh = q.shape
    Hkv = k.shape[2]
    G = Hq // Hkv
    SB = SWA_BLOCK
    nb = S // SB
    qf = q.astype(jnp.float32).reshape(B, nb, SB, Hkv, G, Dh) * (Dh ** -0.5)
    kb = k.astype(jnp.float32).reshape(B, nb, SB, Hkv, Dh)
    vb = v.astype(jnp.float32).reshape(B, nb, SB, Hkv, Dh)
    zeros = jnp.zeros_like(kb[:, :1])
    kk = jnp.concatenate([jnp.concatenate([zeros, kb[:, :-1]], axis=1), kb], axis=2)
    vv = jnp.concatenate([jnp.concatenate([zeros, vb[:, :-1]], axis=1), vb], axis=2)
    qi = jnp.arange(SB)
    ki = jnp.arange(2 * SB) - SB
    dist = qi[:, None] - ki[None, :]
    in_win = (dist >= 0) & (dist < SWA_WINDOW)
    k_exists = (jnp.arange(nb)[:, None] * SB + ki[None, :]) >= 0
    mask = in_win[None] & k_exists[:, None, :]
    s = jnp.einsum('bnqkgd,bnskd->bkgnqs', qf, kk)
    bias = -slopes.reshape(Hkv, G)[:, :, None, None, None] * dist.astype(jnp.float32)
    s = jnp.where(mask, s + bias, NEG)
    sink = jnp.broadcast_to(sinks.astype(jnp.float32).reshape(Hkv, G)[None, :, :, None, None, None],
                            s.shape[:-1] + (1,))
    p = jax.nn.softmax(jnp.concatenate([s, sink], axis=-1), axis=-1)[..., :-1]
    o = jnp.einsum('bkgnqs,bnskd->bnqkgd', p, vv)
    return o.reshape(B, S, Hq, Dh)


def setup_inputs(seed: int = 0) -> dict:
    key = jax.random.key(seed)
    ks = jax.random.split(key, 20)
    f32 = jnp.float32

    def w(k, shape, fan_in):
        return jax.random.normal(k, shape, f32) * (fan_in ** -0.5)

    def gain(k, shape):
        return 1.0 + 0.05 * jax.random.normal(k, shape, f32)

    return {
        "x": jax.random.normal(ks[0], (BATCH, SEQ, D_MODEL), f32),
        "norm_ffn1": gain(ks[1], (DEPTH, D_MODEL)),
        "w1_gate": w(ks[2], (DEPTH, D_MODEL, D_FF), D_MODEL),
        "w1_up": w(ks[3], (DEPTH, D_MODEL, D_FF), D_MODEL),
        "w1_down": w(ks[4], (DEPTH, D_FF, D_MODEL), D_FF),
        "norm_mix": gain(ks[5], (DEPTH, D_MODEL)),
        "w_in": w(ks[6], (DEPTH, D_MODEL, PROJ_WIDTH), D_MODEL),
        "lam_q1": 0.1 * jax.random.normal(ks[7], (DEPTH, DIFF_QK_DIM), f32),
        "lam_k1": 0.1 * jax.random.normal(ks[8], (DEPTH, DIFF_QK_DIM), f32),
        "lam_q2": 0.1 * jax.random.normal(ks[9], (DEPTH, DIFF_QK_DIM), f32),
        "lam_k2": 0.1 * jax.random.normal(ks[10], (DEPTH, DIFF_QK_DIM), f32),
        "diff_subln": gain(ks[11], (DEPTH, DIFF_V_DIM)),
        "sinks": 0.5 * jax.random.normal(ks[12], (DEPTH, SWA_Q_HEADS), f32),
        "w_out": w(ks[13], (DEPTH, MIX_WIDTH, D_MODEL), MIX_WIDTH),
        "norm_ffn2": gain(ks[14], (DEPTH, D_MODEL)),
        "w2_gate": w(ks[15], (DEPTH, D_MODEL, D_FF), D_MODEL),
        "w2_up": w(ks[16], (DEPTH, D_MODEL, D_FF), D_MODEL),
        "w2_down": w(ks[17], (DEPTH, D_FF, D_MODEL), D_FF),
        "final_norm": gain(ks[18], (D_MODEL,)),
    }


def reference(x, norm_ffn1, w1_gate, w1_up, w1_down, norm_mix, w_in, lam_q1, lam_k1, lam_q2, lam_k2,
              diff_subln, sinks, w_out, norm_ffn2, w2_gate, w2_up, w2_down, final_norm):
    B, S, _ = x.shape
    slopes = _alibi_slopes()
    slopes_c = slopes[:SWA_Q_HEADS]
    slopes_b = slopes[SWA_Q_HEADS:SWA_Q_HEADS + DIFF_HEADS]
    slopes_a = slopes[SWA_Q_HEADS + DIFF_HEADS:]
    splits = _split_points()
    for l in range(DEPTH):
        x = x + 0.5 * swiglu(rmsnorm(x, norm_ffn1[l]), w1_gate[l], w1_up[l], w1_down[l])
        h = rmsnorm(x, norm_mix[l])
        p = h @ w_in[l]
        aq, ak, av, bq, bk, bv, cq, ck, cv = jnp.split(p, splits, axis=-1)
        o_a = moba_attention(aq.reshape(B, S, MOBA_HEADS, HEAD_DIM),
                             ak.reshape(B, S, MOBA_HEADS, HEAD_DIM),
                             av.reshape(B, S, MOBA_HEADS, HEAD_DIM), slopes_a)
        lam_init = 0.8 - 0.6 * math.exp(-0.3 * l)
        lam = (jnp.exp(jnp.sum(lam_q1[l].astype(jnp.float32) * lam_k1[l].astype(jnp.float32)))
               - jnp.exp(jnp.sum(lam_q2[l].astype(jnp.float32) * lam_k2[l].astype(jnp.float32)))
               + lam_init)
        o_b = diff_attention(bq.reshape(B, S, DIFF_HEADS, 2, DIFF_QK_DIM),
                             bk.reshape(B, S, DIFF_HEADS, 2, DIFF_QK_DIM),
                             bv.reshape(B, S, DIFF_HEADS, DIFF_V_DIM),
                             slopes_b, lam, diff_subln[l], lam_init)
        o_c = swa_attention(cq.reshape(B, S, SWA_Q_HEADS, HEAD_DIM),
                            ck.reshape(B, S, SWA_KV_HEADS, HEAD_DIM),
                            cv.reshape(B, S, SWA_KV_HEADS, HEAD_DIM), slopes_c, sinks[l])
        mix = jnp.concatenate([o_a.reshape(B, S, A_V), o_b.reshape(B, S, B_V),
                               o_c.reshape(B, S, C_Q)], axis=-1).astype(x.dtype)
        x = x + mix @ w_out[l]
        x = x + 0.5 * swiglu(rmsnorm(x, norm_ffn2[l]), w2_gate[l], w2_up[l], w2_down[l])
    return rmsnorm(x, final_norm)
```

```python
import math
from contextlib import ExitStack
import numpy as np
import ml_dtypes
import concourse.bass as bass
import concourse.mybir as mybir
from concourse.bass_utils import run_bass_kernel_spmd

F32 = mybir.dt.float32
BF16 = mybir.dt.bfloat16
AF = mybir.ActivationFunctionType
ALU = mybir.AluOpType
AX = mybir.AxisListType

D = 1024
T = 2048
DFF = 2816
NF = DFF // 128
G = 2
import os
NG = int(os.environ.get('KDEBUG_NG', NF // G))
DEPTH = 4
EPS = 1e-6
NSLOT = 6
FUSED = True
SAME_ENGINE_SYNC = True

SLOPES = 2.0 ** (-8.0 * (np.arange(16, dtype=np.float64) + 1.0) / 16.0)
SL_C = SLOPES[0:8]
SL_B = SLOPES[8:12]
SL_A = SLOPES[12:16]

CB_MASKC = 0
CB_TRI = 128
CB_ID = 256
CB_E = 384
CB_ONES = 1408
CB_N = 1536
CF_TBAB = 0
CF_TBC = 128
CF_PAST = 144
CF_A30 = 272
CF_BC = 400
CF_ONES = 528
CF_EPS = 592
CF_N = 600
PR_G = 0
PR_SUB = 104
PR_LAM = 108
PR_SINK = 620
PR_N = 652


class Op:
    __slots__ = ("eng", "fn", "deps", "needed", "sem", "val", "dma", "epoch")


class Prog:
    ENGS = ("pe", "act", "dve", "pool", "sp")

    def __init__(self):
        self.ops = {e: [] for e in self.ENGS}
        self.lastw = {}
        self.readers = {}
        self.epoch = 0

    def add(self, eng, fn, reads=(), writes=(), dma=None):
        op = Op()
        op.eng, op.fn, op.dma, op.epoch = eng, fn, dma, self.epoch
        op.needed = False
        op.sem = None
        op.val = 0
        deps = {}
        for t in reads:
            w = self.lastw.get(t)
            if w is not None:
                deps[id(w)] = w
        for t in writes:
            w = self.lastw.get(t)
            if w is not None:
                deps[id(w)] = w
            for r in self.readers.get(t, ()):
                deps[id(r)] = r
        out = []
        for d in deps.values():
            if d is op:
                continue
            if d.eng == eng and d.dma is None:
                if eng == "pe" or eng == "sp" or not SAME_ENGINE_SYNC:
                    continue
            out.append(d)
        op.deps = out
        for t in reads:
            if isinstance(t, str) and t.startswith("c_"):
                continue
            self.readers.setdefault(t, []).append(op)
        for t in writes:
            self.lastw[t] = op
            self.readers[t] = []
        self.ops[eng].append(op)
        return op

    def emit(self, nc, stack):
        sems = {}

        def getsem(key):
            if key not in sems:
                sems[key] = stack.enter_context(nc.semaphore("s%d" % len(sems)))
            return sems[key]

        for e in self.ENGS:
            for op in self.ops[e]:
                for d in op.deps:
                    d.needed = True
        cnt = {}
        for e in self.ENGS:
            for op in self.ops[e]:
                if op.dma is not None:
                    key = ("dma", op.dma)
                    cnt[key] = cnt.get(key, 0) + 16
                    op.sem, op.val = getsem(key), cnt[key]
                elif op.needed:
                    key = (e, op.epoch)
                    cnt[key] = cnt.get(key, 0) + 1
                    op.sem, op.val = getsem(key), cnt[key]
        block = stack.enter_context(nc.Block())
        stats = {}

        def run(eng_name, eng):
            waited = {}
            nw = 0
            for op in self.ops[eng_name]:
                need = {}
                for d in op.deps:
                    k = id(d.sem)
                    if waited.get(k, 0) >= d.val:
                        continue
                    if k not in need or need[k][1] < d.val:
                        need[k] = (d.sem, d.val)
                for k, (s, v) in need.items():
                    eng.wait_ge(s, v)
                    waited[k] = v
                    nw += 1
                inst = op.fn(eng)
                if op.dma is not None:
                    inst.then_inc(op.sem, 16)
                elif op.needed:
                    inst.then_inc(op.sem, 1)
            stats[eng_name] = (len(self.ops[eng_name]), nw)

        @block.tensor
        def _(e):
            run("pe", e)

        @block.scalar
        def _(e):
            run("act", e)

        @block.vector
        def _(e):
            run("dve", e)

        @block.gpsimd
        def _(e):
            run("pool", e)

        @block.sync
        def _(e):
            run("sp", e)

        return stats


def bcast_mid(ap, n):
    l = [list(x) for x in ap.ap]
    return bass.AP(ap.tensor, ap.offset, [l[0], [0, n]] + l[1:])


def build(layer_ids, do_final, stop_after=None):
    NL = len(layer_ids)
    nc = bass.Bass("TRN2", target_bir_lowering=False)
    dr = {}

    def din(name, shape, dt=F32):
        dr[name] = nc.dram_tensor(name, list(shape), dt, kind="ExternalInput").ap()
        return dr[name]

    xin = din("xT", [D, T])
    w1g = din("w1g", [NL, D, DFF]); w1u = din("w1u", [NL, D, DFF]); w1d = din("w1d", [NL, DFF, D])
    w2g = din("w2g", [NL, D, DFF]); w2u = din("w2u", [NL, D, DFF]); w2d = din("w2d", [NL, DFF, D])
    win = din("win", [NL, D, 8 * 384]); wout = din("wout", [NL, D, D])
    cbd = din("cb", [128, CB_N], BF16); cfd = din("cf", [128, CF_N]); prd = din("par", [128, PR_N])
    posd = din("posrow", [1, 1024])
    outd = nc.dram_tensor("outT", [D, T], F32, kind="ExternalOutput").ap()

    P = Prog()
    st = ExitStack()
    with st:
        def sb(name, shape, dt):
            return st.enter_context(nc.sbuf_tensor(name, list(shape), dt))

        xT = sb("xT_sb", [128, 8, T], F32)
        hT = sb("hT", [128, 8, T], BF16)
        ring = sb("ring", [128, NSLOT, 2048], BF16)
        qk = sb("qk", [128, 2, T], BF16)
        vaug = sb("vaug", [128, 16, 2, 65], BF16)
        mix = sb("mix", [128, 2, T], BF16)
        actb = sb("actb", [128, 4096], BF16)
        pt = sb("pt", [128, 3, 1024], BF16)
        mnegt = sb("mnegt", [128, T], BF16)
        mneg = sb("mneg", [128, 2, 128], BF16)
        gm = sb("gm", [128, 16], F32)
        top = sb("top", [128, 16], F32)
        sel = sb("sel", [128, 16], F32)
        kms = sb("kms", [128, 8], F32)
        kmean = sb("kmean", [128, 8], BF16)
        rstd = sb("rstd", [128, 2, 512], F32)
        sg = sb("sg", [128, 2, 512], BF16)
        bcsb = sb("bcsb", [128, 512], F32)
        o1 = sb("o1", [128, 512], F32)
        o2 = sb("o2", [128, 512], F32)
        osq = sb("osq", [128, 512], BF16)
        rden = sb("rden", [128, 512], F32)
        sinkrow = sb("sinkrow", [128, 1024], F32)
        cb = sb("cb_sb", [128, CB_N], BF16)
        cf = sb("cf_sb", [128, CF_N], F32)
        par = sb("par_sb", [128, PR_N], F32)
        lamt = sb("lamt", [128, 16], F32)
        lamp = sb("lamp", [128, 2, 32], F32)
        ps = st.enter_context(nc.psum_tensor("ps", [128, 7, 512], F32))
        psb = st.enter_context(nc.psum_tensor("psb", [128, 1024], BF16))

        ones_bf = cb[:, CB_ONES:CB_ONES + 128]
        ident = cb[:, CB_ID:CB_ID + 128]
        tri = cb[:, CB_TRI:CB_TRI + 128]
        maskc = cb[:, CB_MASKC:CB_MASKC + 256]

        def mm(out, lhsT, rhs, start, stop, reads, writes, tp=None):
            kw = {}
            if tp is not None and tp[0] == 96:
                kw["tile_position"] = tp
            return P.add("pe", lambda e: e.matmul(out, lhsT=lhsT, rhs=rhs, start=start, stop=stop, **kw),
                         reads, writes)

        def act(out, in_, func, reads, writes, bias=None, scale=None):
            kw = {}
            if bias is not None:
                kw["bias"] = bias
            if scale is not None:
                kw["scale"] = scale
            return P.add("act", lambda e: e.activation(out, in_, func, **kw), reads, writes)

        def tt(eng, out, in0, in1, op, reads, writes):
            return P.add(eng, lambda e: e.tensor_tensor(out, in0, in1, op), reads, writes)

        def stt(out, in0, scalar, in1, op0, op1, reads, writes):
            return P.add("dve", lambda e: e.scalar_tensor_tensor(out, in0, scalar, in1, op0, op1), reads, writes)

        def ts(eng, out, in0, s1, s2, op0, op1, reads, writes):
            if op1 is None:
                return P.add(eng, lambda e: e.tensor_scalar(out, in0, s1, None, op0), reads, writes)
            return P.add(eng, lambda e: e.tensor_scalar(out, in0, s1, s2, op0, op1), reads, writes)

        def recip(out, in_, reads, writes):
            return P.add("dve", lambda e: e.reciprocal(out, in_), reads, writes)

        def copy(eng, out, in_, reads, writes):
            if eng == "act":
                return P.add("act", lambda e: e.copy(out, in_), reads, writes)
            return P.add(eng, lambda e: e.tensor_copy(out, in_), reads, writes)

        def dma(eng, out, in_, key, reads, writes):
            return P.add(eng, lambda e: e.dma_start(out=out, in_=in_), reads, writes, dma=key)

        def xtok(kc, tc):
            return ("x", kc, tc)

        loads = []

        def colblk(w, l, c0, n):
            return w[l, :, c0:c0 + n].rearrange("(kc p) n -> p kc n", p=128)

        def rowblk(w, l, r0):
            return w[l, r0:r0 + 256, :].rearrange("(rc p) n -> p rc n", p=128)

        def ffn_loads(wg, wu, wd, l):
            for g in range(NG):
                loads.append(("c", colblk(wg, l, g * 256, 256), 256))
                loads.append(("c", colblk(wu, l, g * 256, 256), 256))
                loads.append(("r", rowblk(wd, l, g * 256), 0))

        for l in range(NL):
            ffn_loads(w1g, w1u, w1d, l)
            for p in range(8):
                loads.append(("c", colblk(win, l, p * 384, 256), 256))
                loads.append(("c", colblk(win, l, p * 384 + 256, 128), 128))
                if p % 2 == 1:
                    loads.append(("r", rowblk(wout, l, (p // 2) * 256), 0))
            ffn_loads(w2g, w2u, w2d, l)
        wstate = {"rec": 0, "next": 0}

        def slot_view(i):
            kind, src, n = loads[i]
            s = i % NSLOT
            flat = ring[:, s, :]
            if kind == "c":
                return flat.rearrange("p (kc n) -> p kc n", n=256)
            return flat.rearrange("p (rc n) -> p rc n", n=1024)

        def take(n):
            a = wstate["next"]
            wstate["next"] = a + n
            upto = min(len(loads), a + NSLOT)
            while wstate["rec"] < upto:
                i = wstate["rec"]
                kind, src, ncol = loads[i]
                v = slot_view(i)
                dst = v[:, :, 0:ncol] if kind == "c" else v
                dma("pool", dst, src, ("slot", i % NSLOT), [], [("slot", i % NSLOT)])
                wstate["rec"] += 1
            return [(slot_view(i), ("slot", i % NSLOT)) for i in range(a, a + n)]

        dma("sp", cb[:, :], cbd, "cst", [], ["c_cb"])
        dma("sp", cf[:, :], cfd, "cst", [], ["c_cf"])
        dma("sp", par[:, :], prd, "cst", [], ["c_par"])
        for kc in range(8):
            dma("sp", xT[:, kc, :], xin[kc * 128:(kc + 1) * 128, :], "xin", [], [xtok(kc, tc) for tc in range(4)])
        P.add("pool", lambda e: e.memset(vaug[:, :, :, :], 1.0), [], [("v", t) for t in range(16)])
        P.add("pool", lambda e: e.memset(mneg[:, :, :], 0.0), [], ["mneg"])

        def rmsnorm(gcol, final=False):
            sq = actb[:, :].rearrange("p (k n) -> p k n", n=512)
            for tc in range(4):
                cs = slice(tc * 512, (tc + 1) * 512)
                bank = 5 + (tc % 2)
                rb = rstd[:, tc % 2, :]
                act(sq, xT[:, :, cs], AF.Square, [xtok(kc, tc) for kc in range(8)], [("act", 0), ("act", 1)])
                for kc in range(8):
                    mm(ps[:, bank, :], ones_bf, sq[:, kc, :], kc == 0, kc == 7,
                       ["c_cb", ("act", 0), ("act", 1)], [("ps", bank)])
                act(rb, ps[:, bank, :], AF.Sqrt, [("ps", bank)], [("rstd", tc % 2)], bias=cf[:, CF_EPS:CF_EPS + 1],
                    scale=1.0 / D)
                recip(rb, rb, [("rstd", tc % 2)], [("rstd", tc % 2)])
                for kc in range(8):
                    if final:
                        stt(xT[:, kc, cs], xT[:, kc, cs], par[:, gcol + kc:gcol + kc + 1], rb, ALU.mult, ALU.mult,
                            [xtok(kc, tc), ("rstd", tc % 2), "c_par"], [xtok(kc, tc)])
                    else:
                        stt(hT[:, kc, cs], xT[:, kc, cs], par[:, gcol + kc:gcol + kc + 1], rb, ALU.mult, ALU.mult,
                            [xtok(kc, tc), ("rstd", tc % 2), "c_par"], [("h", kc, tc)])


        def ffn():
            pend = None
            cnt = 0
            for g in range(NG):
                if pend is not None:
                    pend()
                    pend = None
                (wgv, tg), (wuv, tu), (wdv, td) = take(3)
                for tc in range(4):
                    cs = slice(tc * 512, (tc + 1) * 512)
                    ai = cnt % 2
                    av = actb[:, ai * 1024:(ai + 1) * 1024].rearrange("p (f n) -> p f n", n=512)
                    for fi in range(G):
                        bg, bu = 2 * (fi % 2), 2 * (fi % 2) + 1
                        for kc in range(8):
                            mm(ps[:, bg, :], wgv[:, kc, fi * 128:(fi + 1) * 128], hT[:, kc, cs], kc == 0, kc == 7,
                               [tg, ("h", kc, tc)], [("ps", bg)])
                        for kc in range(8):
                            mm(ps[:, bu, :], wuv[:, kc, fi * 128:(fi + 1) * 128], hT[:, kc, cs], kc == 0, kc == 7,
                               [tu, ("h", kc, tc)], [("ps", bu)])
                        act(sg[:, fi % 2, :], ps[:, bg, :], AF.Silu, [("ps", bg)], [("sg", fi % 2)])
                        tt("dve", av[:, fi, :], sg[:, fi % 2, :], ps[:, bu, :], ALU.mult,
                           [("sg", fi % 2), ("ps", bu)], [("act", ai)])
                    if pend is not None:
                        pend()

                    def down(av=av, ai=ai, wdv=wdv, td=td, tc=tc, cs=cs):
                        for dc in range(8):
                            b = 4 + (dc % 3)
                            for fi in range(G):
                                mm(ps[:, b, :], wdv[:, fi, dc * 128:(dc + 1) * 128], av[:, fi, :], fi == 0, fi == G - 1,
                                   [td, ("act", ai)], [("ps", b)])
                            stt(xT[:, dc, cs], ps[:, b, :], 0.5, xT[:, dc, cs], ALU.mult, ALU.add,
                                [("ps", b), xtok(dc, tc)], [xtok(dc, tc)])
                    pend = down
                    cnt += 1
            if pend is not None:
                pend()

        def proj_qk(wv, tw, kind):
            n = 0
            for j in range(2):
                for tc in range(4):
                    cs = slice(tc * 512, (tc + 1) * 512)
                    b = n % 4
                    n += 1
                    for kc in range(8):
                        mm(ps[:, b, :], wv[:, kc, j * 128:(j + 1) * 128], hT[:, kc, cs], kc == 0, kc == 7,
                           [tw, ("h", kc, tc)], [("ps", b)])
                    FL = os.environ.get("KDEBUG_FL", "")
                    copy("act" if (n % 2 and "dvecopy" not in FL) else "dve", qk[:, j, cs], ps[:, b, :], [("ps", b)], [("qk", j, tc)])
                    if kind == "A" and j == 1 and "nored" not in FL:
                        P.add("dve", lambda e, tc=tc, cs=cs: e.reduce_sum(
                            kms[:, 2 * tc:2 * tc + 2], qk[:, 1, cs].rearrange("p (n l) -> p n l", l=256), AX.X),
                            [("qk", 1, tc)], ["kms"])
            if kind == "A":
                copy("dve", kmean[:, :], kms[:, :], ["kms"], ["kmean"])

        def proj_v(wv, tw, per_t=None):
            for t in range(16):
                b = 4 + (t % 2)
                cs = slice(t * 128, (t + 1) * 128)
                oc = slice(0, 128)
                for kc in range(8):
                    mm(ps[:, b, oc], hT[:, kc, cs], wv[:, kc, 0:128], kc == 0, kc == 7,
                       [tw, ("h", kc, t // 4)], [("ps", b)])
                copy("act" if t % 2 else "dve", vaug[:, t, :, 0:64],
                     ps[:, b, oc].rearrange("p (h d) -> p h d", d=64), [("ps", b)], [("v", t)])
                if per_t is not None:
                    per_t(t)

        def normalize(ob, dst, dtok, h_sink=None, to=None):
            if h_sink is not None:
                srow = bcast_mid(sinkrow[64:65, h_sink * 128:(h_sink + 1) * 128], 4)
                tt("dve", rden[64:65, :].rearrange("p (a b) -> p a b", b=128),
                   ps[64:65, ob, :].rearrange("p (a b) -> p a b", b=128), srow, ALU.add,
                   [("ps", ob), "sinkrow"], ["rden"])
                recip(rden[64:65, :], rden[64:65, :], ["rden"], ["rden"])
            else:
                recip(rden[64:65, :], ps[64:65, ob, :], [("ps", ob)], ["rden"])
            mm(ps[0:64, 6, :], cf[64:65, CF_ONES:CF_ONES + 64], rden[64:65, :], True, True,
               ["c_cf", "rden"], [("ps", 6)])
            copy("act", bcsb[0:64, :], ps[0:64, 6, :], [("ps", 6)], ["bcsb"])
            tt("dve", dst, ps[0:64, ob, :], bcsb[0:64, :], ALU.mult, [("ps", ob), "bcsb"], dtok)

        def attn_full(kind, pidx, li):
            nmap = 2 if kind == "B" else 1
            scale = 32 ** -0.5 if kind == "B" else 0.125
            for hl in range(2):
                h = 2 * pidx + hl
                tbcol = CF_TBAB + (h if kind == "A" else 4 + h) * 16
                for c in range(4):
                    nkt = 4 * c + 4
                    obanks = [4, 5] if kind == "B" else [4 + ((hl * 4 + c) % 2)]
                    steps = []
                    for kt in range(nkt):
                        j = kt - 4 * c
                        col0 = max(j, 0) * 128
                        ncols = 512 - col0
                        qs = slice(c * 512 + col0, (c + 1) * 512)
                        ks = slice(kt * 128, (kt + 1) * 128)
                        pi = kt % 3
                        if kind == "A":
                            sb_ = kt % 3
                            stb = [sb_]
                        else:
                            sb_ = 2 * (kt % 2)
                            stb = [sb_, sb_ + 1]

                        def st_fn(kt=kt, j=j, col0=col0, ncols=ncols, qs=qs, ks=ks, stb=stb):
                            if kind == "A":
                                base = hl * 64
                                mm(ps[:, stb[0], 0:ncols], qk[base:base + 64, 1, ks], qk[base:base + 64, 0, qs],
                                   True, False, [("qk", 1, kt // 4), ("qk", 0, c)], [("ps", stb[0])], tp=(base, 0))
                                mm(ps[:, stb[0], 0:ncols],
                                   cb[base:base + 64, CB_E + (kt // 2) * 128:CB_E + (kt // 2) * 128 + 128],
                                   mnegt[base:base + 64, qs], False, True,
                                   ["c_cb", ("mnegt", c)], [("ps", stb[0])], tp=(base, 0))
                            else:
                                for m in range(2):
                                    base = hl * 64 + m * 32
                                    mm(ps[:, stb[m], 0:ncols], qk[base:base + 32, 1, ks], qk[base:base + 32, 0, qs],
                                       True, True, [("qk", 1, kt // 4), ("qk", 0, c)], [("ps", stb[m])], tp=(base, 0))

                        def ex_fn(kt=kt, j=j, ncols=ncols, stb=stb, pi=pi):
                            bias = cf[:, tbcol + j + 12:tbcol + j + 13]
                            if kind == "A":
                                act(pt[:, pi, 0:ncols], ps[:, stb[0], 0:ncols], AF.Exp, [("ps", stb[0]), "c_cf"],
                                    [("pt", pi)], bias=bias, scale=scale)
                                if j >= 0:
                                    tt("pool", pt[:, pi, 0:128], pt[:, pi, 0:128], tri, ALU.mult,
                                       [("pt", pi), "c_cb"], [("pt", pi)])
                            else:
                                pv = pt[:, pi, :].rearrange("p (m n) -> p m n", n=512)
                                act(pv[:, :, 0:ncols], ps[:, stb[0]:stb[0] + 2, 0:ncols], AF.Exp,
                                    [("ps", stb[0]), ("ps", stb[1]), "c_cf"], [("pt", pi)], bias=bias, scale=scale)
                                if j >= 0:
                                    for m in range(2):
                                        tt("pool", pv[:, m, 0:128], pv[:, m, 0:128], tri, ALU.mult,
                                           [("pt", pi), "c_cb"], [("pt", pi)])

                        def pv_fn(kt=kt, col0=col0, ncols=ncols, pi=pi):
                            for m in range(nmap):
                                rhs = pt[:, pi, m * 512:m * 512 + ncols]
                                mm(ps[0:65, obanks[m], col0:512], vaug[:, kt, hl, :], rhs, kt == 0, kt == nkt - 1,
                                   [("v", kt), ("pt", pi)], [("ps", obanks[m])])
                        steps.append((st_fn, ex_fn, pv_fn))
                    LOOK = 2 if kind == "A" else 1
                    for i in range(nkt + LOOK):
                        if i < nkt:
                            steps[i][0]()
                            steps[i][1]()
                        if i - LOOK >= 0:
                            steps[i - LOOK][2]()
                    cs = slice(c * 512, (c + 1) * 512)
                    dst = mix[hl * 64:(hl + 1) * 64, pidx % 2, cs]
                    dtok = [("mix", pidx % 2, c, hl)]
                    if kind == "A":
                        normalize(obanks[0], dst, dtok)
                    else:
                        normalize(4, o1[0:64, :], ["o1"])
                        normalize(5, o2[0:64, :], ["o2"])
                        stt(o1[0:64, :], o2[0:64, :], lamt[0:64, 4:5], o1[0:64, :], ALU.mult, ALU.add,
                            ["o1", "o2", "lamt"], ["o1"])
                        tt("dve", osq[0:64, :], o1[0:64, :], o1[0:64, :], ALU.mult, ["o1"], ["osq"])
                        mm(ps[0:64, 6, :], cb[0:64, CB_ONES:CB_ONES + 64], osq[0:64, :], True, True,
                           ["c_cb", "osq"], [("ps", 6)])
                        act(bcsb[0:64, :], ps[0:64, 6, :], AF.Sqrt, [("ps", 6)], ["bcsb"],
                            bias=cf[0:64, CF_EPS:CF_EPS + 1], scale=1.0 / 64)
                        recip(bcsb[0:64, :], bcsb[0:64, :], ["bcsb"], ["bcsb"])
                        stt(dst, o1[0:64, :], lamt[0:64, 5:6], bcsb[0:64, :], ALU.mult, ALU.mult,
                            ["o1", "bcsb", "lamt"], dtok)

        def moba_gate(pidx):
            def step(qt):
                b = qt // 2
                qs = slice(qt * 128, (qt + 1) * 128)
                gc = 0
                gbs = (3, 6)
                for hl in range(2):
                    base = hl * 64
                    mm(ps[:, gbs[hl], 0:8], qk[base:base + 64, 0, qs], kmean[base:base + 64, 0:8],
                       True, True, [("qk", 0, qt // 4), "kmean"], [("ps", gbs[hl])], tp=(base, 0))
                if "g0" in os.environ.get("KDEBUG_FL", ""):
                    return
                for hl in range(2):
                    tt("dve", gm[:, hl * 8:hl * 8 + 8], ps[:, gbs[hl], 0:8],
                       cf[:, CF_PAST + b * 16 + hl * 8:CF_PAST + b * 16 + hl * 8 + 8], ALU.add,
                       [("ps", gbs[hl]), "c_cf"], ["gm"])
                if "g1" in os.environ.get("KDEBUG_FL", ""):
                    return
                for hl in range(2):
                    P.add("dve", lambda e, hl=hl: e.max(top[:, hl * 8:hl * 8 + 8], gm[:, hl * 8:hl * 8 + 8]),
                          ["gm"], [("top", hl)])
                for hl in range(2):
                    stt(sel[:, hl * 8:hl * 8 + 8], gm[:, hl * 8:hl * 8 + 8], top[:, hl * 8 + 2:hl * 8 + 3],
                        cf[:, CF_A30 + b * 16 + hl * 8:CF_A30 + b * 16 + hl * 8 + 8], ALU.is_ge, ALU.mult,
                        ["gm", ("top", hl), "c_cf"], [("sel", hl)])
                mi = qt % 2
                tt("dve", mneg[:, mi, :].rearrange("p (h n) -> p h n", n=64)[:, :, 0:8],
                   sel[:, :].rearrange("p (h n) -> p h n", n=8),
                   cf[:, CF_BC + b * 16:CF_BC + b * 16 + 16].rearrange("p (h n) -> p h n", n=8), ALU.add,
                   [("sel", 0), ("sel", 1), "c_cf", "mneg"], [("mneg", mi)])
                if "g2" in os.environ.get("KDEBUG_FL", ""):
                    return
                pc = (qt % 8) * 128
                ptok = ("psb", (qt % 8) // 4)
                P.add("pe", lambda e, mi=mi, pc=pc: e.transpose(psb[:, pc:pc + 128], mneg[:, mi, :], ident),
                      [("mneg", mi), "c_cb"], [ptok])
                if "g3" in os.environ.get("KDEBUG_FL", ""):
                    return
                if qt % 4 == 3:
                    c = qt // 4
                    hc = ((qt % 8) // 4) * 512
                    copy("act", mnegt[:, c * 512:(c + 1) * 512], psb[:, hc:hc + 512], [ptok], [("mnegt", c)])
            return step

        def swa(pidx, li):
            cp = pidx - 4
            for hl in range(2):
                h = 2 * cp + hl
                base = hl * 64
                for c in range(4):
                    ob = 4 + ((hl * 4 + c) % 2)
                    steps = []
                    for qi in range(4):
                        qt = 4 * c + qi
                        qs = slice(qt * 128, (qt + 1) * 128)
                        sbk = qt % 4
                        pi = qt % 3

                        def st_fn(qt=qt, qs=qs, sbk=sbk):
                            mm(ps[:, sbk, 128:256], qk[base:base + 64, 1, qs], qk[base:base + 64, 0, qs], True, True,
                               [("qk", 1, qt // 4), ("qk", 0, qt // 4)], [("ps", sbk)], tp=(base, 0))
                            if qt > 0:
                                ks = slice((qt - 1) * 128, qt * 128)
                                mm(ps[:, sbk, 0:128], qk[base:base + 64, 1, ks], qk[base:base + 64, 0, qs], True, True,
                                   [("qk", 1, (qt - 1) // 4), ("qk", 0, qt // 4)], [("ps", sbk)], tp=(base, 0))

                        def ex_fn(qt=qt, sbk=sbk, pi=pi):
                            act(pt[:, pi, 128:256], ps[:, sbk, 128:256], AF.Exp, [("ps", sbk), "c_cf"], [("pt", pi)],
                                bias=cf[:, CF_TBC + h * 2 + 1:CF_TBC + h * 2 + 2], scale=0.125)
                            if qt > 0:
                                act(pt[:, pi, 0:128], ps[:, sbk, 0:128], AF.Exp, [("ps", sbk), "c_cf"], [("pt", pi)],
                                    bias=cf[:, CF_TBC + h * 2:CF_TBC + h * 2 + 1], scale=0.125)
                                tt("pool", pt[:, pi, 0:256], pt[:, pi, 0:256], maskc, ALU.mult,
                                   [("pt", pi), "c_cb"], [("pt", pi)])
                            else:
                                tt("pool", pt[:, pi, 128:256], pt[:, pi, 128:256], tri, ALU.mult,
                                   [("pt", pi), "c_cb"], [("pt", pi)])

                        def pv_fn(qt=qt, qi=qi, pi=pi):
                            oc = slice(qi * 128, (qi + 1) * 128)
                            if qt > 0:
                                mm(ps[0:65, ob, oc], vaug[:, qt - 1, hl, :], pt[:, pi, 0:128], True, False,
                                   [("v", qt - 1), ("pt", pi)], [("ps", ob)])
                                mm(ps[0:65, ob, oc], vaug[:, qt, hl, :], pt[:, pi, 128:256], False, True,
                                   [("v", qt), ("pt", pi)], [("ps", ob)])
                            else:
                                mm(ps[0:65, ob, oc], vaug[:, qt, hl, :], pt[:, pi, 128:256], True, True,
                                   [("v", qt), ("pt", pi)], [("ps", ob)])
                        steps.append((st_fn, ex_fn, pv_fn))
                    LOOK = 2
                    for i in range(4 + LOOK):
                        if i < 4:
                            steps[i][0]()
                            steps[i][1]()
                        if i - LOOK >= 0:
                            steps[i - LOOK][2]()
                    cs = slice(c * 512, (c + 1) * 512)
                    normalize(ob, mix[hl * 64:(hl + 1) * 64, pidx % 2, cs], [("mix", pidx % 2, c, hl)], h_sink=h)

        def w_out(wv, tw):
            n = 0
            for tc in range(4):
                cs = slice(tc * 512, (tc + 1) * 512)
                for dc in range(8):
                    b = n % 4
                    n += 1
                    for rc in range(2):
                        mm(ps[:, b, :], wv[:, rc, dc * 128:(dc + 1) * 128], mix[:, rc, cs], rc == 0, rc == 1,
                           [tw, ("mix", rc, tc, 0), ("mix", rc, tc, 1)], [("ps", b)])
                    tt("dve", xT[:, dc, cs], ps[:, b, :], xT[:, dc, cs], ALU.add, [("ps", b), xtok(dc, tc)],
                       [xtok(dc, tc)])

        def layer_params(li, ltrue):
            lam_init = 0.8 - 0.6 * math.exp(-0.3 * ltrue)
            base = PR_LAM + li * 128
            tt("dve", lamp[:, 0, :], par[:, base:base + 32], par[:, base + 32:base + 64], ALU.mult, ["c_par", "lamp"], ["lamp"])
            tt("dve", lamp[:, 1, :], par[:, base + 64:base + 96], par[:, base + 96:base + 128], ALU.mult, ["c_par", "lamp"], ["lamp"])
            P.add("dve", lambda e: e.reduce_sum(lamt[:, 0:2], lamp[:, :, :], AX.X), ["lamp", "lamt"], ["lamt"])
            act(lamt[:, 2:4], lamt[:, 0:2], AF.Exp, ["lamt"], ["lamt"])
            tt("dve", lamt[:, 4:5], lamt[:, 3:4], lamt[:, 2:3], ALU.subtract, ["lamt"], ["lamt"])
            ts("dve", lamt[:, 4:5], lamt[:, 4:5], -lam_init, None, ALU.add, None, ["lamt"], ["lamt"])
            ts("dve", lamt[:, 5:6], par[:, PR_SUB + li:PR_SUB + li + 1], 1.0 - lam_init, None, ALU.mult, None,
               ["c_par", "lamt"], ["lamt"])
            dma("sp", sinkrow[64:65, :], posd, "sink", ["sinkrow"], ["sinkrow"])
            for h in range(8):
                act(sinkrow[64:65, h * 128:(h + 1) * 128], sinkrow[64:65, h * 128:(h + 1) * 128], AF.Exp,
                    ["sinkrow", "c_par"], ["sinkrow"], bias=par[64:65, PR_SINK + li * 8 + h:PR_SINK + li * 8 + h + 1],
                    scale=1.0)

        for li, ltrue in enumerate(layer_ids):
            P.epoch = li
            if stop_after == "pro":
                break
            layer_params(li, ltrue)
            if stop_after == "params":
                break
            rmsnorm(PR_G + (li * 3 + 0) * 8)
            if stop_after == "norm":
                break
            ffn()
            if stop_after == "ffn1":
                break
            rmsnorm(PR_G + (li * 3 + 1) * 8)
            for pidx in range(8):
                kind = "A" if pidx < 2 else ("B" if pidx < 4 else "C")
                nl = 3 if pidx % 2 == 1 else 2
                got = take(nl)
                (wqk, tqk), (wvv, tvv) = got[0], got[1]
                SUB = os.environ.get("KDEBUG_SUB", "")
                if SUB == "take":
                    break
                proj_qk(wqk, tqk, kind)
                if SUB == "qk":
                    break
                proj_v(wvv, tvv, moba_gate(pidx) if (kind == "A" and SUB != "v") else None)
                if SUB in ("v", "gate"):
                    break
                if kind == "A":
                    attn_full("A", pidx, li)
                elif kind == "B":
                    attn_full("B", pidx - 2, li)
                else:
                    swa(pidx, li)
                if pidx % 2 == 1:
                    w_out(got[2][0], got[2][1])
                if stop_after == ("pair", pidx):
                    break
            if stop_after is not None:
                break
            rmsnorm(PR_G + (li * 3 + 2) * 8)
            ffn()
        if do_final and stop_after is None:
            rmsnorm(PR_G + 96, final=True)
        outs = []
        for kc in range(8):
            outs.append(dma("sp", outd[kc * 128:(kc + 1) * 128, :], xT[:, kc, :], "out",
                            [xtok(kc, tc) for tc in range(4)], [("outdone", kc)]))
        P.add("sp", lambda e: e.nop(), [("outdone", kc) for kc in range(8)] + [("slot", s_) for s_ in range(NSLOT)], [])
        stats = P.emit(nc, st)
    return nc, stats


def _consts():
    cbm = np.zeros((128, CB_N), np.float32)
    k = np.arange(128)[:, None]
    q = np.arange(128)[None, :]
    cbm[:, CB_MASKC:CB_MASKC + 128] = (q < k)
    cbm[:, CB_TRI:CB_TRI + 128] = (q >= k)
    cbm[:, CB_ID:CB_ID + 128] = np.eye(128)
    E = np.zeros((128, 8, 128), np.float32)
    for hl in range(2):
        for n in range(8):
            E[hl * 64 + n, n, :] = 1.0
    cbm[:, CB_E:CB_E + 1024] = E.reshape(128, 1024)
    cbm[:, CB_ONES:CB_ONES + 128] = 1.0
    cf = np.zeros((128, CF_N), np.float64)
    p = np.arange(128, dtype=np.float64)
    ab = np.concatenate([SL_A, SL_B])
    for s in range(8):
        for d in range(-12, 4):
            cf[:, CF_TBAB + s * 16 + d + 12] = ab[s] * (p + 128.0 * d)
    for h in range(8):
        cf[:, CF_TBC + h * 2] = SL_C[h] * (p - 192.0)
        cf[:, CF_TBC + h * 2 + 1] = SL_C[h] * (p - 64.0)
    for b in range(8):
        for hl in range(2):
            for n in range(8):
                cf[:, CF_PAST + b * 16 + hl * 8 + n] = 0.0 if n < b else -1e30
                cf[:, CF_A30 + b * 16 + hl * 8 + n] = 30000.0 if n < b else 0.0
                cf[:, CF_BC + b * 16 + hl * 8 + n] = 0.0 if n == b else -30000.0
    cf[:, CF_ONES:CF_ONES + 64] = 1.0
    cf[:, CF_EPS] = EPS
    pos = np.zeros((1, 1024), np.float64)
    for h in range(8):
        pos[0, h * 128:(h + 1) * 128] = SL_C[h] * (np.arange(128) - 64.0)
    return cbm.astype(ml_dtypes.bfloat16), cf.astype(np.float32), pos.astype(np.float32)


def _perm_win():
    cols = []
    for j in range(2):
        cols += list(range(128 * j, 128 * j + 128)) + list(range(256 + 128 * j, 256 + 128 * j + 128)) + \
            list(range(512 + 128 * j, 512 + 128 * j + 128))
    for j in range(2):
        cols += list(range(768 + 128 * j, 768 + 128 * j + 128)) + list(range(1024 + 128 * j, 1024 + 128 * j + 128)) + \
            list(range(1280 + 128 * j, 1280 + 128 * j + 128))
    for j in range(4):
        kv = j // 2
        kc = list(range(2048 + 64 * kv, 2048 + 64 * kv + 64))
        vc = list(range(2176 + 64 * kv, 2176 + 64 * kv + 64))
        cols += list(range(1536 + 128 * j, 1536 + 128 * j + 128)) + kc + kc + vc + vc
    return np.asarray(cols, np.int64)


def _params(inp, layer_ids):
    par = np.zeros((128, PR_N), np.float32)

    def gl(v):
        return np.ascontiguousarray(np.asarray(v, np.float32).reshape(8, 128).T)
    for li, l in enumerate(layer_ids):
        par[:, PR_G + (li * 3 + 0) * 8:PR_G + (li * 3 + 0) * 8 + 8] = gl(inp["norm_ffn1"][l])
        par[:, PR_G + (li * 3 + 1) * 8:PR_G + (li * 3 + 1) * 8 + 8] = gl(inp["norm_mix"][l])
        par[:, PR_G + (li * 3 + 2) * 8:PR_G + (li * 3 + 2) * 8 + 8] = gl(inp["norm_ffn2"][l])
        par[:, PR_SUB + li] = np.tile(np.asarray(inp["diff_subln"][l], np.float32), 2)
        for j, nm in enumerate(("lam_q1", "lam_k1", "lam_q2", "lam_k2")):
            par[:, PR_LAM + li * 128 + j * 32:PR_LAM + li * 128 + j * 32 + 32] = np.asarray(inp[nm][l], np.float32)[None, :]
        par[:, PR_SINK + li * 8:PR_SINK + li * 8 + 8] = np.asarray(inp["sinks"][l], np.float32)[None, :]
    par[:, PR_G + 96:PR_G + 104] = gl(inp["final_norm"])
    return par


_CACHE = {}


def _get_nc(layer_ids, do_final, stop_after=None):
    key = (tuple(layer_ids), do_final, stop_after)
    if key not in _CACHE:
        _CACHE[key] = build(list(layer_ids), do_final, stop_after)[0]
    return _CACHE[key]


def run_layers(xT_list, inp, layer_ids, do_final, stop_after=None, core_ids=None):
    cbm, cf, pos = _consts()
    perm = _perm_win()
    ls = list(layer_ids)
    f32 = lambda a: np.ascontiguousarray(np.asarray(a, np.float32))
    shared = {
        "w1g": f32(inp["w1_gate"][ls]), "w1u": f32(inp["w1_up"][ls]), "w1d": f32(inp["w1_down"][ls]),
        "w2g": f32(inp["w2_gate"][ls]), "w2u": f32(inp["w2_up"][ls]), "w2d": f32(inp["w2_down"][ls]),
        "win": f32(np.asarray(inp["w_in"], np.float32)[ls][:, :, perm]), "wout": f32(inp["w_out"][ls]),
        "cb": cbm, "cf": cf, "par": _params(inp, ls), "posrow": pos,
    }
    nc = _get_nc(ls, do_final, stop_after)
    n = len(xT_list)
    in_maps = [dict(shared, xT=np.ascontiguousarray(x)) for x in xT_list]
    res = run_bass_kernel_spmd(nc, in_maps, core_ids=list(range(n)) if core_ids is None else core_ids)
    return [r["outT"] for r in res.results]


def kernel(**inputs):
    inp = {k: np.asarray(v) for k, v in inputs.items()}
    x = np.asarray(inp["x"], np.float32)
    B = x.shape[0]
    xs = [np.ascontiguousarray(x[b].T) for b in range(B)]
    if FUSED:
        outs = run_layers(xs, inp, range(DEPTH), True)
    else:
        outs = xs
        for l in range(DEPTH):
            outs = run_layers(outs, inp, [l], l == DEPTH - 1)
    return np.stack([np.ascontiguousarray(o.T) for o in outs], axis=0).astype(np.float32)
```

```python
import math
from contextlib import ExitStack
import numpy as np
import ml_dtypes
import concourse.bass as bass
import concourse.mybir as mybir
from concourse.bass_utils import run_bass_kernel_spmd

F32 = mybir.dt.float32
BF16 = mybir.dt.bfloat16
AF = mybir.ActivationFunctionType
ALU = mybir.AluOpType
AX = mybir.AxisListType

D = 1024
T = 2048
DFF = 2816
NF = DFF // 128
G = 2
import os
NG = int(os.environ.get('KDEBUG_NG', NF // G))
DEPTH = 4
EPS = 1e-6
NSLOT = 6
FUSED = True
SAME_ENGINE_SYNC = True

SLOPES = 2.0 ** (-8.0 * (np.arange(16, dtype=np.float64) + 1.0) / 16.0)
SL_C = SLOPES[0:8]
SL_B = SLOPES[8:12]
SL_A = SLOPES[12:16]

CB_MASKC = 0
CB_TRI = 128
CB_ID = 256
CB_E = 384
CB_ONES = 1408
CB_N = 1536
CF_TBAB = 0
CF_TBC = 128
CF_PAST = 144
CF_A30 = 272
CF_BC = 400
CF_ONES = 528
CF_EPS = 592
CF_N = 600
PR_G = 0
PR_SUB = 104
PR_LAM = 108
PR_SINK = 620
PR_N = 652


class Op:
    __slots__ = ("eng", "fn", "deps", "needed", "sem", "val", "dma", "epoch")


class Prog:
    ENGS = ("pe", "act", "dve", "pool", "sp")

    def __init__(self):
        self.ops = {e: [] for e in self.ENGS}
        self.lastw = {}
        self.readers = {}
        self.epoch = 0

    def add(self, eng, fn, reads=(), writes=(), dma=None):
        op = Op()
        op.eng, op.fn, op.dma, op.epoch = eng, fn, dma, self.epoch
        op.needed = False
        op.sem = None
        op.val = 0
        deps = {}
        for t in reads:
            w = self.lastw.get(t)
            if w is not None:
                deps[id(w)] = w
        for t in writes:
            w = self.lastw.get(t)
            if w is not None:
                deps[id(w)] = w
            for r in self.readers.get(t, ()):
                deps[id(r)] = r
        out = []
        for d in deps.values():
            if d is op:
                continue
            if d.eng == eng and d.dma is None:
                if eng == "pe" or eng == "sp" or not SAME_ENGINE_SYNC:
                    continue
            out.append(d)
        op.deps = out
        for t in reads:
            if isinstance(t, str) and t.startswith("c_"):
                continue
            self.readers.setdefault(t, []).append(op)
        for t in writes:
            self.lastw[t] = op
            self.readers[t] = []
        self.ops[eng].append(op)
        return op

    def emit(self, nc, stack):
        sems = {}

        def getsem(key):
            if key not in sems:
                sems[key] = stack.enter_context(nc.semaphore("s%d" % len(sems)))
            return sems[key]

        for e in self.ENGS:
            for op in self.ops[e]:
                for d in op.deps:
                    d.needed = True
        cnt = {}
        for e in self.ENGS:
            for op in self.ops[e]:
                if op.dma is not None:
                    key = ("dma", op.dma)
                    cnt[key] = cnt.get(key, 0) + 16
                    op.sem, op.val = getsem(key), cnt[key]
                elif op.needed:
                    key = (e, op.epoch)
                    cnt[key] = cnt.get(key, 0) + 1
                    op.sem, op.val = getsem(key), cnt[key]
        block = stack.enter_context(nc.Block())
        stats = {}

        def run(eng_name, eng):
            waited = {}
            nw = 0
            for op in self.ops[eng_name]:
                need = {}
                for d in op.deps:
                    k = id(d.sem)
                    if waited.get(k, 0) >= d.val:
                        continue
                    if k not in need or need[k][1] < d.val:
                        need[k] = (d.sem, d.val)
                for k, (s, v) in need.items():
                    eng.wait_ge(s, v)
                    waited[k] = v
                    nw += 1
                inst = op.fn(eng)
                if op.dma is not None:
                    inst.then_inc(op.sem, 16)
                elif op.needed:
                    inst.then_inc(op.sem, 1)
            stats[eng_name] = (len(self.ops[eng_name]), nw)

        @block.tensor
        def _(e):
            run("pe", e)

        @block.scalar
        def _(e):
            run("act", e)

        @block.vector
        def _(e):
            run("dve", e)

        @block.gpsimd
        def _(e):
            run("pool", e)

        @block.sync
        def _(e):
            run("sp", e)

        return stats


def bcast_mid(ap, n):
    l = [list(x) for x in ap.ap]
    return bass.AP(ap.tensor, ap.offset, [l[0], [0, n]] + l[1:])


def build(layer_ids, do_final, stop_after=None):
    NL = len(layer_ids)
    nc = bass.Bass("TRN2", target_bir_lowering=False)
    dr = {}

    def din(name, shape, dt=F32):
        dr[name] = nc.dram_tensor(name, list(shape), dt, kind="ExternalInput").ap()
        return dr[name]

    xin = din("xT", [D, T])
    w1g = din("w1g", [NL, D, DFF]); w1u = din("w1u", [NL, D, DFF]); w1d = din("w1d", [NL, DFF, D])
    w2g = din("w2g", [NL, D, DFF]); w2u = din("w2u", [NL, D, DFF]); w2d = din("w2d", [NL, DFF, D])
    win = din("win", [NL, D, 8 * 384]); wout = din("wout", [NL, D, D])
    cbd = din("cb", [128, CB_N], BF16); cfd = din("cf", [128, CF_N]); prd = din("par", [128, PR_N])
    posd = din("posrow", [1, 1024])
    outd = nc.dram_tensor("outT", [D, T], F32, kind="ExternalOutput").ap()

    P = Prog()
    st = ExitStack()
    with st:
        def sb(name, shape, dt):
            return st.enter_context(nc.sbuf_tensor(name, list(shape), dt))

        xT = sb("xT_sb", [128, 8, T], F32)
        hT = sb("hT", [128, 8, T], BF16)
        ring = sb("ring", [128, NSLOT, 2048], BF16)
        qk = sb("qk", [128, 2, T], BF16)
        vaug = sb("vaug", [128, 16, 2, 65], BF16)
        mix = sb("mix", [128, 2, T], BF16)
        actb = sb("actb", [128, 4096], BF16)
        pt = sb("pt", [128, 3, 1024], BF16)
        mnegt = sb("mnegt", [128, T], BF16)
        mneg = sb("mneg", [128, 2, 128], BF16)
        gm = sb("gm", [128, 16], F32)
        top = sb("top", [128, 16], F32)
        sel = sb("sel", [128, 16], F32)
        kms = sb("kms", [128, 8], F32)
        kmean = sb("kmean", [128, 8], BF16)
        rstd = sb("rstd", [128, 2, 512], F32)
        sg = sb("sg", [128, 2, 512], BF16)
        bcsb = sb("bcsb", [128, 512], F32)
        o1 = sb("o1", [128, 512], F32)
        o2 = sb("o2", [128, 512], F32)
        osq = sb("osq", [128, 512], BF16)
        rden = sb("rden", [128, 512], F32)
        sinkrow = sb("sinkrow", [128, 1024], F32)
        cb = sb("cb_sb", [128, CB_N], BF16)
        cf = sb("cf_sb", [128, CF_N], F32)
        par = sb("par_sb", [128, PR_N], F32)
        lamt = sb("lamt", [128, 16], F32)
        lamp = sb("lamp", [128, 2, 32], F32)
        ps = st.enter_context(nc.psum_tensor("ps", [128, 7, 512], F32))
        psb = st.enter_context(nc.psum_tensor("psb", [128, 1024], BF16))

        ones_bf = cb[:, CB_ONES:CB_ONES + 128]
        ident = cb[:, CB_ID:CB_ID + 128]
        tri = cb[:, CB_TRI:CB_TRI + 128]
        maskc = cb[:, CB_MASKC:CB_MASKC + 256]

        def mm(out, lhsT, rhs, start, stop, reads, writes, tp=None):
            kw = {}
            if tp is not None and tp[0] == 96:
                kw["tile_position"] = tp
            return P.add("pe", lambda e: e.matmul(out, lhsT=lhsT, rhs=rhs, start=start, stop=stop, **kw),
                         reads, writes)

        def act(out, in_, func, reads, writes, bias=None, scale=None):
            kw = {}
            if bias is not None:
                kw["bias"] = bias
            if scale is not None:
                kw["scale"] = scale
            return P.add("act", lambda e: e.activation(out, in_, func, **kw), reads, writes)

        def tt(eng, out, in0, in1, op, reads, writes):
            return P.add(eng, lambda e: e.tensor_tensor(out, in0, in1, op), reads, writes)

        def stt(out, in0, scalar, in1, op0, op1, reads, writes):
            return P.add("dve", lambda e: e.scalar_tensor_tensor(out, in0, scalar, in1, op0, op1), reads, writes)

        def ts(eng, out, in0, s1, s2, op0, op1, reads, writes):
            if op1 is None:
                return P.add(eng, lambda e: e.tensor_scalar(out, in0, s1, None, op0), reads, writes)
            return P.add(eng, lambda e: e.tensor_scalar(out, in0, s1, s2, op0, op1), reads, writes)

        def recip(out, in_, reads, writes):
            return P.add("dve", lambda e: e.reciprocal(out, in_), reads, writes)

        def copy(eng, out, in_, reads, writes):
            if eng == "act":
                return P.add("act", lambda e: e.copy(out, in_), reads, writes)
            return P.add(eng, lambda e: e.tensor_copy(out, in_), reads, writes)

        def dma(eng, out, in_, key, reads, writes):
            return P.add(eng, lambda e: e.dma_start(out=out, in_=in_), reads, writes, dma=key)

        def xtok(kc, tc):
            return ("x", kc, tc)

        loads = []

        def colblk(w, l, c0, n):
            return w[l, :, c0:c0 + n].rearrange("(kc p) n -> p kc n", p=128)

        def rowblk(w, l, r0):
            return w[l, r0:r0 + 256, :].rearrange("(rc p) n -> p rc n", p=128)

        def ffn_loads(wg, wu, wd, l):
            for g in range(NG):
                loads.append(("c", colblk(wg, l, g * 256, 256), 256))
                loads.append(("c", colblk(wu, l, g * 256, 256), 256))
                loads.append(("r", rowblk(wd, l, g * 256), 0))

        for l in range(NL):
            ffn_loads(w1g, w1u, w1d, l)
            for p in range(8):
                loads.append(("c", colblk(win, l, p * 384, 256), 256))
                loads.append(("c", colblk(win, l, p * 384 + 256, 128), 128))
                if p % 2 == 1:
                    loads.append(("r", rowblk(wout, l, (p // 2) * 256), 0))
            ffn_loads(w2g, w2u, w2d, l)
        wstate = {"rec": 0, "next": 0}

        def slot_view(i):
            kind, src, n = loads[i]
            s = i % NSLOT
            flat = ring[:, s, :]
            if kind == "c":
                return flat.rearrange("p (kc n) -> p kc n", n=256)
            return flat.rearrange("p (rc n) -> p rc n", n=1024)

        def take(n):
            a = wstate["next"]
            wstate["next"] = a + n
            upto = min(len(loads), a + NSLOT)
            while wstate["rec"] < upto:
                i = wstate["rec"]
                kind, src, ncol = loads[i]
                v = slot_view(i)
                dst = v[:, :, 0:ncol] if kind == "c" else v
                dma("pool", dst, src, ("slot", i % NSLOT), [], [("slot", i % NSLOT)])
                wstate["rec"] += 1
            return [(slot_view(i), ("slot", i % NSLOT)) for i in range(a, a + n)]

        dma("sp", cb[:, :], cbd, "cst", [], ["c_cb"])
        dma("sp", cf[:, :], cfd, "cst", [], ["c_cf"])
        dma("sp", par[:, :], prd, "cst", [], ["c_par"])
        for kc in range(8):
            dma("sp", xT[:, kc, :], xin[kc * 128:(kc + 1) * 128, :], "xin", [], [xtok(kc, tc) for tc in range(4)])
        P.add("pool", lambda e: e.memset(vaug[:, :, :, :], 1.0), [], [("v", t) for t in range(16)])
        P.add("pool", lambda e: e.memset(mneg[:, :, :], 0.0), [], ["mneg"])

        def rmsnorm(gcol, final=False):
            sq = actb[:, :].rearrange("p (k n) -> p k n", n=512)
            for tc in range(4):
                cs = slice(tc * 512, (tc + 1) * 512)
                bank = 5 + (tc % 2)
                rb = rstd[:, tc % 2, :]
                act(sq, xT[:, :, cs], AF.Square, [xtok(kc, tc) for kc in range(8)], [("act", 0), ("act", 1)])
                for kc in range(8):
                    mm(ps[:, bank, :], ones_bf, sq[:, kc, :], kc == 0, kc == 7,
                       ["c_cb", ("act", 0), ("act", 1)], [("ps", bank)])
                act(rb, ps[:, bank, :], AF.Sqrt, [("ps", bank)], [("rstd", tc % 2)], bias=cf[:, CF_EPS:CF_EPS + 1],
                    scale=1.0 / D)
                recip(rb, rb, [("rstd", tc % 2)], [("rstd", tc % 2)])
                for kc in range(8):
                    if final:
                        stt(xT[:, kc, cs], xT[:, kc, cs], par[:, gcol + kc:gcol + kc + 1], rb, ALU.mult, ALU.mult,
                            [xtok(kc, tc), ("rstd", tc % 2), "c_par"], [xtok(kc, tc)])
                    else:
                        stt(hT[:, kc, cs], xT[:, kc, cs], par[:, gcol + kc:gcol + kc + 1], rb, ALU.mult, ALU.mult,
                            [xtok(kc, tc), ("rstd", tc % 2), "c_par"], [("h", kc, tc)])


        def ffn():
            pend = None
            cnt = 0
            for g in range(NG):
                if pend is not None:
                    pend()
                    pend = None
                (wgv, tg), (wuv, tu), (wdv, td) = take(3)
                for tc in range(4):
                    cs = slice(tc * 512, (tc + 1) * 512)
                    ai = cnt % 2
                    av = actb[:, ai * 1024:(ai + 1) * 1024].rearrange("p (f n) -> p f n", n=512)
                    for fi in range(G):
                        bg, bu = 2 * (fi % 2), 2 * (fi % 2) + 1
                        for kc in range(8):
                            mm(ps[:, bg, :], wgv[:, kc, fi * 128:(fi + 1) * 128], hT[:, kc, cs], kc == 0, kc == 7,
                               [tg, ("h", kc, tc)], [("ps", bg)])
                        for kc in range(8):
                            mm(ps[:, bu, :], wuv[:, kc, fi * 128:(fi + 1) * 128], hT[:, kc, cs], kc == 0, kc == 7,
                               [tu, ("h", kc, tc)], [("ps", bu)])
                        act(sg[:, fi % 2, :], ps[:, bg, :], AF.Silu, [("ps", bg)], [("sg", fi % 2)])
                        tt("dve", av[:, fi, :], sg[:, fi % 2, :], ps[:, bu, :], ALU.mult,
                           [("sg", fi % 2), ("ps", bu)], [("act", ai)])
                    if pend is not None:
                        pend()

                    def down(av=av, ai=ai, wdv=wdv, td=td, tc=tc, cs=cs):
                        for dc in range(8):
                            b = 4 + (dc % 3)
                            for fi in range(G):
                                mm(ps[:, b, :], wdv[:, fi, dc * 128:(dc + 1) * 128], av[:, fi, :], fi == 0, fi == G - 1,
                                   [td, ("act", ai)], [("ps", b)])
                            stt(xT[:, dc, cs], ps[:, b, :], 0.5, xT[:, dc, cs], ALU.mult, ALU.add,
                                [("ps", b), xtok(dc, tc)], [xtok(dc, tc)])
                    pend = down
                    cnt += 1
            if pend is not None:
                pend()

        def proj_qk(wv, tw, kind):
            n = 0
            for j in range(2):
                for tc in range(4):
                    cs = slice(tc * 512, (tc + 1) * 512)
                    b = n % 4
                    n += 1
                    for kc in range(8):
                        mm(ps[:, b, :], wv[:, kc, j * 128:(j + 1) * 128], hT[:, kc, cs], kc == 0, kc == 7,
                           [tw, ("h", kc, tc)], [("ps", b)])
                    FL = os.environ.get("KDEBUG_FL", "")
                    copy("act" if (n % 2 and "dvecopy" not in FL) else "dve", qk[:, j, cs], ps[:, b, :], [("ps", b)], [("qk", j, tc)])
                    if kind == "A" and j == 1 and "nored" not in FL:
                        P.add("dve", lambda e, tc=tc, cs=cs: e.reduce_sum(
                            kms[:, 2 * tc:2 * tc + 2], qk[:, 1, cs].rearrange("p (n l) -> p n l", l=256), AX.X),
                            [("qk", 1, tc)], ["kms"])
            if kind == "A":
                copy("dve", kmean[:, :], kms[:, :], ["kms"], ["kmean"])

        def proj_v(wv, tw, per_t=None):
            for t in range(16):
                b = 4 + (t % 2)
                cs = slice(t * 128, (t + 1) * 128)
                oc = slice(0, 128)
                for kc in range(8):
                    mm(ps[:, b, oc], hT[:, kc, cs], wv[:, kc, 0:128], kc == 0, kc == 7,
                       [tw, ("h", kc, t // 4)], [("ps", b)])
                copy("act" if t % 2 else "dve", vaug[:, t, :, 0:64],
                     ps[:, b, oc].rearrange("p (h d) -> p h d", d=64), [("ps", b)], [("v", t)])
                if per_t is not None:
                    per_t(t)

        def normalize(ob, dst, dtok, h_sink=None, to=None):
            if h_sink is not None:
                srow = bcast_mid(sinkrow[64:65, h_sink * 128:(h_sink + 1) * 128], 4)
                tt("dve", rden[64:65, :].rearrange("p (a b) -> p a b", b=128),
                   ps[64:65, ob, :].rearrange("p (a b) -> p a b", b=128), srow, ALU.add,
                   [("ps", ob), "sinkrow"], ["rden"])
                recip(rden[64:65, :], rden[64:65, :], ["rden"], ["rden"])
            else:
                recip(rden[64:65, :], ps[64:65, ob, :], [("ps", ob)], ["rden"])
            mm(ps[0:64, 6, :], cf[64:65, CF_ONES:CF_ONES + 64], rden[64:65, :], True, True,
               ["c_cf", "rden"], [("ps", 6)])
            copy("act", bcsb[0:64, :], ps[0:64, 6, :], [("ps", 6)], ["bcsb"])
            tt("dve", dst, ps[0:64, ob, :], bcsb[0:64, :], ALU.mult, [("ps", ob), "bcsb"], dtok)

        def attn_full(kind, pidx, li):
            nmap = 2 if kind == "B" else 1
            scale = 32 ** -0.5 if kind == "B" else 0.125
            for hl in range(2):
                h = 2 * pidx + hl
                tbcol = CF_TBAB + (h if kind == "A" else 4 + h) * 16
                for c in range(4):
                    nkt = 4 * c + 4
                    obanks = [4, 5] if kind == "B" else [4 + ((hl * 4 + c) % 2)]
                    steps = []
                    for kt in range(nkt):
                        j = kt - 4 * c
                        col0 = max(j, 0) * 128
                        ncols = 512 - col0
                        qs = slice(c * 512 + col0, (c + 1) * 512)
                        ks = slice(kt * 128, (kt + 1) * 128)
                        pi = kt % 3
                        if kind == "A":
                            sb_ = kt % 3
                            stb = [sb_]
                        else:
                            sb_ = 2 * (kt % 2)
                            stb = [sb_, sb_ + 1]

                        def st_fn(kt=kt, j=j, col0=col0, ncols=ncols, qs=qs, ks=ks, stb=stb):
                            if kind == "A":
                                base = hl * 64
                                mm(ps[:, stb[0], 0:ncols], qk[base:base + 64, 1, ks], qk[base:base + 64, 0, qs],
                                   True, False, [("qk", 1, kt // 4), ("qk", 0, c)], [("ps", stb[0])], tp=(base, 0))
                                mm(ps[:, stb[0], 0:ncols],
                                   cb[base:base + 64, CB_E + (kt // 2) * 128:CB_E + (kt // 2) * 128 + 128],
                                   mnegt[base:base + 64, qs], False, True,
                                   ["c_cb", ("mnegt", c)], [("ps", stb[0])], tp=(base, 0))
                            else:
                                for m in range(2):
                                    base = hl * 64 + m * 32
                                    mm(ps[:, stb[m], 0:ncols], qk[base:base + 32, 1, ks], qk[base:base + 32, 0, qs],
                                       True, True, [("qk", 1, kt // 4), ("qk", 0, c)], [("ps", stb[m])], tp=(base, 0))

                        def ex_fn(kt=kt, j=j, ncols=ncols, stb=stb, pi=pi):
                            bias = cf[:, tbcol + j + 12:tbcol + j + 13]
                            if kind == "A":
                                act(pt[:, pi, 0:ncols], ps[:, stb[0], 0:ncols], AF.Exp, [("ps", stb[0]), "c_cf"],
                                    [("pt", pi)], bias=bias, scale=scale)
                                if j >= 0:
                                    tt("pool", pt[:, pi, 0:128], pt[:, pi, 0:128], tri, ALU.mult,
                                       [("pt", pi), "c_cb"], [("pt", pi)])
                            else:
                                pv = pt[:, pi, :].rearrange("p (m n) -> p m n", n=512)
                                act(pv[:, :, 0:ncols], ps[:, stb[0]:stb[0] + 2, 0:ncols], AF.Exp,
                                    [("ps", stb[0]), ("ps", stb[1]), "c_cf"], [("pt", pi)], bias=bias, scale=scale)
                                if j >= 0:
                                    for m in range(2):
                                        tt("pool", pv[:, m, 0:128], pv[:, m, 0:128], tri, ALU.mult,
                                           [("pt", pi), "c_cb"], [("pt", pi)])

                        def pv_fn(kt=kt, col0=col0, ncols=ncols, pi=pi):
                            for m in range(nmap):
                                rhs = pt[:, pi, m * 512:m * 512 + ncols]
                                mm(ps[0:65, obanks[m], col0:512], vaug[:, kt, hl, :], rhs, kt == 0, kt == nkt - 1,
                                   [("v", kt), ("pt", pi)], [("ps", obanks[m])])
                        steps.append((st_fn, ex_fn, pv_fn))
                    LOOK = 2 if kind == "A" else 1
                    for i in range(nkt + LOOK):
                        if i < nkt:
                            steps[i][0]()
                            steps[i][1]()
                        if i - LOOK >= 0:
                            steps[i - LOOK][2]()
                    cs = slice(c * 512, (c + 1) * 512)
                    dst = mix[hl * 64:(hl + 1) * 64, pidx % 2, cs]
                    dtok = [("mix", pidx % 2, c, hl)]
                    if kind == "A":
                        normalize(obanks[0], dst, dtok)
                    else:
                        normalize(4, o1[0:64, :], ["o1"])
                        normalize(5, o2[0:64, :], ["o2"])
                        stt(o1[0:64, :], o2[0:64, :], lamt[0:64, 4:5], o1[0:64, :], ALU.mult, ALU.add,
                            ["o1", "o2", "lamt"], ["o1"])
                        tt("dve", osq[0:64, :], o1[0:64, :], o1[0:64, :], ALU.mult, ["o1"], ["osq"])
                        mm(ps[0:64, 6, :], cb[0:64, CB_ONES:CB_ONES + 64], osq[0:64, :], True, True,
                           ["c_cb", "osq"], [("ps", 6)])
                        act(bcsb[0:64, :], ps[0:64, 6, :], AF.Sqrt, [("ps", 6)], ["bcsb"],
                            bias=cf[0:64, CF_EPS:CF_EPS + 1], scale=1.0 / 64)
                        recip(bcsb[0:64, :], bcsb[0:64, :], ["bcsb"], ["bcsb"])
                        stt(dst, o1[0:64, :], lamt[0:64, 5:6], bcsb[0:64, :], ALU.mult, ALU.mult,
                            ["o1", "bcsb", "lamt"], dtok)

        def moba_gate(pidx):
            def step(qt):
                b = qt // 2
                qs = slice(qt * 128, (qt + 1) * 128)
                gc = 0
                gbs = (3, 6)
                for hl in range(2):
                    base = hl * 64
                    mm(ps[:, gbs[hl], 0:8], qk[base:base + 64, 0, qs], kmean[base:base + 64, 0:8],
                       True, True, [("qk", 0, qt // 4), "kmean"], [("ps", gbs[hl])], tp=(base, 0))
                if "g0" in os.environ.get("KDEBUG_FL", ""):
                    return
                for hl in range(2):
                    tt("dve", gm[:, hl * 8:hl * 8 + 8], ps[:, gbs[hl], 0:8],
                       cf[:, CF_PAST + b * 16 + hl * 8:CF_PAST + b * 16 + hl * 8 + 8], ALU.add,
                       [("ps", gbs[hl]), "c_cf"], ["gm"])
                if "g1" in os.environ.get("KDEBUG_FL", ""):
                    return
                for hl in range(2):
                    P.add("dve", lambda e, hl=hl: e.max(top[:, hl * 8:hl * 8 + 8], gm[:, hl * 8:hl * 8 + 8]),
                          ["gm"], [("top", hl)])
                for hl in range(2):
                    stt(sel[:, hl * 8:hl * 8 + 8], gm[:, hl * 8:hl * 8 + 8], top[:, hl * 8 + 2:hl * 8 + 3],
                        cf[:, CF_A30 + b * 16 + hl * 8:CF_A30 + b * 16 + hl * 8 + 8], ALU.is_ge, ALU.mult,
                        ["gm", ("top", hl), "c_cf"], [("sel", hl)])
                mi = qt % 2
                tt("dve", mneg[:, mi, :].rearrange("p (h n) -> p h n", n=64)[:, :, 0:8],
                   sel[:, :].rearrange("p (h n) -> p h n", n=8),
                   cf[:, CF_BC + b * 16:CF_BC + b * 16 + 16].rearrange("p (h n) -> p h n", n=8), ALU.add,
                   [("sel", 0), ("sel", 1), "c_cf", "mneg"], [("mneg", mi)])
                if "g2" in os.environ.get("KDEBUG_FL", ""):
                    return
                pc = (qt % 8) * 128
                ptok = "psb"
                P.add("pe", lambda e, mi=mi, pc=pc: e.transpose(psb[:, pc:pc + 128], mneg[:, mi, :], ident),
                      [("mneg", mi), "c_cb"], [ptok])
                if "g3" in os.environ.get("KDEBUG_FL", ""):
                    return
                if qt % 4 == 3:
                    c = qt // 4
                    hc = ((qt % 8) // 4) * 512
                    copy("act", mnegt[:, c * 512:(c + 1) * 512], psb[:, hc:hc + 512], [ptok], [("mnegt", c)])
            return step

        def swa(pidx, li):
            cp = pidx - 4
            for hl in range(2):
                h = 2 * cp + hl
                base = hl * 64
                for c in range(4):
                    ob = 4 + ((hl * 4 + c) % 2)
                    steps = []
                    for qi in range(4):
                        qt = 4 * c + qi
                        qs = slice(qt * 128, (qt + 1) * 128)
                        sbk = qt % 4
                        pi = qt % 3

                        def st_fn(qt=qt, qs=qs, sbk=sbk):
                            mm(ps[:, sbk, 128:256], qk[base:base + 64, 1, qs], qk[base:base + 64, 0, qs], True, True,
                               [("qk", 1, qt // 4), ("qk", 0, qt // 4)], [("ps", sbk)], tp=(base, 0))
                            if qt > 0:
                                ks = slice((qt - 1) * 128, qt * 128)
                                mm(ps[:, sbk, 0:128], qk[base:base + 64, 1, ks], qk[base:base + 64, 0, qs], True, True,
                                   [("qk", 1, (qt - 1) // 4), ("qk", 0, qt // 4)], [("ps", sbk)], tp=(base, 0))

                        def ex_fn(qt=qt, sbk=sbk, pi=pi):
                            act(pt[:, pi, 128:256], ps[:, sbk, 128:256], AF.Exp, [("ps", sbk), "c_cf"], [("pt", pi)],
                                bias=cf[:, CF_TBC + h * 2 + 1:CF_TBC + h * 2 + 2], scale=0.125)
                            if qt > 0:
                                act(pt[:, pi, 0:128], ps[:, sbk, 0:128], AF.Exp, [("ps", sbk), "c_cf"], [("pt", pi)],
                                    bias=cf[:, CF_TBC + h * 2:CF_TBC + h * 2 + 1], scale=0.125)
                                tt("pool", pt[:, pi, 0:256], pt[:, pi, 0:256], maskc, ALU.mult,
                                   [("pt", pi), "c_cb"], [("pt", pi)])
                            else:
                                tt("pool", pt[:, pi, 128:256], pt[:, pi, 128:256], tri, ALU.mult,
                                   [("pt", pi), "c_cb"], [("pt", pi)])

                        def pv_fn(qt=qt, qi=qi, pi=pi):
                            oc = slice(qi * 128, (qi + 1) * 128)
                            if qt > 0:
                                mm(ps[0:65, ob, oc], vaug[:, qt - 1, hl, :], pt[:, pi, 0:128], True, False,
                                   [("v", qt - 1), ("pt", pi)], [("ps", ob)])
                                mm(ps[0:65, ob, oc], vaug[:, qt, hl, :], pt[:, pi, 128:256], False, True,
                                   [("v", qt), ("pt", pi)], [("ps", ob)])
                            else:
                                mm(ps[0:65, ob, oc], vaug[:, qt, hl, :], pt[:, pi, 128:256], True, True,
                                   [("v", qt), ("pt", pi)], [("ps", ob)])
                        steps.append((st_fn, ex_fn, pv_fn))
                    LOOK = 2
                    for i in range(4 + LOOK):
                        if i < 4:
                            steps[i][0]()
                            steps[i][1]()
                        if i - LOOK >= 0:
                            steps[i - LOOK][2]()
                    cs = slice(c * 512, (c + 1) * 512)
                    normalize(ob, mix[hl * 64:(hl + 1) * 64, pidx % 2, cs], [("mix", pidx % 2, c, hl)], h_sink=h)

        def w_out(wv, tw):
            n = 0
            for tc in range(4):
                cs = slice(tc * 512, (tc + 1) * 512)
                for dc in range(8):
                    b = n % 4
                    n += 1
                    for rc in range(2):
                        mm(ps[:, b, :], wv[:, rc, dc * 128:(dc + 1) * 128], mix[:, rc, cs], rc == 0, rc == 1,
                           [tw, ("mix", rc, tc, 0), ("mix", rc, tc, 1)], [("ps", b)])
                    tt("dve", xT[:, dc, cs], ps[:, b, :], xT[:, dc, cs], ALU.add, [("ps", b), xtok(dc, tc)],
                       [xtok(dc, tc)])

        def layer_params(li, ltrue):
            lam_init = 0.8 - 0.6 * math.exp(-0.3 * ltrue)
            base = PR_LAM + li * 128
            tt("dve", lamp[:, 0, :], par[:, base:base + 32], par[:, base + 32:base + 64], ALU.mult, ["c_par", "lamp"], ["lamp"])
            tt("dve", lamp[:, 1, :], par[:, base + 64:base + 96], par[:, base + 96:base + 128], ALU.mult, ["c_par", "lamp"], ["lamp"])
            P.add("dve", lambda e: e.reduce_sum(lamt[:, 0:2], lamp[:, :, :], AX.X), ["lamp", "lamt"], ["lamt"])
            act(lamt[:, 2:4], lamt[:, 0:2], AF.Exp, ["lamt"], ["lamt"])
            tt("dve", lamt[:, 4:5], lamt[:, 3:4], lamt[:, 2:3], ALU.subtract, ["lamt"], ["lamt"])
            ts("dve", lamt[:, 4:5], lamt[:, 4:5], -lam_init, None, ALU.add, None, ["lamt"], ["lamt"])
            ts("dve", lamt[:, 5:6], par[:, PR_SUB + li:PR_SUB + li + 1], 1.0 - lam_init, None, ALU.mult, None,
               ["c_par", "lamt"], ["lamt"])
            dma("sp", sinkrow[64:65, :], posd, "sink", ["sinkrow"], ["sinkrow"])
            for h in range(8):
                act(sinkrow[64:65, h * 128:(h + 1) * 128], sinkrow[64:65, h * 128:(h + 1) * 128], AF.Exp,
                    ["sinkrow", "c_par"], ["sinkrow"], bias=par[64:65, PR_SINK + li * 8 + h:PR_SINK + li * 8 + h + 1],
                    scale=1.0)

        for li, ltrue in enumerate(layer_ids):
            P.epoch = li
            if stop_after == "pro":
                break
            layer_params(li, ltrue)
            if stop_after == "params":
                break
            rmsnorm(PR_G + (li * 3 + 0) * 8)
            if stop_after == "norm":
                break
            ffn()
            if stop_after == "ffn1":
                break
            rmsnorm(PR_G + (li * 3 + 1) * 8)
            for pidx in range(8):
                kind = "A" if pidx < 2 else ("B" if pidx < 4 else "C")
                nl = 3 if pidx % 2 == 1 else 2
                got = take(nl)
                (wqk, tqk), (wvv, tvv) = got[0], got[1]
                SUB = os.environ.get("KDEBUG_SUB", "")
                if SUB == "take":
                    break
                proj_qk(wqk, tqk, kind)
                if SUB == "qk":
                    break
                proj_v(wvv, tvv, moba_gate(pidx) if (kind == "A" and SUB != "v") else None)
                if SUB in ("v", "gate"):
                    break
                if kind == "A":
                    attn_full("A", pidx, li)
                elif kind == "B":
                    attn_full("B", pidx - 2, li)
                else:
                    swa(pidx, li)
                if pidx % 2 == 1:
                    w_out(got[2][0], got[2][1])
                if stop_after == ("pair", pidx):
                    break
            if stop_after is not None:
                break
            rmsnorm(PR_G + (li * 3 + 2) * 8)
            ffn()
        if do_final and stop_after is None:
            rmsnorm(PR_G + 96, final=True)
        outs = []
        for kc in range(8):
            outs.append(dma("sp", outd[kc * 128:(kc + 1) * 128, :], xT[:, kc, :], "out",
                            [xtok(kc, tc) for tc in range(4)], [("outdone", kc)]))
        P.add("sp", lambda e: e.nop(), [("outdone", kc) for kc in range(8)] + [("slot", s_) for s_ in range(NSLOT)], [])
        stats = P.emit(nc, st)
    return nc, stats


def _consts():
    cbm = np.zeros((128, CB_N), np.float32)
    k = np.arange(128)[:, None]
    q = np.arange(128)[None, :]
    cbm[:, CB_MASKC:CB_MASKC + 128] = (q < k)
    cbm[:, CB_TRI:CB_TRI + 128] = (q >= k)
    cbm[:, CB_ID:CB_ID + 128] = np.eye(128)
    E = np.zeros((128, 8, 128), np.float32)
    for hl in range(2):
        for n in range(8):
            E[hl * 64 + n, n, :] = 1.0
    cbm[:, CB_E:CB_E + 1024] = E.reshape(128, 1024)
    cbm[:, CB_ONES:CB_ONES + 128] = 1.0
    cf = np.zeros((128, CF_N), np.float64)
    p = np.arange(128, dtype=np.float64)
    ab = np.concatenate([SL_A, SL_B])
    for s in range(8):
        for d in range(-12, 4):
            cf[:, CF_TBAB + s * 16 + d + 12] = ab[s] * (p + 128.0 * d)
    for h in range(8):
        cf[:, CF_TBC + h * 2] = SL_C[h] * (p - 192.0)
        cf[:, CF_TBC + h * 2 + 1] = SL_C[h] * (p - 64.0)
    for b in range(8):
        for hl in range(2):
            for n in range(8):
                cf[:, CF_PAST + b * 16 + hl * 8 + n] = 0.0 if n < b else -1e30
                cf[:, CF_A30 + b * 16 + hl * 8 + n] = 30000.0 if n < b else 0.0
                cf[:, CF_BC + b * 16 + hl * 8 + n] = 0.0 if n == b else -30000.0
    cf[:, CF_ONES:CF_ONES + 64] = 1.0
    cf[:, CF_EPS] = EPS
    pos = np.zeros((1, 1024), np.float64)
    for h in range(8):
        pos[0, h * 128:(h + 1) * 128] = SL_C[h] * (np.arange(128) - 64.0)
    return cbm.astype(ml_dtypes.bfloat16), cf.astype(np.float32), pos.astype(np.float32)


def _perm_win():
    cols = []
    for j in range(2):
        cols += list(range(128 * j, 128 * j + 128)) + list(range(256 + 128 * j, 256 + 128 * j + 128)) + \
            list(range(512 + 128 * j, 512 + 128 * j + 128))
    for j in range(2):
        cols += list(range(768 + 128 * j, 768 + 128 * j + 128)) + list(range(1024 + 128 * j, 1024 + 128 * j + 128)) + \
            list(range(1280 + 128 * j, 1280 + 128 * j + 128))
    for j in range(4):
        kv = j // 2
        kc = list(range(2048 + 64 * kv, 2048 + 64 * kv + 64))
        vc = list(range(2176 + 64 * kv, 2176 + 64 * kv + 64))
        cols += list(range(1536 + 128 * j, 1536 + 128 * j + 128)) + kc + kc + vc + vc
    return np.asarray(cols, np.int64)


def _params(inp, layer_ids):
    par = np.zeros((128, PR_N), np.float32)

    def gl(v):
        return np.ascontiguousarray(np.asarray(v, np.float32).reshape(8, 128).T)
    for li, l in enumerate(layer_ids):
        par[:, PR_G + (li * 3 + 0) * 8:PR_G + (li * 3 + 0) * 8 + 8] = gl(inp["norm_ffn1"][l])
        par[:, PR_G + (li * 3 + 1) * 8:PR_G + (li * 3 + 1) * 8 + 8] = gl(inp["norm_mix"][l])
        par[:, PR_G + (li * 3 + 2) * 8:PR_G + (li * 3 + 2) * 8 + 8] = gl(inp["norm_ffn2"][l])
        par[:, PR_SUB + li] = np.tile(np.asarray(inp["diff_subln"][l], np.float32), 2)
        for j, nm in enumerate(("lam_q1", "lam_k1", "lam_q2", "lam_k2")):
            par[:, PR_LAM + li * 128 + j * 32:PR_LAM + li * 128 + j * 32 + 32] = np.asarray(inp[nm][l], np.float32)[None, :]
        par[:, PR_SINK + li * 8:PR_SINK + li * 8 + 8] = np.asarray(inp["sinks"][l], np.float32)[None, :]
    par[:, PR_G + 96:PR_G + 104] = gl(inp["final_norm"])
    return par


_CACHE = {}


def _get_nc(layer_ids, do_final, stop_after=None):
    key = (tuple(layer_ids), do_final, stop_after)
    if key not in _CACHE:
        _CACHE[key] = build(list(layer_ids), do_final, stop_after)[0]
    return _CACHE[key]


def run_layers(xT_list, inp, layer_ids, do_final, stop_after=None, core_ids=None):
    cbm, cf, pos = _consts()
    perm = _perm_win()
    ls = list(layer_ids)
    f32 = lambda a: np.ascontiguousarray(np.asarray(a, np.float32))
    shared = {
        "w1g": f32(inp["w1_gate"][ls]), "w1u": f32(inp["w1_up"][ls]), "w1d": f32(inp["w1_down"][ls]),
        "w2g": f32(inp["w2_gate"][ls]), "w2u": f32(inp["w2_up"][ls]), "w2d": f32(inp["w2_down"][ls]),
        "win": f32(np.asarray(inp["w_in"], np.float32)[ls][:, :, perm]), "wout": f32(inp["w_out"][ls]),
        "cb": cbm, "cf": cf, "par": _params(inp, ls), "posrow": pos,
    }
    nc = _get_nc(ls, do_final, stop_after)
    n = len(xT_list)
    in_maps = [dict(shared, xT=np.ascontiguousarray(x)) for x in xT_list]
    res = run_bass_kernel_spmd(nc, in_maps, core_ids=list(range(n)) if core_ids is None else core_ids)
    return [r["outT"] for r in res.results]


def kernel(**inputs):
    inp = {k: np.asarray(v) for k, v in inputs.items()}
    x = np.asarray(inp["x"], np.float32)
    B = x.shape[0]
    xs = [np.ascontiguousarray(x[b].T) for b in range(B)]
    if FUSED:
        outs = run_layers(xs, inp, range(DEPTH), True)
    else:
        outs = xs
        for l in range(DEPTH):
            outs = run_layers(outs, inp, [l], l == DEPTH - 1)
    return np.stack([np.ascontiguousarray(o.T) for o in outs], axis=0).astype(np.float32)
```

```python
import math
from contextlib import ExitStack
import numpy as np
import ml_dtypes
import concourse.bass as bass
import concourse.mybir as mybir
from concourse.bass_utils import run_bass_kernel_spmd

F32 = mybir.dt.float32
BF16 = mybir.dt.bfloat16
AF = mybir.ActivationFunctionType
ALU = mybir.AluOpType
AX = mybir.AxisListType

D = 1024
T = 2048
DFF = 2816
NF = DFF // 128
G = 2
NG = NF // G
DEPTH = 4
EPS = 1e-6
NSLOT = 6
FUSED = True
SAME_ENGINE_SYNC = True

SLOPES = 2.0 ** (-8.0 * (np.arange(16, dtype=np.float64) + 1.0) / 16.0)
SL_C = SLOPES[0:8]
SL_B = SLOPES[8:12]
SL_A = SLOPES[12:16]

CB_MASKC = 0
CB_TRI = 128
CB_ID = 256
CB_E = 384
CB_ONES = 2432
CB_N = 2560
CF_TBAB = 0
CF_TBC = 128
CF_PAST = 144
CF_A30 = 272
CF_BC = 400
CF_ONES = 528
CF_EPS = 592
CF_ID = 600
CF_N = 728
PR_G = 0
PR_SUB = 104
PR_LAM = 108
PR_SINK = 620
PR_N = 652


class Op:
    __slots__ = ("eng", "fn", "deps", "needed", "sem", "val", "dma", "epoch")


class Prog:
    ENGS = ("pe", "act", "dve", "pool", "sp")

    def __init__(self):
        self.ops = {e: [] for e in self.ENGS}
        self.lastw = {}
        self.readers = {}
        self.epoch = 0

    def add(self, eng, fn, reads=(), writes=(), dma=None):
        op = Op()
        op.eng, op.fn, op.dma, op.epoch = eng, fn, dma, self.epoch
        op.needed = False
        op.sem = None
        op.val = 0
        deps = {}
        for t in reads:
            w = self.lastw.get(t)
            if w is not None:
                deps[id(w)] = w
        for t in writes:
            w = self.lastw.get(t)
            if w is not None:
                deps[id(w)] = w
            for r in self.readers.get(t, ()):
                deps[id(r)] = r
        out = []
        for d in deps.values():
            if d is op:
                continue
            if d.eng == eng and d.dma is None:
                if eng == "pe" or eng == "sp" or not SAME_ENGINE_SYNC:
                    continue
            out.append(d)
        op.deps = out
        for t in reads:
            if isinstance(t, str) and t.startswith("c_"):
                continue
            self.readers.setdefault(t, []).append(op)
        for t in writes:
            self.lastw[t] = op
            self.readers[t] = []
        self.ops[eng].append(op)
        return op

    def emit(self, nc, stack):
        sems = {}

        def getsem(key):
            if key not in sems:
                sems[key] = stack.enter_context(nc.semaphore("s%d" % len(sems)))
            return sems[key]

        for e in self.ENGS:
            for op in self.ops[e]:
                for d in op.deps:
                    d.needed = True
        cnt = {}
        for e in self.ENGS:
            for op in self.ops[e]:
                if op.dma is not None:
                    key = ("dma", op.dma)
                    cnt[key] = cnt.get(key, 0) + 16
                    op.sem, op.val = getsem(key), cnt[key]
                elif op.needed:
                    key = (e, op.epoch)
                    cnt[key] = cnt.get(key, 0) + 1
                    op.sem, op.val = getsem(key), cnt[key]
        block = stack.enter_context(nc.Block())
        stats = {}

        def run(eng_name, eng):
            waited = {}
            nw = 0
            for op in self.ops[eng_name]:
                need = {}
                for d in op.deps:
                    k = id(d.sem)
                    if waited.get(k, 0) >= d.val:
                        continue
                    if k not in need or need[k][1] < d.val:
                        need[k] = (d.sem, d.val)
                for k, (s, v) in need.items():
                    eng.wait_ge(s, v)
                    waited[k] = v
                    nw += 1
                inst = op.fn(eng)
                if op.dma is not None:
                    inst.then_inc(op.sem, 16)
                elif op.needed:
                    inst.then_inc(op.sem, 1)
            stats[eng_name] = (len(self.ops[eng_name]), nw)

        @block.tensor
        def _(e):
            run("pe", e)

        @block.scalar
        def _(e):
            run("act", e)

        @block.vector
        def _(e):
            run("dve", e)

        @block.gpsimd
        def _(e):
            run("pool", e)

        @block.sync
        def _(e):
            run("sp", e)

        return stats


def bcast_mid(ap, n):
    l = [list(x) for x in ap.ap]
    return bass.AP(ap.tensor, ap.offset, [l[0], [0, n]] + l[1:])


def build(layer_ids, do_final, stop_after=None):
    NL = len(layer_ids)
    nc = bass.Bass("TRN2", target_bir_lowering=False)
    dr = {}

    def din(name, shape, dt=F32):
        dr[name] = nc.dram_tensor(name, list(shape), dt, kind="ExternalInput").ap()
        return dr[name]

    xin = din("xT", [D, T])
    w1g = din("w1g", [NL, D, DFF]); w1u = din("w1u", [NL, D, DFF]); w1d = din("w1d", [NL, DFF, D])
    w2g = din("w2g", [NL, D, DFF]); w2u = din("w2u", [NL, D, DFF]); w2d = din("w2d", [NL, DFF, D])
    win = din("win", [NL, D, 8 * 384]); wout = din("wout", [NL, D, D])
    cbd = din("cb", [128, CB_N], BF16); cfd = din("cf", [128, CF_N]); prd = din("par", [128, PR_N])
    posd = din("posrow", [128, 1024])
    outd = nc.dram_tensor("outT", [D, T], F32, kind="ExternalOutput").ap()

    P = Prog()
    st = ExitStack()
    with st:
        def sb(name, shape, dt):
            return st.enter_context(nc.sbuf_tensor(name, list(shape), dt))

        xT = sb("xT_sb", [128, 8, T], F32)
        hT = sb("hT", [128, 8, T], BF16)
        ring = sb("ring", [128, NSLOT, 2048], BF16)
        qk = sb("qk", [128, 2, T], BF16)
        vaug = sb("vaug", [128, 16, 2, 128], BF16)
        mix = sb("mix", [128, 2, T], BF16)
        actb = sb("actb", [128, 4096], BF16)
        pt = sb("pt", [128, 3, 1024], BF16)
        mnegt = sb("mnegt", [128, T], BF16)
        mneg = sb("mneg", [128, 2, 128], F32)
        gm = sb("gm", [128, 16], F32)
        top = sb("top", [128, 16], F32)
        sel = sb("sel", [128, 16], F32)
        kms = sb("kms", [128, 8], F32)
        kmz = sb("kmz", [128, 2, 8], BF16)
        rstd = sb("rstd", [128, 2, 512], F32)
        sg = sb("sg", [128, 2, 512], BF16)
        bcsb = sb("bcsb", [128, 512], F32)
        osq = sb("osq", [128, 512], BF16)
        rden = sb("rden", [128, 512], F32)
        sinkrow = sb("sinkrow", [128, 1024], F32)
        cb = sb("cb_sb", [128, CB_N], BF16)
        cf = sb("cf_sb", [128, CF_N], F32)
        par = sb("par_sb", [128, PR_N], F32)
        lamt = sb("lamt", [128, 16], F32)
        lamp = sb("lamp", [128, 2, 32], F32)
        ps = st.enter_context(nc.psum_tensor("ps", [128, 8, 512], F32))
        o1 = rstd[:, 0, :]
        o2 = rstd[:, 1, :]
        TO1, TO2 = ("rstd", 0), ("rstd", 1)
        identf = cf[:, CF_ID:CF_ID + 128]
        KZ = [(actb[:, 0:2048], [("act", 0), ("act", 1)]), (actb[:, 2048:4096], [("act", 2)]),
              (qk[:, 1, :], [("qk", 1, tc_) for tc_ in range(4)]), (mnegt[:, :], [("mnegt", c_) for c_ in range(4)])]

        ones_bf = cb[:, CB_ONES:CB_ONES + 128]
        ident = cb[:, CB_ID:CB_ID + 128]
        tri = cb[:, CB_TRI:CB_TRI + 128]
        maskc = cb[:, CB_MASKC:CB_MASKC + 256]

        def mm(out, lhsT, rhs, start, stop, reads, writes, tp=None):
            kw = {}
            if tp is not None and tp[0] == 96:
                kw["tile_position"] = tp
            return P.add("pe", lambda e: e.matmul(out, lhsT=lhsT, rhs=rhs, start=start, stop=stop, **kw),
                         reads, writes)

        def act(out, in_, func, reads, writes, bias=None, scale=None):
            kw = {}
            if bias is not None:
                kw["bias"] = bias
            if scale is not None:
                kw["scale"] = scale
            return P.add("act", lambda e: e.activation(out, in_, func, **kw), reads, writes)

        def tt(eng, out, in0, in1, op, reads, writes):
            return P.add(eng, lambda e: e.tensor_tensor(out, in0, in1, op), reads, writes)

        def stt(out, in0, scalar, in1, op0, op1, reads, writes):
            return P.add("dve", lambda e: e.scalar_tensor_tensor(out, in0, scalar, in1, op0, op1), reads, writes)

        def ts(eng, out, in0, s1, s2, op0, op1, reads, writes):
            if op1 is None:
                return P.add(eng, lambda e: e.tensor_scalar(out, in0, s1, None, op0), reads, writes)
            return P.add(eng, lambda e: e.tensor_scalar(out, in0, s1, s2, op0, op1), reads, writes)

        def recip(out, in_, reads, writes):
            return P.add("dve", lambda e: e.reciprocal(out, in_), reads, writes)

        def copy(eng, out, in_, reads, writes):
            if eng == "act":
                return P.add("act", lambda e: e.copy(out, in_), reads, writes)
            return P.add(eng, lambda e: e.tensor_copy(out, in_), reads, writes)

        def dma(eng, out, in_, key, reads, writes):
            return P.add(eng, lambda e: e.dma_start(out=out, in_=in_), reads, writes, dma=key)

        def xtok(kc, tc):
            return ("x", kc, tc)

        loads = []

        def colblk(w, l, c0, n):
            return w[l, :, c0:c0 + n].rearrange("(kc p) n -> p kc n", p=128)

        def rowblk(w, l, r0):
            return w[l, r0:r0 + 256, :].rearrange("(rc p) n -> p rc n", p=128)

        def ffn_loads(wg, wu, wd, l):
            for g in range(NG):
                loads.append(("c", colblk(wg, l, g * 256, 256), 256))
                loads.append(("c", colblk(wu, l, g * 256, 256), 256))
                loads.append(("r", rowblk(wd, l, g * 256), 0))

        for l in range(NL):
            ffn_loads(w1g, w1u, w1d, l)
            for p in range(8):
                loads.append(("c", colblk(win, l, p * 384, 256), 256))
                loads.append(("c", colblk(win, l, p * 384 + 256, 128), 128))
                if p % 2 == 1:
                    loads.append(("r", rowblk(wout, l, (p // 2) * 256), 0))
            ffn_loads(w2g, w2u, w2d, l)
        wstate = {"rec": 0, "next": 0}

        def slot_view(i):
            kind, src, n = loads[i]
            s = i % NSLOT
            flat = ring[:, s, :]
            if kind == "c":
                return flat.rearrange("p (kc n) -> p kc n", n=256)
            return flat.rearrange("p (rc n) -> p rc n", n=1024)

        def take(n):
            a = wstate["next"]
            wstate["next"] = a + n
            upto = min(len(loads), a + NSLOT)
            while wstate["rec"] < upto:
                i = wstate["rec"]
                kind, src, ncol = loads[i]
                v = slot_view(i)
                dst = v[:, :, 0:ncol] if kind == "c" else v
                dma("pool", dst, src, ("slot", i % NSLOT), [], [("slot", i % NSLOT)])
                wstate["rec"] += 1
            return [(slot_view(i), ("slot", i % NSLOT)) for i in range(a, a + n)]

        dma("sp", cb[:, :], cbd, "cst", [], ["c_cb"])
        dma("sp", cf[:, :], cfd, "cst", [], ["c_cf"])
        dma("sp", par[:, :], prd, "cst", [], ["c_par"])
        for kc in range(8):
            dma("sp", xT[:, kc, :], xin[kc * 128:(kc + 1) * 128, :], "xin", [], [xtok(kc, tc) for tc in range(4)])
        P.add("pool", lambda e: e.memset(vaug[:, :, :, :], 1.0), [], [("v", t) for t in range(16)])
        P.add("pool", lambda e: e.memset(mneg[:, :, :], 0.0), [], ["mneg"])
        P.add("pool", lambda e: e.memset(kmz[:, :, :], 0.0), [], ["kmz"])

        def rmsnorm(gcol, final=False):
            sq = actb[:, :].rearrange("p (k n) -> p k n", n=512)
            for tc in range(4):
                cs = slice(tc * 512, (tc + 1) * 512)
                bank = 5 + (tc % 2)
                rb = rstd[:, tc % 2, :]
                act(sq, xT[:, :, cs], AF.Square, [xtok(kc, tc) for kc in range(8)], [("act", 0), ("act", 1), ("act", 2)])
                for kc in range(8):
                    mm(ps[:, bank, :], ones_bf, sq[:, kc, :], kc == 0, kc == 7,
                       ["c_cb", ("act", 0), ("act", 1), ("act", 2)], [("ps", bank)])
                act(rb, ps[:, bank, :], AF.Sqrt, [("ps", bank)], [("rstd", tc % 2)], bias=cf[:, CF_EPS:CF_EPS + 1],
                    scale=1.0 / D)
                recip(rb, rb, [("rstd", tc % 2)], [("rstd", tc % 2)])
                for kc in range(8):
                    if final:
                        stt(xT[:, kc, cs], xT[:, kc, cs], par[:, gcol + kc:gcol + kc + 1], rb, ALU.mult, ALU.mult,
                            [xtok(kc, tc), ("rstd", tc % 2), "c_par"], [xtok(kc, tc)])
                    else:
                        stt(hT[:, kc, cs], xT[:, kc, cs], par[:, gcol + kc:gcol + kc + 1], rb, ALU.mult, ALU.mult,
                            [xtok(kc, tc), ("rstd", tc % 2), "c_par"], [("h", kc, tc)])


        def ffn():
            pend = None
            cnt = 0
            for g in range(NG):
                if pend is not None:
                    pend()
                    pend = None
                (wgv, tg), (wuv, tu), (wdv, td) = take(3)
                for tc in range(4):
                    cs = slice(tc * 512, (tc + 1) * 512)
                    ai = cnt % 2
                    av = actb[:, ai * 1024:(ai + 1) * 1024].rearrange("p (f n) -> p f n", n=512)
                    for fi in range(G):
                        bg, bu = 2 * (fi % 2), 2 * (fi % 2) + 1
                        for kc in range(8):
                            mm(ps[:, bg, :], wgv[:, kc, fi * 128:(fi + 1) * 128], hT[:, kc, cs], kc == 0, kc == 7,
                               [tg, ("h", kc, tc)], [("ps", bg)])
                        for kc in range(8):
                            mm(ps[:, bu, :], wuv[:, kc, fi * 128:(fi + 1) * 128], hT[:, kc, cs], kc == 0, kc == 7,
                               [tu, ("h", kc, tc)], [("ps", bu)])
                        act(sg[:, fi % 2, :], ps[:, bg, :], AF.Silu, [("ps", bg)], [("sg", fi % 2)])
                        tt("dve", av[:, fi, :], sg[:, fi % 2, :], ps[:, bu, :], ALU.mult,
                           [("sg", fi % 2), ("ps", bu)], [("act", ai)])
                    if pend is not None:
                        pend()

                    def down(av=av, ai=ai, wdv=wdv, td=td, tc=tc, cs=cs):
                        for dc in range(8):
                            b = 4 + (dc % 4)
                            for fi in range(G):
                                mm(ps[:, b, :], wdv[:, fi, dc * 128:(dc + 1) * 128], av[:, fi, :], fi == 0, fi == G - 1,
                                   [td, ("act", ai)], [("ps", b)])
                            stt(xT[:, dc, cs], ps[:, b, :], 0.5, xT[:, dc, cs], ALU.mult, ALU.add,
                                [("ps", b), xtok(dc, tc)], [xtok(dc, tc)])
                    pend = down
                    cnt += 1
            if pend is not None:
                pend()

        def proj_qk(wv, tw, kind):
            if kind == "B":
                for buf, toks in KZ:
                    P.add("pool", lambda e, buf=buf: e.memset(buf, 0.0), [], toks)
            else:
                P.add("pool", lambda e: e.memset(actb[64:128, 0:2048], 0.0), [], KZ[0][1])
                P.add("pool", lambda e: e.memset(actb[0:64, 2048:4096], 0.0), [], KZ[1][1])
            n = 0
            for j in range(2):
                for tc in range(4):
                    cs = slice(tc * 512, (tc + 1) * 512)
                    b = n % 4
                    n += 1
                    eng = "act" if n % 2 else "dve"
                    for kc in range(8):
                        mm(ps[:, b, :], wv[:, kc, j * 128:(j + 1) * 128], hT[:, kc, cs], kc == 0, kc == 7,
                           [tw, ("h", kc, tc)], [("ps", b)])
                    if j == 0:
                        copy(eng, qk[:, 0, cs], ps[:, b, :], [("ps", b)], [("qk", 0, tc)])
                    elif kind == "B":
                        copy(eng, KZ[0][0][0:32, cs], ps[0:32, b, :], [("ps", b)], KZ[0][1])
                        copy(eng, KZ[1][0][32:64, cs], ps[32:64, b, :], [("ps", b)], KZ[1][1])
                        copy(eng, KZ[2][0][64:96, cs], ps[64:96, b, :], [("ps", b)], KZ[2][1])
                        copy(eng, KZ[3][0][64:128, cs], ps[64:128, b, :], [("ps", b)], KZ[3][1])
                        P.add("pool", lambda e, cs=cs: e.memset(KZ[3][0][64:96, cs], 0.0), [], KZ[3][1])
                    else:
                        copy(eng, KZ[0][0][0:64, cs], ps[0:64, b, :], [("ps", b)], KZ[0][1])
                        copy(eng, KZ[1][0][64:128, cs], ps[64:128, b, :], [("ps", b)], KZ[1][1])
                        if kind == "A":
                            P.add("dve", lambda e, tc=tc, cs=cs: e.reduce_sum(
                                kms[0:64, 2 * tc:2 * tc + 2],
                                KZ[0][0][0:64, cs].rearrange("p (n l) -> p n l", l=256), AX.X),
                                KZ[0][1], ["kms"])
                            P.add("dve", lambda e, tc=tc, cs=cs: e.reduce_sum(
                                kms[64:128, 2 * tc:2 * tc + 2],
                                KZ[1][0][64:128, cs].rearrange("p (n l) -> p n l", l=256), AX.X),
                                KZ[1][1], ["kms"])
            if kind == "A":
                copy("dve", kmz[0:64, 0, :], kms[0:64, :], ["kms", "kmz"], ["kmz"])
                copy("dve", kmz[64:128, 1, :], kms[64:128, :], ["kms", "kmz"], ["kmz"])

        def proj_v(wv, tw, per_t=None):
            for t in range(16):
                b = 4 + (t % 2)
                cs = slice(t * 128, (t + 1) * 128)
                oc = slice(0, 128)
                for kc in range(8):
                    mm(ps[:, b, oc], hT[:, kc, cs], wv[:, kc, 0:128], kc == 0, kc == 7,
                       [tw, ("h", kc, t // 4)], [("ps", b)])
                copy("act" if t % 2 else "dve", vaug[:, t, :, 0:64],
                     ps[:, b, oc].rearrange("p (h d) -> p h d", d=64), [("ps", b)], [("v", t)])
                if per_t is not None:
                    per_t(t)

        ncount = [0]

        def normalize(ob, dst, dtok, h_sink=None):
            buf, tk = (rden, "rden") if ncount[0] % 2 == 0 else (bcsb, "bcsb")
            ncount[0] += 1
            if h_sink is not None:
                srow = bcast_mid(sinkrow[64:128, h_sink * 128:(h_sink + 1) * 128], 4)
                tt("dve", buf[0:64, :].rearrange("p (a b) -> p a b", b=128),
                   ps[64:128, ob, :].rearrange("p (a b) -> p a b", b=128), srow, ALU.add,
                   [("ps", ob), "sinkrow"], [tk])
                if h_sink == 0:
                    recip(buf[0:64, :], buf[0:64, :], [tk], [tk])
                else:
                    act(buf[0:64, :], buf[0:64, :], AF.Ln, [tk], [tk])
            else:
                act(buf[0:64, :], ps[64:128, ob, :], AF.Ln, [("ps", ob)], [tk])
            if h_sink != 0:
                act(buf[0:64, :], buf[0:64, :], AF.Exp, [tk], [tk], scale=-1.0)
            tt("dve", dst, ps[0:64, ob, :], buf[0:64, :], ALU.mult, [("ps", ob), tk], dtok)

        def attn_full(kind, pidx, li):
            nmap = 2 if kind == "B" else 1
            scale = 32 ** -0.5 if kind == "B" else 0.125
            pend2 = [None]
            for hl in range(2):
                h = 2 * pidx + hl
                tbcol = CF_TBAB + (h if kind == "A" else 4 + h) * 16
                for c in range(4):
                    nkt = 4 * c + 4
                    it = hl * 4 + c
                    if kind == "B":
                        obanks = [4, 5] if it % 2 == 0 else [6, 7]
                    else:
                        obanks = [4 + (it % 2)]
                    steps = []
                    for kt in range(nkt):
                        j = kt - 4 * c
                        col0 = max(j, 0) * 128
                        ncols = 512 - col0
                        qs = slice(c * 512 + col0, (c + 1) * 512)
                        ks = slice(kt * 128, (kt + 1) * 128)
                        pi = kt % 3
                        if kind == "A":
                            stb = [kt % 3]
                        else:
                            stb = [2 * (kt % 2), 2 * (kt % 2) + 1]

                        def st_fn(kt=kt, j=j, col0=col0, ncols=ncols, qs=qs, ks=ks, stb=stb):
                            if kind == "A":
                                kzb, kzt = KZ[hl]
                                mm(ps[:, stb[0], 0:ncols], kzb[:, ks], qk[:, 0, qs],
                                   True, False, kzt + [("qk", 0, c)], [("ps", stb[0])])
                                ec = CB_E + hl * 1024 + (kt // 2) * 128
                                mm(ps[:, stb[0], 0:ncols], cb[:, ec:ec + 128], mnegt[:, qs], False, True,
                                   ["c_cb", ("mnegt", c)], [("ps", stb[0])])
                            else:
                                for m in range(2):
                                    kzb, kzt = KZ[2 * hl + m]
                                    mm(ps[:, stb[m], 0:ncols], kzb[:, ks], qk[:, 0, qs],
                                       True, True, kzt + [("qk", 0, c)], [("ps", stb[m])])

                        def ex_fn(kt=kt, j=j, ncols=ncols, stb=stb, pi=pi):
                            bias = cf[:, tbcol + j + 12:tbcol + j + 13]
                            if kind == "A":
                                act(pt[:, pi, 0:ncols], ps[:, stb[0], 0:ncols], AF.Exp, [("ps", stb[0]), "c_cf"],
                                    [("pt", pi)], bias=bias, scale=scale)
                                if j >= 0:
                                    tt("pool", pt[:, pi, 0:128], pt[:, pi, 0:128], tri, ALU.mult,
                                       [("pt", pi), "c_cb"], [("pt", pi)])
                            else:
                                pv = pt[:, pi, :].rearrange("p (m n) -> p m n", n=512)
                                act(pv[:, :, 0:ncols], ps[:, stb[0]:stb[0] + 2, 0:ncols], AF.Exp,
                                    [("ps", stb[0]), ("ps", stb[1]), "c_cf"], [("pt", pi)], bias=bias, scale=scale)
                                if j >= 0:
                                    for m in range(2):
                                        tt("pool", pv[:, m, 0:128], pv[:, m, 0:128], tri, ALU.mult,
                                           [("pt", pi), "c_cb"], [("pt", pi)])

                        def pv_fn(kt=kt, col0=col0, ncols=ncols, pi=pi):
                            for m in range(nmap):
                                rhs = pt[:, pi, m * 512:m * 512 + ncols]
                                mm(ps[:, obanks[m], col0:512], vaug[:, kt, hl, :], rhs, kt == 0, kt == nkt - 1,
                                   [("v", kt), ("pt", pi)], [("ps", obanks[m])])
                        steps.append((st_fn, ex_fn, pv_fn))
                    LOOK = 2 if kind == "A" else 1
                    for i in range(nkt + LOOK):
                        if i < nkt:
                            steps[i][0]()
                            steps[i][1]()
                        if i - LOOK >= 0:
                            steps[i - LOOK][2]()
                    cs = slice(c * 512, (c + 1) * 512)
                    dst = mix[hl * 64:(hl + 1) * 64, pidx % 2, cs]
                    dtok = [("mix", pidx % 2, c, hl)]
                    if kind == "A":
                        normalize(obanks[0], dst, dtok)
                    else:
                        if pend2[0] is not None:
                            pend2[0]()
                        normalize(obanks[0], o1[0:64, :], [TO1])
                        normalize(obanks[1], o2[0:64, :], [TO2])
                        stt(o1[0:64, :], o2[0:64, :], lamt[0:64, 4:5], o1[0:64, :], ALU.mult, ALU.add,
                            [TO1, TO2, "lamt"], [TO1])
                        tt("dve", osq[0:64, :], o1[0:64, :], o1[0:64, :], ALU.mult, [TO1], ["osq"])

                        def phase2(dst=dst, dtok=dtok):
                            mm(ps[0:64, 3, :], cb[0:64, CB_ONES:CB_ONES + 64], osq[0:64, :], True, True,
                               ["c_cb", "osq"], [("ps", 3)])
                            act(o2[0:64, :], ps[0:64, 3, :], AF.Sqrt, [("ps", 3)], [TO2],
                                bias=cf[0:64, CF_EPS:CF_EPS + 1], scale=1.0 / 64)
                            recip(o2[0:64, :], o2[0:64, :], [TO2], [TO2])
                            stt(dst, o1[0:64, :], lamt[0:64, 5:6], o2[0:64, :], ALU.mult, ALU.mult,
                                [TO1, TO2, "lamt"], dtok)
                        pend2[0] = phase2
            if pend2[0] is not None:
                pend2[0]()

        def moba_gate(pidx):
            def step(qt):
                b = qt // 2
                qs = slice(qt * 128, (qt + 1) * 128)
                mm(ps[:, 3, 0:16], qk[:, 0, qs], kmz[:, :, :].rearrange("p h n -> p (h n)"),
                   True, True, [("qk", 0, qt // 4), "kmz"], [("ps", 3)])
                tt("dve", gm[:, :], ps[:, 3, 0:16], cf[:, CF_PAST + b * 16:CF_PAST + b * 16 + 16], ALU.add,
                   [("ps", 3), "c_cf"], ["gm"])
                for hl in range(2):
                    P.add("dve", lambda e, hl=hl: e.max(top[:, hl * 8:hl * 8 + 8], gm[:, hl * 8:hl * 8 + 8]),
                          ["gm"], [("top", hl)])
                for hl in range(2):
                    stt(sel[:, hl * 8:hl * 8 + 8], gm[:, hl * 8:hl * 8 + 8], top[:, hl * 8 + 2:hl * 8 + 3],
                        cf[:, CF_A30 + b * 16 + hl * 8:CF_A30 + b * 16 + hl * 8 + 8], ALU.is_ge, ALU.mult,
                        ["gm", ("top", hl), "c_cf"], [("sel", hl)])
                mi = qt % 2
                tt("dve", mneg[:, mi, :].rearrange("p (h n) -> p h n", n=64)[:, :, 0:8],
                   sel[:, :].rearrange("p (h n) -> p h n", n=8),
                   cf[:, CF_BC + b * 16:CF_BC + b * 16 + 16].rearrange("p (h n) -> p h n", n=8), ALU.add,
                   [("sel", 0), ("sel", 1), "c_cf", "mneg"], [("mneg", mi)])
                pc = (qt % 4) * 128
                P.add("pe", lambda e, mi=mi, pc=pc: e.transpose(ps[:, 7, pc:pc + 128], mneg[:, mi, :], identf),
                      [("mneg", mi), "c_cf"], [("ps", 7)])
                if qt % 4 == 3:
                    c = qt // 4
                    copy("act", mnegt[:, c * 512:(c + 1) * 512], ps[:, 7, :], [("ps", 7)], [("mnegt", c)])
            return step

        def swa(pidx, li):
            cp = pidx - 4
            for hl in range(2):
                h = 2 * cp + hl
                kzb, kzt = KZ[hl]
                for c in range(4):
                    ob = 4 + ((hl * 4 + c) % 2)
                    steps = []
                    for qi in range(4):
                        qt = 4 * c + qi
                        qs = slice(qt * 128, (qt + 1) * 128)
                        sbk = qt % 4
                        pi = qt % 3

                        def st_fn(qt=qt, qs=qs, sbk=sbk):
                            mm(ps[:, sbk, 128:256], kzb[:, qs], qk[:, 0, qs], True, True,
                               kzt + [("qk", 0, qt // 4)], [("ps", sbk)])
                            if qt > 0:
                                ks = slice((qt - 1) * 128, qt * 128)
                                mm(ps[:, sbk, 0:128], kzb[:, ks], qk[:, 0, qs], True, True,
                                   kzt + [("qk", 0, qt // 4)], [("ps", sbk)])

                        def ex_fn(qt=qt, sbk=sbk, pi=pi):
                            act(pt[:, pi, 128:256], ps[:, sbk, 128:256], AF.Exp, [("ps", sbk), "c_cf"], [("pt", pi)],
                                bias=cf[:, CF_TBC + h * 2 + 1:CF_TBC + h * 2 + 2], scale=0.125)
                            if qt > 0:
                                act(pt[:, pi, 0:128], ps[:, sbk, 0:128], AF.Exp, [("ps", sbk), "c_cf"], [("pt", pi)],
                                    bias=cf[:, CF_TBC + h * 2:CF_TBC + h * 2 + 1], scale=0.125)
                                tt("pool", pt[:, pi, 0:256], pt[:, pi, 0:256], maskc, ALU.mult,
                                   [("pt", pi), "c_cb"], [("pt", pi)])
                            else:
                                tt("pool", pt[:, pi, 128:256], pt[:, pi, 128:256], tri, ALU.mult,
                                   [("pt", pi), "c_cb"], [("pt", pi)])

                        def pv_fn(qt=qt, qi=qi, pi=pi):
                            oc = slice(qi * 128, (qi + 1) * 128)
                            if qt > 0:
                                mm(ps[:, ob, oc], vaug[:, qt - 1, hl, :], pt[:, pi, 0:128], True, False,
                                   [("v", qt - 1), ("pt", pi)], [("ps", ob)])
                                mm(ps[:, ob, oc], vaug[:, qt, hl, :], pt[:, pi, 128:256], False, True,
                                   [("v", qt), ("pt", pi)], [("ps", ob)])
                            else:
                                mm(ps[:, ob, oc], vaug[:, qt, hl, :], pt[:, pi, 128:256], True, True,
                                   [("v", qt), ("pt", pi)], [("ps", ob)])
                        steps.append((st_fn, ex_fn, pv_fn))
                    LOOK = 2
                    for i in range(4 + LOOK):
                        if i < 4:
                            steps[i][0]()
                            steps[i][1]()
                        if i - LOOK >= 0:
                            steps[i - LOOK][2]()
                    cs = slice(c * 512, (c + 1) * 512)
                    normalize(ob, mix[hl * 64:(hl + 1) * 64, pidx % 2, cs], [("mix", pidx % 2, c, hl)], h_sink=h)

        def w_out(wv, tw):
            n = 0
            for tc in range(4):
                cs = slice(tc * 512, (tc + 1) * 512)
                for dc in range(8):
                    b = n % 4
                    n += 1
                    for rc in range(2):
                        mm(ps[:, b, :], wv[:, rc, dc * 128:(dc + 1) * 128], mix[:, rc, cs], rc == 0, rc == 1,
                           [tw, ("mix", rc, tc, 0), ("mix", rc, tc, 1)], [("ps", b)])
                    tt("dve", xT[:, dc, cs], ps[:, b, :], xT[:, dc, cs], ALU.add, [("ps", b), xtok(dc, tc)],
                       [xtok(dc, tc)])

        def layer_params(li, ltrue):
            lam_init = 0.8 - 0.6 * math.exp(-0.3 * ltrue)
            base = PR_LAM + li * 128
            tt("dve", lamp[:, 0, :], par[:, base:base + 32], par[:, base + 32:base + 64], ALU.mult, ["c_par", "lamp"], ["lamp"])
            tt("dve", lamp[:, 1, :], par[:, base + 64:base + 96], par[:, base + 96:base + 128], ALU.mult, ["c_par", "lamp"], ["lamp"])
            P.add("dve", lambda e: e.reduce_sum(lamt[:, 0:2], lamp[:, :, :], AX.X), ["lamp", "lamt"], ["lamt"])
            act(lamt[:, 2:4], lamt[:, 0:2], AF.Exp, ["lamt"], ["lamt"])
            tt("dve", lamt[:, 4:5], lamt[:, 3:4], lamt[:, 2:3], ALU.subtract, ["lamt"], ["lamt"])
            ts("dve", lamt[:, 4:5], lamt[:, 4:5], -lam_init, None, ALU.add, None, ["lamt"], ["lamt"])
            ts("dve", lamt[:, 5:6], par[:, PR_SUB + li:PR_SUB + li + 1], 1.0 - lam_init, None, ALU.mult, None,
               ["c_par", "lamt"], ["lamt"])
            dma("sp", sinkrow[:, :], posd, "sink", ["sinkrow"], ["sinkrow"])
            for h in range(8):
                act(sinkrow[:, h * 128:(h + 1) * 128], sinkrow[:, h * 128:(h + 1) * 128], AF.Exp,
                    ["sinkrow", "c_par"], ["sinkrow"], bias=par[:, PR_SINK + li * 8 + h:PR_SINK + li * 8 + h + 1],
                    scale=1.0)

        for li, ltrue in enumerate(layer_ids):
            P.epoch = li
            if stop_after == "pro":
                break
            layer_params(li, ltrue)
            if stop_after == "params":
                break
            rmsnorm(PR_G + (li * 3 + 0) * 8)
            if stop_after == "norm":
                break
            ffn()
            if stop_after == "ffn1":
                break
            rmsnorm(PR_G + (li * 3 + 1) * 8)
            for pidx in range(8):
                kind = "A" if pidx < 2 else ("B" if pidx < 4 else "C")
                nl = 3 if pidx % 2 == 1 else 2
                got = take(nl)
                (wqk, tqk), (wvv, tvv) = got[0], got[1]
                proj_qk(wqk, tqk, kind)
                proj_v(wvv, tvv, moba_gate(pidx) if kind == "A" else None)
                if kind == "A":
                    attn_full("A", pidx, li)
                elif kind == "B":
                    attn_full("B", pidx - 2, li)
                else:
                    swa(pidx, li)
                if pidx % 2 == 1:
                    w_out(got[2][0], got[2][1])
                if stop_after == ("pair", pidx):
                    break
            if stop_after is not None:
                break
            rmsnorm(PR_G + (li * 3 + 2) * 8)
            ffn()
        if do_final and stop_after is None:
            rmsnorm(PR_G + 96, final=True)
        outs = []
        for kc in range(8):
            outs.append(dma("sp", outd[kc * 128:(kc + 1) * 128, :], xT[:, kc, :], "out",
                            [xtok(kc, tc) for tc in range(4)], [("outdone", kc)]))
        P.add("sp", lambda e: e.nop(), [("outdone", kc) for kc in range(8)] + [("slot", s_) for s_ in range(NSLOT)], [])
        stats = P.emit(nc, st)
    return nc, stats


def _consts():
    cbm = np.zeros((128, CB_N), np.float32)
    k = np.arange(128)[:, None]
    q = np.arange(128)[None, :]
    cbm[:, CB_MASKC:CB_MASKC + 128] = (q < k)
    cbm[:, CB_TRI:CB_TRI + 128] = (q >= k)
    cbm[:, CB_ID:CB_ID + 128] = np.eye(128)
    E = np.zeros((128, 2, 8, 128), np.float32)
    for hl in range(2):
        for n in range(8):
            E[hl * 64 + n, hl, n, :] = 1.0
    cbm[:, CB_E:CB_E + 2048] = E.reshape(128, 2048)
    cbm[:, CB_ONES:CB_ONES + 128] = 1.0
    cf = np.zeros((128, CF_N), np.float64)
    p = np.arange(128, dtype=np.float64)
    ab = np.concatenate([SL_A, SL_B])
    for s in range(8):
        for d in range(-12, 4):
            cf[:, CF_TBAB + s * 16 + d + 12] = ab[s] * (p + 128.0 * d)
    for h in range(8):
        cf[:, CF_TBC + h * 2] = SL_C[h] * (p - 192.0)
        cf[:, CF_TBC + h * 2 + 1] = SL_C[h] * (p - 64.0)
    for b in range(8):
        for hl in range(2):
            for n in range(8):
                cf[:, CF_PAST + b * 16 + hl * 8 + n] = 0.0 if n < b else -1e30
                cf[:, CF_A30 + b * 16 + hl * 8 + n] = 30000.0 if n < b else 0.0
                cf[:, CF_BC + b * 16 + hl * 8 + n] = 0.0 if n == b else -30000.0
    cf[:, CF_ONES:CF_ONES + 64] = 1.0
    cf[:, CF_EPS] = EPS
    cf[:, CF_ID:CF_ID + 128] = np.eye(128)
    pos = np.zeros((128, 1024), np.float64)
    for h in range(8):
        pos[:, h * 128:(h + 1) * 128] = (SL_C[h] * (np.arange(128) - 64.0))[None, :]
    return cbm.astype(ml_dtypes.bfloat16), cf.astype(np.float32), pos.astype(np.float32)


def _perm_win():
    cols = []
    for j in range(2):
        cols += list(range(128 * j, 128 * j + 128)) + list(range(256 + 128 * j, 256 + 128 * j + 128)) + \
            list(range(512 + 128 * j, 512 + 128 * j + 128))
    for j in range(2):
        cols += list(range(768 + 128 * j, 768 + 128 * j + 128)) + list(range(1024 + 128 * j, 1024 + 128 * j + 128)) + \
            list(range(1280 + 128 * j, 1280 + 128 * j + 128))
    for j in range(4):
        kv = j // 2
        kc = list(range(2048 + 64 * kv, 2048 + 64 * kv + 64))
        vc = list(range(2176 + 64 * kv, 2176 + 64 * kv + 64))
        cols += list(range(1536 + 128 * j, 1536 + 128 * j + 128)) + kc + kc + vc + vc
    return np.asarray(cols, np.int64)


def _params(inp, layer_ids):
    par = np.zeros((128, PR_N), np.float32)

    def gl(v):
        return np.ascontiguousarray(np.asarray(v, np.float32).reshape(8, 128).T)
    for li, l in enumerate(layer_ids):
        par[:, PR_G + (li * 3 + 0) * 8:PR_G + (li * 3 + 0) * 8 + 8] = gl(inp["norm_ffn1"][l])
        par[:, PR_G + (li * 3 + 1) * 8:PR_G + (li * 3 + 1) * 8 + 8] = gl(inp["norm_mix"][l])
        par[:, PR_G + (li * 3 + 2) * 8:PR_G + (li * 3 + 2) * 8 + 8] = gl(inp["norm_ffn2"][l])
        par[:, PR_SUB + li] = np.tile(np.asarray(inp["diff_subln"][l], np.float32), 2)
        for j, nm in enumerate(("lam_q1", "lam_k1", "lam_q2", "lam_k2")):
            par[:, PR_LAM + li * 128 + j * 32:PR_LAM + li * 128 + j * 32 + 32] = np.asarray(inp[nm][l], np.float32)[None, :]
        par[:, PR_SINK + li * 8:PR_SINK + li * 8 + 8] = np.asarray(inp["sinks"][l], np.float32)[None, :]
    par[:, PR_G + 96:PR_G + 104] = gl(inp["final_norm"])
    return par


_CACHE = {}


def _get_nc(layer_ids, do_final, stop_after=None):
    key = (tuple(layer_ids), do_final, stop_after)
    if key not in _CACHE:
        _CACHE[key] = build(list(layer_ids), do_final, stop_after)[0]
    return _CACHE[key]


def run_layers(xT_list, inp, layer_ids, do_final, stop_after=None, core_ids=None):
    cbm, cf, pos = _consts()
    perm = _perm_win()
    ls = list(layer_ids)
    f32 = lambda a: np.ascontiguousarray(np.asarray(a, np.float32))
    shared = {
        "w1g": f32(inp["w1_gate"][ls]), "w1u": f32(inp["w1_up"][ls]), "w1d": f32(inp["w1_down"][ls]),
        "w2g": f32(inp["w2_gate"][ls]), "w2u": f32(inp["w2_up"][ls]), "w2d": f32(inp["w2_down"][ls]),
        "win": f32(np.asarray(inp["w_in"], np.float32)[ls][:, :, perm]), "wout": f32(inp["w_out"][ls]),
        "cb": cbm, "cf": cf, "par": _params(inp, ls), "posrow": pos,
    }
    nc = _get_nc(ls, do_final, stop_after)
    n = len(xT_list)
    in_maps = [dict(shared, xT=np.ascontiguousarray(x)) for x in xT_list]
    res = run_bass_kernel_spmd(nc, in_maps, core_ids=list(range(n)) if core_ids is None else core_ids)
    return [r["outT"] for r in res.results]


def kernel(**inputs):
    inp = {k: np.asarray(v) for k, v in inputs.items()}
    x = np.asarray(inp["x"], np.float32)
    B = x.shape[0]
    xs = [np.ascontiguousarray(x[b].T) for b in range(B)]
    if FUSED:
        outs = run_layers(xs, inp, range(DEPTH), True)
    else:
        outs = xs
        for l in range(DEPTH):
            outs = run_layers(outs, inp, [l], l == DEPTH - 1)
    return np.stack([np.ascontiguousarray(o.T) for o in outs], axis=0).astype(np.float32)
```

```python
import math
from contextlib import ExitStack
import numpy as np
import ml_dtypes
import concourse.bass as bass
import concourse.mybir as mybir
from concourse.bass_utils import run_bass_kernel_spmd

F32 = mybir.dt.float32
BF16 = mybir.dt.bfloat16
AF = mybir.ActivationFunctionType
ALU = mybir.AluOpType
AX = mybir.AxisListType

D = 1024
T = 2048
DFF = 2816
NF = DFF // 128
G = 2
NG = NF // G
DEPTH = 4
EPS = 1e-6
NSLOT = 6
FUSED = True
SAME_ENGINE_SYNC = True

SLOPES = 2.0 ** (-8.0 * (np.arange(16, dtype=np.float64) + 1.0) / 16.0)
SL_C = SLOPES[0:8]
SL_B = SLOPES[8:12]
SL_A = SLOPES[12:16]

CB_MASKC = 0
CB_TRI = 128
CB_ID = 256
CB_E = 384
CB_ONES = 2432
CB_N = 2560
CF_TBAB = 0
CF_TBC = 128
CF_PAST = 144
CF_A30 = 272
CF_BC = 400
CF_ONES = 528
CF_EPS = 592
CF_ID = 600
CF_N = 728
PR_G = 0
PR_SUB = 104
PR_LAM = 108
PR_SINK = 620
PR_N = 652


class Op:
    __slots__ = ("eng", "fn", "deps", "needed", "sem", "val", "dma", "epoch")


class Prog:
    ENGS = ("pe", "act", "dve", "pool", "sp")

    def __init__(self):
        self.ops = {e: [] for e in self.ENGS}
        self.lastw = {}
        self.readers = {}
        self.epoch = 0

    def add(self, eng, fn, reads=(), writes=(), dma=None):
        op = Op()
        op.eng, op.fn, op.dma, op.epoch = eng, fn, dma, self.epoch
        op.needed = False
        op.sem = None
        op.val = 0
        deps = {}
        for t in reads:
            w = self.lastw.get(t)
            if w is not None:
                deps[id(w)] = w
        for t in writes:
            w = self.lastw.get(t)
            if w is not None:
                deps[id(w)] = w
            for r in self.readers.get(t, ()):
                deps[id(r)] = r
        out = []
        for d in deps.values():
            if d is op:
                continue
            if d.eng == eng and d.dma is None:
                if eng == "pe" or eng == "sp" or not SAME_ENGINE_SYNC:
                    continue
            out.append(d)
        op.deps = out
        for t in reads:
            if isinstance(t, str) and t.startswith("c_"):
                continue
            self.readers.setdefault(t, []).append(op)
        for t in writes:
            self.lastw[t] = op
            self.readers[t] = []
        self.ops[eng].append(op)
        return op

    def emit(self, nc, stack):
        sems = {}

        def getsem(key):
            if key not in sems:
                sems[key] = stack.enter_context(nc.semaphore("s%d" % len(sems)))
            return sems[key]

        for e in self.ENGS:
            for op in self.ops[e]:
                for d in op.deps:
                    d.needed = True
        cnt = {}
        for e in self.ENGS:
            for op in self.ops[e]:
                if op.dma is not None:
                    key = ("dma", op.dma)
                    cnt[key] = cnt.get(key, 0) + 16
                    op.sem, op.val = getsem(key), cnt[key]
                elif op.needed:
                    key = (e, op.epoch)
                    cnt[key] = cnt.get(key, 0) + 1
                    op.sem, op.val = getsem(key), cnt[key]
        block = stack.enter_context(nc.Block())
        stats = {}

        def run(eng_name, eng):
            waited = {}
            nw = 0
            for op in self.ops[eng_name]:
                need = {}
                for d in op.deps:
                    k = id(d.sem)
                    if waited.get(k, 0) >= d.val:
                        continue
                    if k not in need or need[k][1] < d.val:
                        need[k] = (d.sem, d.val)
                for k, (s, v) in need.items():
                    eng.wait_ge(s, v)
                    waited[k] = v
                    nw += 1
                inst = op.fn(eng)
                if op.dma is not None:
                    inst.then_inc(op.sem, 16)
                elif op.needed:
                    inst.then_inc(op.sem, 1)
            stats[eng_name] = (len(self.ops[eng_name]), nw)

        @block.tensor
        def _(e):
            run("pe", e)

        @block.scalar
        def _(e):
            run("act", e)

        @block.vector
        def _(e):
            run("dve", e)

        @block.gpsimd
        def _(e):
            run("pool", e)

        @block.sync
        def _(e):
            run("sp", e)

        return stats


def bcast_mid(ap, n):
    l = [list(x) for x in ap.ap]
    return bass.AP(ap.tensor, ap.offset, [l[0], [0, n]] + l[1:])


def build(layer_ids, do_final, stop_after=None):
    NL = len(layer_ids)
    nc = bass.Bass("TRN2", target_bir_lowering=False)
    dr = {}

    def din(name, shape, dt=F32):
        dr[name] = nc.dram_tensor(name, list(shape), dt, kind="ExternalInput").ap()
        return dr[name]

    xin = din("xT", [D, T])
    w1g = din("w1g", [NL, D, DFF]); w1u = din("w1u", [NL, D, DFF]); w1d = din("w1d", [NL, DFF, D])
    w2g = din("w2g", [NL, D, DFF]); w2u = din("w2u", [NL, D, DFF]); w2d = din("w2d", [NL, DFF, D])
    win = din("win", [NL, D, 8 * 384]); wout = din("wout", [NL, D, D])
    cbd = din("cb", [128, CB_N], BF16); cfd = din("cf", [128, CF_N]); prd = din("par", [128, PR_N])
    posd = din("posrow", [128, 1024])
    outd = nc.dram_tensor("outT", [D, T], F32, kind="ExternalOutput").ap()

    P = Prog()
    st = ExitStack()
    with st:
        def sb(name, shape, dt):
            return st.enter_context(nc.sbuf_tensor(name, list(shape), dt))

        xT = sb("xT_sb", [128, 8, T], F32)
        hT = sb("hT", [128, 8, T], BF16)
        ring = sb("ring", [128, NSLOT, 2048], BF16)
        qk = sb("qk", [128, 2, T], BF16)
        vaug = sb("vaug", [128, 16, 2, 128], BF16)
        mix = sb("mix", [128, 2, T], BF16)
        actb = sb("actb", [128, 4096], BF16)
        pt = sb("pt", [128, 3, 1024], BF16)
        mnegt = sb("mnegt", [128, T], BF16)
        mneg = sb("mneg", [128, 2, 128], F32)
        gm = sb("gm", [128, 16], F32)
        top = sb("top", [128, 16], F32)
        sel = sb("sel", [128, 16], F32)
        kms = sb("kms", [128, 8], F32)
        kmz = sb("kmz", [128, 2, 8], BF16)
        rstd = sb("rstd", [128, 2, 512], F32)
        sg = sb("sg", [128, 2, 512], BF16)
        bcsb = sb("bcsb", [128, 512], F32)
        osq = sb("osq", [128, 512], BF16)
        rden = sb("rden", [128, 512], F32)
        sinkrow = sb("sinkrow", [128, 1024], F32)
        cb = sb("cb_sb", [128, CB_N], BF16)
        cf = sb("cf_sb", [128, CF_N], F32)
        par = sb("par_sb", [128, PR_N], F32)
        lamt = sb("lamt", [128, 16], F32)
        lamp = sb("lamp", [128, 2, 32], F32)
        ps = st.enter_context(nc.psum_tensor("ps", [128, 8, 512], F32))
        o1 = rstd[:, 0, :]
        o2 = rstd[:, 1, :]
        TO1, TO2 = ("rstd", 0), ("rstd", 1)
        identf = cf[:, CF_ID:CF_ID + 128]
        KZ = [(actb[:, 0:2048], [("act", 0), ("act", 1)]), (actb[:, 2048:4096], [("act", 2)]),
              (qk[:, 1, :], [("qk", 1, tc_) for tc_ in range(4)]), (mnegt[:, :], [("mnegt", c_) for c_ in range(4)])]

        ones_bf = cb[:, CB_ONES:CB_ONES + 128]
        ident = cb[:, CB_ID:CB_ID + 128]
        tri = cb[:, CB_TRI:CB_TRI + 128]
        maskc = cb[:, CB_MASKC:CB_MASKC + 256]

        def mm(out, lhsT, rhs, start, stop, reads, writes, tp=None):
            kw = {}
            if tp is not None and tp[0] == 96:
                kw["tile_position"] = tp
            return P.add("pe", lambda e: e.matmul(out, lhsT=lhsT, rhs=rhs, start=start, stop=stop, **kw),
                         reads, writes)

        def act(out, in_, func, reads, writes, bias=None, scale=None):
            kw = {}
            if bias is not None:
                kw["bias"] = bias
            if scale is not None:
                kw["scale"] = scale
            return P.add("act", lambda e: e.activation(out, in_, func, **kw), reads, writes)

        def tt(eng, out, in0, in1, op, reads, writes):
            return P.add(eng, lambda e: e.tensor_tensor(out, in0, in1, op), reads, writes)

        def stt(out, in0, scalar, in1, op0, op1, reads, writes):
            return P.add("dve", lambda e: e.scalar_tensor_tensor(out, in0, scalar, in1, op0, op1), reads, writes)

        def ts(eng, out, in0, s1, s2, op0, op1, reads, writes):
            if op1 is None:
                return P.add(eng, lambda e: e.tensor_scalar(out, in0, s1, None, op0), reads, writes)
            return P.add(eng, lambda e: e.tensor_scalar(out, in0, s1, s2, op0, op1), reads, writes)

        def recip(out, in_, reads, writes):
            return P.add("dve", lambda e: e.reciprocal(out, in_), reads, writes)

        def copy(eng, out, in_, reads, writes):
            if eng == "act":
                return P.add("act", lambda e: e.copy(out, in_), reads, writes)
            return P.add(eng, lambda e: e.tensor_copy(out, in_), reads, writes)

        def dma(eng, out, in_, key, reads, writes):
            return P.add(eng, lambda e: e.dma_start(out=out, in_=in_), reads, writes, dma=key)

        def xtok(kc, tc):
            return ("x", kc, tc)

        loads = []

        def colblk(w, l, c0, n):
            return w[l, :, c0:c0 + n].rearrange("(kc p) n -> p kc n", p=128)

        def rowblk(w, l, r0):
            return w[l, r0:r0 + 256, :].rearrange("(rc p) n -> p rc n", p=128)

        def ffn_loads(wg, wu, wd, l):
            for g in range(NG):
                loads.append(("c", colblk(wg, l, g * 256, 256), 256))
                loads.append(("c", colblk(wu, l, g * 256, 256), 256))
                loads.append(("r", rowblk(wd, l, g * 256), 0))

        for l in range(NL):
            ffn_loads(w1g, w1u, w1d, l)
            for p in range(8):
                loads.append(("c", colblk(win, l, p * 384, 256), 256))
                loads.append(("c", colblk(win, l, p * 384 + 256, 128), 128))
                if p % 2 == 1:
                    loads.append(("r", rowblk(wout, l, (p // 2) * 256), 0))
            ffn_loads(w2g, w2u, w2d, l)
        wstate = {"rec": 0, "next": 0}

        def slot_view(i):
            kind, src, n = loads[i]
            s = i % NSLOT
            flat = ring[:, s, :]
            if kind == "c":
                return flat.rearrange("p (kc n) -> p kc n", n=256)
            return flat.rearrange("p (rc n) -> p rc n", n=1024)

        def take(n):
            a = wstate["next"]
            wstate["next"] = a + n
            upto = min(len(loads), a + NSLOT)
            while wstate["rec"] < upto:
                i = wstate["rec"]
                kind, src, ncol = loads[i]
                v = slot_view(i)
                dst = v[:, :, 0:ncol] if kind == "c" else v
                dma("pool", dst, src, ("slot", i % NSLOT), [], [("slot", i % NSLOT)])
                wstate["rec"] += 1
            return [(slot_view(i), ("slot", i % NSLOT)) for i in range(a, a + n)]

        dma("sp", cb[:, :], cbd, "cst", [], ["c_cb"])
        dma("sp", cf[:, :], cfd, "cst", [], ["c_cf"])
        dma("sp", par[:, :], prd, "cst", [], ["c_par"])
        for kc in range(8):
            dma("sp", xT[:, kc, :], xin[kc * 128:(kc + 1) * 128, :], "xin", [], [xtok(kc, tc) for tc in range(4)])
        P.add("pool", lambda e: e.memset(vaug[:, :, :, :], 1.0), [], [("v", t) for t in range(16)])
        P.add("pool", lambda e: e.memset(mneg[:, :, :], 0.0), [], ["mneg"])
        P.add("pool", lambda e: e.memset(kmz[:, :, :], 0.0), [], ["kmz"])

        def rmsnorm(gcol, final=False):
            sq = actb[:, :].rearrange("p (k n) -> p k n", n=512)
            for tc in range(4):
                cs = slice(tc * 512, (tc + 1) * 512)
                bank = 5 + (tc % 2)
                rb = rstd[:, tc % 2, :]
                act(sq, xT[:, :, cs], AF.Square, [xtok(kc, tc) for kc in range(8)], [("act", 0), ("act", 1), ("act", 2)])
                for kc in range(8):
                    mm(ps[:, bank, :], ones_bf, sq[:, kc, :], kc == 0, kc == 7,
                       ["c_cb", ("act", 0), ("act", 1), ("act", 2)], [("ps", bank)])
                act(rb, ps[:, bank, :], AF.Ln, [("ps", bank)], [("rstd", tc % 2)], bias=cf[:, CF_EPS:CF_EPS + 1],
                    scale=1.0 / D)
                act(rb, rb, AF.Exp, [("rstd", tc % 2)], [("rstd", tc % 2)], scale=-0.5)
                for kc in range(8):
                    if final:
                        stt(xT[:, kc, cs], xT[:, kc, cs], par[:, gcol + kc:gcol + kc + 1], rb, ALU.mult, ALU.mult,
                            [xtok(kc, tc), ("rstd", tc % 2), "c_par"], [xtok(kc, tc)])
                    else:
                        stt(hT[:, kc, cs], xT[:, kc, cs], par[:, gcol + kc:gcol + kc + 1], rb, ALU.mult, ALU.mult,
                            [xtok(kc, tc), ("rstd", tc % 2), "c_par"], [("h", kc, tc)])


        def ffn():
            pend = None
            cnt = 0
            for g in range(NG):
                if pend is not None:
                    pend()
                    pend = None
                (wgv, tg), (wuv, tu), (wdv, td) = take(3)
                for tc in range(4):
                    cs = slice(tc * 512, (tc + 1) * 512)
                    ai = cnt % 2
                    av = actb[:, ai * 1024:(ai + 1) * 1024].rearrange("p (f n) -> p f n", n=512)
                    for fi in range(G):
                        bg, bu = 2 * (fi % 2), 2 * (fi % 2) + 1
                        for kc in range(8):
                            mm(ps[:, bg, :], wgv[:, kc, fi * 128:(fi + 1) * 128], hT[:, kc, cs], kc == 0, kc == 7,
                               [tg, ("h", kc, tc)], [("ps", bg)])
                        for kc in range(8):
                            mm(ps[:, bu, :], wuv[:, kc, fi * 128:(fi + 1) * 128], hT[:, kc, cs], kc == 0, kc == 7,
                               [tu, ("h", kc, tc)], [("ps", bu)])
                        act(sg[:, fi % 2, :], ps[:, bg, :], AF.Silu, [("ps", bg)], [("sg", fi % 2)])
                        tt("dve", av[:, fi, :], sg[:, fi % 2, :], ps[:, bu, :], ALU.mult,
                           [("sg", fi % 2), ("ps", bu)], [("act", ai)])
                    if pend is not None:
                        pend()

                    def down(av=av, ai=ai, wdv=wdv, td=td, tc=tc, cs=cs):
                        for dc in range(8):
                            b = 4 + (dc % 4)
                            for fi in range(G):
                                mm(ps[:, b, :], wdv[:, fi, dc * 128:(dc + 1) * 128], av[:, fi, :], fi == 0, fi == G - 1,
                                   [td, ("act", ai)], [("ps", b)])
                            stt(xT[:, dc, cs], ps[:, b, :], 0.5, xT[:, dc, cs], ALU.mult, ALU.add,
                                [("ps", b), xtok(dc, tc)], [xtok(dc, tc)])
                    pend = down
                    cnt += 1
            if pend is not None:
                pend()

        def proj_qk(wv, tw, kind):
            if kind == "B":
                for buf, toks in KZ:
                    P.add("pool", lambda e, buf=buf: e.memset(buf, 0.0), [], toks)
            else:
                P.add("pool", lambda e: e.memset(actb[64:128, 0:2048], 0.0), [], KZ[0][1])
                P.add("pool", lambda e: e.memset(actb[0:64, 2048:4096], 0.0), [], KZ[1][1])
            n = 0
            for j in range(2):
                for tc in range(4):
                    cs = slice(tc * 512, (tc + 1) * 512)
                    b = n % 4
                    n += 1
                    eng = "act" if n % 2 else "dve"
                    for kc in range(8):
                        mm(ps[:, b, :], wv[:, kc, j * 128:(j + 1) * 128], hT[:, kc, cs], kc == 0, kc == 7,
                           [tw, ("h", kc, tc)], [("ps", b)])
                    if j == 0:
                        copy(eng, qk[:, 0, cs], ps[:, b, :], [("ps", b)], [("qk", 0, tc)])
                    elif kind == "B":
                        copy(eng, KZ[0][0][0:32, cs], ps[0:32, b, :], [("ps", b)], KZ[0][1])
                        copy(eng, KZ[1][0][32:64, cs], ps[32:64, b, :], [("ps", b)], KZ[1][1])
                        copy(eng, KZ[2][0][64:96, cs], ps[64:96, b, :], [("ps", b)], KZ[2][1])
                        copy(eng, KZ[3][0][64:128, cs], ps[64:128, b, :], [("ps", b)], KZ[3][1])
                        P.add("pool", lambda e, cs=cs: e.memset(KZ[3][0][64:96, cs], 0.0), [], KZ[3][1])
                    else:
                        copy(eng, KZ[0][0][0:64, cs], ps[0:64, b, :], [("ps", b)], KZ[0][1])
                        copy(eng, KZ[1][0][64:128, cs], ps[64:128, b, :], [("ps", b)], KZ[1][1])
                        if kind == "A":
                            P.add("dve", lambda e, tc=tc, cs=cs: e.reduce_sum(
                                kms[0:64, 2 * tc:2 * tc + 2],
                                KZ[0][0][0:64, cs].rearrange("p (n l) -> p n l", l=256), AX.X),
                                KZ[0][1], ["kms"])
                            P.add("dve", lambda e, tc=tc, cs=cs: e.reduce_sum(
                                kms[64:128, 2 * tc:2 * tc + 2],
                                KZ[1][0][64:128, cs].rearrange("p (n l) -> p n l", l=256), AX.X),
                                KZ[1][1], ["kms"])
            if kind == "A":
                copy("dve", kmz[0:64, 0, :], kms[0:64, :], ["kms", "kmz"], ["kmz"])
                copy("dve", kmz[64:128, 1, :], kms[64:128, :], ["kms", "kmz"], ["kmz"])

        def proj_v(wv, tw, per_t=None):
            for t in range(16):
                b = 4 + (t % 2)
                cs = slice(t * 128, (t + 1) * 128)
                oc = slice(0, 128)
                for kc in range(8):
                    mm(ps[:, b, oc], hT[:, kc, cs], wv[:, kc, 0:128], kc == 0, kc == 7,
                       [tw, ("h", kc, t // 4)], [("ps", b)])
                copy("act" if t % 2 else "dve", vaug[:, t, :, 0:64],
                     ps[:, b, oc].rearrange("p (h d) -> p h d", d=64), [("ps", b)], [("v", t)])
                if per_t is not None:
                    per_t(t)

        ncount = [0]

        def normalize(ob, dst, dtok, h_sink=None):
            buf, tk = (rden, "rden") if ncount[0] % 2 == 0 else (bcsb, "bcsb")
            ncount[0] += 1
            if h_sink is not None:
                srow = bcast_mid(sinkrow[64:128, h_sink * 128:(h_sink + 1) * 128], 4)
                tt("dve", buf[0:64, :].rearrange("p (a b) -> p a b", b=128),
                   ps[64:128, ob, :].rearrange("p (a b) -> p a b", b=128), srow, ALU.add,
                   [("ps", ob), "sinkrow"], [tk])
                if h_sink == 0:
                    recip(buf[0:64, :], buf[0:64, :], [tk], [tk])
                else:
                    act(buf[0:64, :], buf[0:64, :], AF.Ln, [tk], [tk])
            else:
                act(buf[0:64, :], ps[64:128, ob, :], AF.Ln, [("ps", ob)], [tk])
            if h_sink != 0:
                act(buf[0:64, :], buf[0:64, :], AF.Exp, [tk], [tk], scale=-1.0)
            tt("dve", dst, ps[0:64, ob, :], buf[0:64, :], ALU.mult, [("ps", ob), tk], dtok)

        def attn_full(kind, pidx, li):
            nmap = 2 if kind == "B" else 1
            scale = 32 ** -0.5 if kind == "B" else 0.125
            pend = {"p1": None, "p2": None}

            def after_loop(p1):
                if pend["p2"] is not None:
                    pend["p2"]()
                    pend["p2"] = None
                if pend["p1"] is not None:
                    pend["p2"] = pend["p1"]()
                pend["p1"] = p1

            for hl in range(2):
                h = 2 * pidx + hl
                tbcol = CF_TBAB + (h if kind == "A" else 4 + h) * 16
                for c in range(4):
                    nkt = 4 * c + 4
                    it = hl * 4 + c
                    if kind == "B":
                        obanks = [4, 5] if it % 2 == 0 else [6, 7]
                    else:
                        obanks = [4 + (it % 2)]
                    steps = []
                    for kt in range(nkt):
                        j = kt - 4 * c
                        col0 = max(j, 0) * 128
                        ncols = 512 - col0
                        qs = slice(c * 512 + col0, (c + 1) * 512)
                        ks = slice(kt * 128, (kt + 1) * 128)
                        pi = kt % 3
                        if kind == "A":
                            stb = [kt % 3]
                        else:
                            stb = [2 * (kt % 2), 2 * (kt % 2) + 1]

                        def st_fn(kt=kt, j=j, col0=col0, ncols=ncols, qs=qs, ks=ks, stb=stb):
                            if kind == "A":
                                kzb, kzt = KZ[hl]
                                mm(ps[:, stb[0], 0:ncols], kzb[:, ks], qk[:, 0, qs],
                                   True, False, kzt + [("qk", 0, c)], [("ps", stb[0])])
                                ec = CB_E + hl * 1024 + (kt // 2) * 128
                                mm(ps[:, stb[0], 0:ncols], cb[:, ec:ec + 128], mnegt[:, qs], False, True,
                                   ["c_cb", ("mnegt", c)], [("ps", stb[0])])
                            else:
                                for m in range(2):
                                    kzb, kzt = KZ[2 * hl + m]
                                    mm(ps[:, stb[m], 0:ncols], kzb[:, ks], qk[:, 0, qs],
                                       True, True, kzt + [("qk", 0, c)], [("ps", stb[m])])

                        def ex_fn(kt=kt, j=j, ncols=ncols, stb=stb, pi=pi):
                            bias = cf[:, tbcol + j + 12:tbcol + j + 13]
                            if kind == "A":
                                act(pt[:, pi, 0:ncols], ps[:, stb[0], 0:ncols], AF.Exp, [("ps", stb[0]), "c_cf"],
                                    [("pt", pi)], bias=bias, scale=scale)
                                if j >= 0:
                                    tt("pool", pt[:, pi, 0:128], pt[:, pi, 0:128], tri, ALU.mult,
                                       [("pt", pi), "c_cb"], [("pt", pi)])
                            else:
                                pv = pt[:, pi, :].rearrange("p (m n) -> p m n", n=512)
                                act(pv[:, :, 0:ncols], ps[:, stb[0]:stb[0] + 2, 0:ncols], AF.Exp,
                                    [("ps", stb[0]), ("ps", stb[1]), "c_cf"], [("pt", pi)], bias=bias, scale=scale)
                                if j >= 0:
                                    for m in range(2):
                                        tt("pool", pv[:, m, 0:128], pv[:, m, 0:128], tri, ALU.mult,
                                           [("pt", pi), "c_cb"], [("pt", pi)])

                        def pv_fn(kt=kt, col0=col0, ncols=ncols, pi=pi):
                            for m in range(nmap):
                                rhs = pt[:, pi, m * 512:m * 512 + ncols]
                                mm(ps[:, obanks[m], col0:512], vaug[:, kt, hl, :], rhs, kt == 0, kt == nkt - 1,
                                   [("v", kt), ("pt", pi)], [("ps", obanks[m])])
                        steps.append((st_fn, ex_fn, pv_fn))
                    LOOK = 2 if kind == "A" else 1
                    for i in range(nkt + LOOK):
                        if i < nkt:
                            steps[i][0]()
                            steps[i][1]()
                        if i - LOOK >= 0:
                            steps[i - LOOK][2]()
                    cs = slice(c * 512, (c + 1) * 512)
                    dst = mix[hl * 64:(hl + 1) * 64, pidx % 2, cs]
                    dtok = [("mix", pidx % 2, c, hl)]
                    if kind == "A":
                        def post1(ob=obanks[0], dst=dst, dtok=dtok):
                            normalize(ob, dst, dtok)
                            return None
                        after_loop(post1)
                    else:
                      def post1(obanks=obanks, dst=dst, dtok=dtok):
                        normalize(obanks[0], o1[0:64, :], [TO1])
                        normalize(obanks[1], o2[0:64, :], [TO2])
                        stt(o1[0:64, :], o2[0:64, :], lamt[0:64, 4:5], o1[0:64, :], ALU.mult, ALU.add,
                            [TO1, TO2, "lamt"], [TO1])
                        tt("dve", osq[0:64, :], o1[0:64, :], o1[0:64, :], ALU.mult, [TO1], ["osq"])

                        def phase2(dst=dst, dtok=dtok):
                            mm(ps[0:64, 3, :], cb[0:64, CB_ONES:CB_ONES + 64], osq[0:64, :], True, True,
                               ["c_cb", "osq"], [("ps", 3)])
                            act(o2[0:64, :], ps[0:64, 3, :], AF.Ln, [("ps", 3)], [TO2],
                                bias=cf[0:64, CF_EPS:CF_EPS + 1], scale=1.0 / 64)
                            act(o2[0:64, :], o2[0:64, :], AF.Exp, [TO2], [TO2], scale=-0.5)
                            stt(dst, o1[0:64, :], lamt[0:64, 5:6], o2[0:64, :], ALU.mult, ALU.mult,
                                [TO1, TO2, "lamt"], dtok)
                        return phase2
                      after_loop(post1)
            after_loop(None)
            after_loop(None)

        def moba_gate(pidx):
            def step(qt):
                b = qt // 2
                qs = slice(qt * 128, (qt + 1) * 128)
                mm(ps[:, 3, 0:16], qk[:, 0, qs], kmz[:, :, :].rearrange("p h n -> p (h n)"),
                   True, True, [("qk", 0, qt // 4), "kmz"], [("ps", 3)])
                tt("dve", gm[:, :], ps[:, 3, 0:16], cf[:, CF_PAST + b * 16:CF_PAST + b * 16 + 16], ALU.add,
                   [("ps", 3), "c_cf"], ["gm"])
                for hl in range(2):
                    P.add("dve", lambda e, hl=hl: e.max(top[:, hl * 8:hl * 8 + 8], gm[:, hl * 8:hl * 8 + 8]),
                          ["gm"], [("top", hl)])
                for hl in range(2):
                    stt(sel[:, hl * 8:hl * 8 + 8], gm[:, hl * 8:hl * 8 + 8], top[:, hl * 8 + 2:hl * 8 + 3],
                        cf[:, CF_A30 + b * 16 + hl * 8:CF_A30 + b * 16 + hl * 8 + 8], ALU.is_ge, ALU.mult,
                        ["gm", ("top", hl), "c_cf"], [("sel", hl)])
                mi = qt % 2
                tt("dve", mneg[:, mi, :].rearrange("p (h n) -> p h n", n=64)[:, :, 0:8],
                   sel[:, :].rearrange("p (h n) -> p h n", n=8),
                   cf[:, CF_BC + b * 16:CF_BC + b * 16 + 16].rearrange("p (h n) -> p h n", n=8), ALU.add,
                   [("sel", 0), ("sel", 1), "c_cf", "mneg"], [("mneg", mi)])
                if qt > 0:
                    xpose(qt - 1)
                if qt == 15:
                    xpose(15)

            def xpose(qt):
                mi = qt % 2
                pc = (qt % 4) * 128
                P.add("pe", lambda e, mi=mi, pc=pc: e.transpose(ps[:, 7, pc:pc + 128], mneg[:, mi, :], identf),
                      [("mneg", mi), "c_cf"], [("ps", 7)])
                if qt % 4 == 3:
                    c = qt // 4
                    copy("act", mnegt[:, c * 512:(c + 1) * 512], ps[:, 7, :], [("ps", 7)], [("mnegt", c)])
            return step

        def swa(pidx, li):
            cp = pidx - 4
            pendn = [None]
            for hl in range(2):
                h = 2 * cp + hl
                kzb, kzt = KZ[hl]
                for c in range(4):
                    ob = 4 + ((hl * 4 + c) % 2)
                    steps = []
                    for qi in range(4):
                        qt = 4 * c + qi
                        qs = slice(qt * 128, (qt + 1) * 128)
                        sbk = qt % 4
                        pi = qt % 3

                        def st_fn(qt=qt, qs=qs, sbk=sbk):
                            mm(ps[:, sbk, 128:256], kzb[:, qs], qk[:, 0, qs], True, True,
                               kzt + [("qk", 0, qt // 4)], [("ps", sbk)])
                            if qt > 0:
                                ks = slice((qt - 1) * 128, qt * 128)
                                mm(ps[:, sbk, 0:128], kzb[:, ks], qk[:, 0, qs], True, True,
                                   kzt + [("qk", 0, qt // 4)], [("ps", sbk)])

                        def ex_fn(qt=qt, sbk=sbk, pi=pi):
                            act(pt[:, pi, 128:256], ps[:, sbk, 128:256], AF.Exp, [("ps", sbk), "c_cf"], [("pt", pi)],
                                bias=cf[:, CF_TBC + h * 2 + 1:CF_TBC + h * 2 + 2], scale=0.125)
                            if qt > 0:
                                act(pt[:, pi, 0:128], ps[:, sbk, 0:128], AF.Exp, [("ps", sbk), "c_cf"], [("pt", pi)],
                                    bias=cf[:, CF_TBC + h * 2:CF_TBC + h * 2 + 1], scale=0.125)
                                tt("pool", pt[:, pi, 0:256], pt[:, pi, 0:256], maskc, ALU.mult,
                                   [("pt", pi), "c_cb"], [("pt", pi)])
                            else:
                                tt("pool", pt[:, pi, 128:256], pt[:, pi, 128:256], tri, ALU.mult,
                                   [("pt", pi), "c_cb"], [("pt", pi)])

                        def pv_fn(qt=qt, qi=qi, pi=pi):
                            oc = slice(qi * 128, (qi + 1) * 128)
                            if qt > 0:
                                mm(ps[:, ob, oc], vaug[:, qt - 1, hl, :], pt[:, pi, 0:128], True, False,
                                   [("v", qt - 1), ("pt", pi)], [("ps", ob)])
                                mm(ps[:, ob, oc], vaug[:, qt, hl, :], pt[:, pi, 128:256], False, True,
                                   [("v", qt), ("pt", pi)], [("ps", ob)])
                            else:
                                mm(ps[:, ob, oc], vaug[:, qt, hl, :], pt[:, pi, 128:256], True, True,
                                   [("v", qt), ("pt", pi)], [("ps", ob)])
                        steps.append((st_fn, ex_fn, pv_fn))
                    LOOK = 2
                    for i in range(4 + LOOK):
                        if i < 4:
                            steps[i][0]()
                            steps[i][1]()
                        if i - LOOK >= 0:
                            steps[i - LOOK][2]()
                    cs = slice(c * 512, (c + 1) * 512)
                    if pendn[0] is not None:
                        pendn[0]()

                    def postn(ob=ob, hl=hl, cs=cs, c=c, h=h):
                        normalize(ob, mix[hl * 64:(hl + 1) * 64, pidx % 2, cs], [("mix", pidx % 2, c, hl)], h_sink=h)
                    pendn[0] = postn
            if pendn[0] is not None:
                pendn[0]()

        def w_out(wv, tw):
            n = 0
            for tc in range(4):
                cs = slice(tc * 512, (tc + 1) * 512)
                for dc in range(8):
                    b = n % 4
                    n += 1
                    for rc in range(2):
                        mm(ps[:, b, :], wv[:, rc, dc * 128:(dc + 1) * 128], mix[:, rc, cs], rc == 0, rc == 1,
                           [tw, ("mix", rc, tc, 0), ("mix", rc, tc, 1)], [("ps", b)])
                    tt("dve", xT[:, dc, cs], ps[:, b, :], xT[:, dc, cs], ALU.add, [("ps", b), xtok(dc, tc)],
                       [xtok(dc, tc)])

        def layer_params(li, ltrue):
            lam_init = 0.8 - 0.6 * math.exp(-0.3 * ltrue)
            base = PR_LAM + li * 128
            tt("dve", lamp[:, 0, :], par[:, base:base + 32], par[:, base + 32:base + 64], ALU.mult, ["c_par", "lamp"], ["lamp"])
            tt("dve", lamp[:, 1, :], par[:, base + 64:base + 96], par[:, base + 96:base + 128], ALU.mult, ["c_par", "lamp"], ["lamp"])
            P.add("dve", lambda e: e.reduce_sum(lamt[:, 0:2], lamp[:, :, :], AX.X), ["lamp", "lamt"], ["lamt"])
            act(lamt[:, 2:4], lamt[:, 0:2], AF.Exp, ["lamt"], ["lamt"])
            tt("dve", lamt[:, 4:5], lamt[:, 3:4], lamt[:, 2:3], ALU.subtract, ["lamt"], ["lamt"])
            ts("dve", lamt[:, 4:5], lamt[:, 4:5], -lam_init, None, ALU.add, None, ["lamt"], ["lamt"])
            ts("dve", lamt[:, 5:6], par[:, PR_SUB + li:PR_SUB + li + 1], 1.0 - lam_init, None, ALU.mult, None,
               ["c_par", "lamt"], ["lamt"])
            dma("sp", sinkrow[:, :], posd, "sink", ["sinkrow"], ["sinkrow"])
            for h in range(8):
                act(sinkrow[:, h * 128:(h + 1) * 128], sinkrow[:, h * 128:(h + 1) * 128], AF.Exp,
                    ["sinkrow", "c_par"], ["sinkrow"], bias=par[:, PR_SINK + li * 8 + h:PR_SINK + li * 8 + h + 1],
                    scale=1.0)

        for li, ltrue in enumerate(layer_ids):
            P.epoch = li
            if stop_after == "pro":
                break
            layer_params(li, ltrue)
            if stop_after == "params":
                break
            rmsnorm(PR_G + (li * 3 + 0) * 8)
            if stop_after == "norm":
                break
            ffn()
            if stop_after == "ffn1":
                break
            rmsnorm(PR_G + (li * 3 + 1) * 8)
            for pidx in range(8):
                kind = "A" if pidx < 2 else ("B" if pidx < 4 else "C")
                nl = 3 if pidx % 2 == 1 else 2
                got = take(nl)
                (wqk, tqk), (wvv, tvv) = got[0], got[1]
                proj_qk(wqk, tqk, kind)
                proj_v(wvv, tvv, moba_gate(pidx) if kind == "A" else None)
                if kind == "A":
                    attn_full("A", pidx, li)
                elif kind == "B":
                    attn_full("B", pidx - 2, li)
                else:
                    swa(pidx, li)
                if pidx % 2 == 1:
                    w_out(got[2][0], got[2][1])
                if stop_after == ("pair", pidx):
                    break
            if stop_after is not None:
                break
            rmsnorm(PR_G + (li * 3 + 2) * 8)
            ffn()
        if do_final and stop_after is None:
            rmsnorm(PR_G + 96, final=True)
        outs = []
        for kc in range(8):
            outs.append(dma("sp", outd[kc * 128:(kc + 1) * 128, :], xT[:, kc, :], "out",
                            [xtok(kc, tc) for tc in range(4)], [("outdone", kc)]))
        P.add("sp", lambda e: e.nop(), [("outdone", kc) for kc in range(8)] + [("slot", s_) for s_ in range(NSLOT)], [])
        stats = P.emit(nc, st)
    return nc, stats


def _consts():
    cbm = np.zeros((128, CB_N), np.float32)
    k = np.arange(128)[:, None]
    q = np.arange(128)[None, :]
    cbm[:, CB_MASKC:CB_MASKC + 128] = (q < k)
    cbm[:, CB_TRI:CB_TRI + 128] = (q >= k)
    cbm[:, CB_ID:CB_ID + 128] = np.eye(128)
    E = np.zeros((128, 2, 8, 128), np.float32)
    for hl in range(2):
        for n in range(8):
            E[hl * 64 + n, hl, n, :] = 1.0
    cbm[:, CB_E:CB_E + 2048] = E.reshape(128, 2048)
    cbm[:, CB_ONES:CB_ONES + 128] = 1.0
    cf = np.zeros((128, CF_N), np.float64)
    p = np.arange(128, dtype=np.float64)
    ab = np.concatenate([SL_A, SL_B])
    for s in range(8):
        for d in range(-12, 4):
            cf[:, CF_TBAB + s * 16 + d + 12] = ab[s] * (p + 128.0 * d)
    for h in range(8):
        cf[:, CF_TBC + h * 2] = SL_C[h] * (p - 192.0)
        cf[:, CF_TBC + h * 2 + 1] = SL_C[h] * (p - 64.0)
    for b in range(8):
        for hl in range(2):
            for n in range(8):
                cf[:, CF_PAST + b * 16 + hl * 8 + n] = 0.0 if n < b else -1e30
                cf[:, CF_A30 + b * 16 + hl * 8 + n] = 30000.0 if n < b else 0.0
                cf[:, CF_BC + b * 16 + hl * 8 + n] = 0.0 if n == b else -30000.0
    cf[:, CF_ONES:CF_ONES + 64] = 1.0
    cf[:, CF_EPS] = EPS
    cf[:, CF_ID:CF_ID + 128] = np.eye(128)
    pos = np.zeros((128, 1024), np.float64)
    for h in range(8):
        pos[:, h * 128:(h + 1) * 128] = (SL_C[h] * (np.arange(128) - 64.0))[None, :]
    return cbm.astype(ml_dtypes.bfloat16), cf.astype(np.float32), pos.astype(np.float32)


def _perm_win():
    cols = []
    for j in range(2):
        cols += list(range(128 * j, 128 * j + 128)) + list(range(256 + 128 * j, 256 + 128 * j + 128)) + \
            list(range(512 + 128 * j, 512 + 128 * j + 128))
    for j in range(2):
        cols += list(range(768 + 128 * j, 768 + 128 * j + 128)) + list(range(1024 + 128 * j, 1024 + 128 * j + 128)) + \
            list(range(1280 + 128 * j, 1280 + 128 * j + 128))
    for j in range(4):
        kv = j // 2
        kc = list(range(2048 + 64 * kv, 2048 + 64 * kv + 64))
        vc = list(range(2176 + 64 * kv, 2176 + 64 * kv + 64))
        cols += list(range(1536 + 128 * j, 1536 + 128 * j + 128)) + kc + kc + vc + vc
    return np.asarray(cols, np.int64)


def _params(inp, layer_ids):
    par = np.zeros((128, PR_N), np.float32)

    def gl(v):
        return np.ascontiguousarray(np.asarray(v, np.float32).reshape(8, 128).T)
    for li, l in enumerate(layer_ids):
        par[:, PR_G + (li * 3 + 0) * 8:PR_G + (li * 3 + 0) * 8 + 8] = gl(inp["norm_ffn1"][l])
        par[:, PR_G + (li * 3 + 1) * 8:PR_G + (li * 3 + 1) * 8 + 8] = gl(inp["norm_mix"][l])
        par[:, PR_G + (li * 3 + 2) * 8:PR_G + (li * 3 + 2) * 8 + 8] = gl(inp["norm_ffn2"][l])
        par[:, PR_SUB + li] = np.tile(np.asarray(inp["diff_subln"][l], np.float32), 2)
        for j, nm in enumerate(("lam_q1", "lam_k1", "lam_q2", "lam_k2")):
            par[:, PR_LAM + li * 128 + j * 32:PR_LAM + li * 128 + j * 32 + 32] = np.asarray(inp[nm][l], np.float32)[None, :]
        par[:, PR_SINK + li * 8:PR_SINK + li * 8 + 8] = np.asarray(inp["sinks"][l], np.float32)[None, :]
    par[:, PR_G + 96:PR_G + 104] = gl(inp["final_norm"])
    return par


_CACHE = {}
_RUN_KW = {}
_LAST = []


def _get_nc(layer_ids, do_final, stop_after=None):
    key = (tuple(layer_ids), do_final, stop_after)
    if key not in _CACHE:
        _CACHE[key] = build(list(layer_ids), do_final, stop_after)[0]
    return _CACHE[key]


def run_layers(xT_list, inp, layer_ids, do_final, stop_after=None, core_ids=None):
    cbm, cf, pos = _consts()
    perm = _perm_win()
    ls = list(layer_ids)
    f32 = lambda a: np.ascontiguousarray(np.asarray(a, np.float32))
    shared = {
        "w1g": f32(inp["w1_gate"][ls]), "w1u": f32(inp["w1_up"][ls]), "w1d": f32(inp["w1_down"][ls]),
        "w2g": f32(inp["w2_gate"][ls]), "w2u": f32(inp["w2_up"][ls]), "w2d": f32(inp["w2_down"][ls]),
        "win": f32(np.asarray(inp["w_in"], np.float32)[ls][:, :, perm]), "wout": f32(inp["w_out"][ls]),
        "cb": cbm, "cf": cf, "par": _params(inp, ls), "posrow": pos,
    }
    nc = _get_nc(ls, do_final, stop_after)
    n = len(xT_list)
    in_maps = [dict(shared, xT=np.ascontiguousarray(x)) for x in xT_list]
    res = run_bass_kernel_spmd(nc, in_maps, core_ids=list(range(n)) if core_ids is None else core_ids, **_RUN_KW)
    _LAST.clear(); _LAST.append(res)
    return [r["outT"] for r in res.results]


def kernel(**inputs):
    inp = {k: np.asarray(v) for k, v in inputs.items()}
    x = np.asarray(inp["x"], np.float32)
    B = x.shape[0]
    xs = [np.ascontiguousarray(x[b].T) for b in range(B)]
    if FUSED:
        outs = run_layers(xs, inp, range(DEPTH), True)
    else:
        outs = xs
        for l in range(DEPTH):
            outs = run_layers(outs, inp, [l], l == DEPTH - 1)
    return np.stack([np.ascontiguousarray(o.T) for o in outs], axis=0).astype(np.float32)
```

```python
import math
from contextlib import ExitStack
import numpy as np
import ml_dtypes
import concourse.bass as bass
import concourse.mybir as mybir
from concourse.bass_utils import run_bass_kernel_spmd

F32 = mybir.dt.float32
BF16 = mybir.dt.bfloat16
AF = mybir.ActivationFunctionType
ALU = mybir.AluOpType
AX = mybir.AxisListType

D = 1024
T = 2048
DFF = 2816
NF = DFF // 128
G = 2
NG = NF // G
DEPTH = 4
EPS = 1e-6
NSLOT = 6
FUSED = True
SAME_ENGINE_SYNC = True

SLOPES = 2.0 ** (-8.0 * (np.arange(16, dtype=np.float64) + 1.0) / 16.0)
SL_C = SLOPES[0:8]
SL_B = SLOPES[8:12]
SL_A = SLOPES[12:16]

CB_MASKC = 0
CB_TRI = 128
CB_ID = 256
CB_E = 384
CB_ONES = 2432
CB_N = 2560
CF_TBAB = 0
CF_TBC = 128
CF_PAST = 144
CF_A30 = 272
CF_BC = 400
CF_ONES = 528
CF_EPS = 592
CF_ID = 600
CF_N = 728
PR_G = 0
PR_SUB = 104
PR_LAM = 108
PR_SINK = 620
PR_N = 652


class Op:
    __slots__ = ("eng", "fn", "deps", "needed", "sem", "val", "dma", "epoch")


class Prog:
    ENGS = ("pe", "act", "dve", "pool", "sp")

    def __init__(self):
        self.ops = {e: [] for e in self.ENGS}
        self.lastw = {}
        self.readers = {}
        self.epoch = 0

    def add(self, eng, fn, reads=(), writes=(), dma=None):
        op = Op()
        op.eng, op.fn, op.dma, op.epoch = eng, fn, dma, self.epoch
        op.needed = False
        op.sem = None
        op.val = 0
        deps = {}
        for t in reads:
            w = self.lastw.get(t)
            if w is not None:
                deps[id(w)] = w
        for t in writes:
            w = self.lastw.get(t)
            if w is not None:
                deps[id(w)] = w
            for r in self.readers.get(t, ()):
                deps[id(r)] = r
        out = []
        for d in deps.values():
            if d is op:
                continue
            if d.eng == eng and d.dma is None:
                if eng == "pe" or eng == "sp" or not SAME_ENGINE_SYNC:
                    continue
            out.append(d)
        op.deps = out
        for t in reads:
            if isinstance(t, str) and t.startswith("c_"):
                continue
            self.readers.setdefault(t, []).append(op)
        for t in writes:
            self.lastw[t] = op
            self.readers[t] = []
        self.ops[eng].append(op)
        return op

    def emit(self, nc, stack):
        sems = {}

        def getsem(key):
            if key not in sems:
                sems[key] = stack.enter_context(nc.semaphore("s%d" % len(sems)))
            return sems[key]

        for e in self.ENGS:
            for op in self.ops[e]:
                for d in op.deps:
                    d.needed = True
        cnt = {}
        for e in self.ENGS:
            for op in self.ops[e]:
                if op.dma is not None:
                    key = ("dma", op.dma)
                    cnt[key] = cnt.get(key, 0) + 16
                    op.sem, op.val = getsem(key), cnt[key]
                elif op.needed:
                    key = (e, op.epoch)
                    cnt[key] = cnt.get(key, 0) + 1
                    op.sem, op.val = getsem(key), cnt[key]
        block = stack.enter_context(nc.Block())
        stats = {}

        def run(eng_name, eng):
            waited = {}
            nw = 0
            for op in self.ops[eng_name]:
                need = {}
                for d in op.deps:
                    k = id(d.sem)
                    if waited.get(k, 0) >= d.val:
                        continue
                    if k not in need or need[k][1] < d.val:
                        need[k] = (d.sem, d.val)
                for k, (s, v) in need.items():
                    eng.wait_ge(s, v)
                    waited[k] = v
                    nw += 1
                inst = op.fn(eng)
                if op.dma is not None:
                    inst.then_inc(op.sem, 16)
                elif op.needed:
                    inst.then_inc(op.sem, 1)
            stats[eng_name] = (len(self.ops[eng_name]), nw)

        @block.tensor
        def _(e):
            run("pe", e)

        @block.scalar
        def _(e):
            run("act", e)

        @block.vector
        def _(e):
            run("dve", e)

        @block.gpsimd
        def _(e):
            run("pool", e)

        @block.sync
        def _(e):
            run("sp", e)

        return stats


def bcast_mid(ap, n):
    l = [list(x) for x in ap.ap]
    return bass.AP(ap.tensor, ap.offset, [l[0], [0, n]] + l[1:])


def build(layer_ids, do_final, stop_after=None):
    NL = len(layer_ids)
    nc = bass.Bass("TRN2", target_bir_lowering=False)
    dr = {}

    def din(name, shape, dt=F32):
        dr[name] = nc.dram_tensor(name, list(shape), dt, kind="ExternalInput").ap()
        return dr[name]

    xin = din("xT", [D, T])
    w1g = din("w1g", [NL, D, DFF]); w1u = din("w1u", [NL, D, DFF]); w1d = din("w1d", [NL, DFF, D])
    w2g = din("w2g", [NL, D, DFF]); w2u = din("w2u", [NL, D, DFF]); w2d = din("w2d", [NL, DFF, D])
    win = din("win", [NL, D, 8 * 384]); wout = din("wout", [NL, D, D])
    cbd = din("cb", [128, CB_N], BF16); cfd = din("cf", [128, CF_N]); prd = din("par", [128, PR_N])
    posd = din("posrow", [128, 1024])
    outd = nc.dram_tensor("outT", [D, T], F32, kind="ExternalOutput").ap()

    P = Prog()
    st = ExitStack()
    with st:
        def sb(name, shape, dt):
            return st.enter_context(nc.sbuf_tensor(name, list(shape), dt))

        xT = sb("xT_sb", [128, 8, T], F32)
        hT = sb("hT", [128, 8, T], BF16)
        ring = sb("ring", [128, NSLOT, 2048], BF16)
        qk = sb("qk", [128, 2, T], BF16)
        vaug = sb("vaug", [128, 16, 2, 128], BF16)
        mix = sb("mix", [128, 2, T], BF16)
        actb = sb("actb", [128, 4096], BF16)
        pt = sb("pt", [128, 3, 1024], BF16)
        mnegt = sb("mnegt", [128, T], BF16)
        mneg = sb("mneg", [128, 2, 128], F32)
        gm = sb("gm", [128, 16], F32)
        top = sb("top", [128, 16], F32)
        sel = sb("sel", [128, 16], F32)
        kms = sb("kms", [128, 8], F32)
        kmz = sb("kmz", [128, 2, 8], BF16)
        rstd = sb("rstd", [128, 2, 512], F32)
        sg = sb("sg", [128, 2, 512], BF16)
        bcsb = sb("bcsb", [128, 512], F32)
        osq = sb("osq", [128, 512], BF16)
        rden = sb("rden", [128, 512], F32)
        sinkrow = sb("sinkrow", [128, 1024], F32)
        cb = sb("cb_sb", [128, CB_N], BF16)
        cf = sb("cf_sb", [128, CF_N], F32)
        par = sb("par_sb", [128, PR_N], F32)
        lamt = sb("lamt", [128, 16], F32)
        lamp = sb("lamp", [128, 2, 32], F32)
        ps = st.enter_context(nc.psum_tensor("ps", [128, 8, 512], F32))
        o1 = rstd[:, 0, :]
        o2 = rstd[:, 1, :]
        TO1, TO2 = ("rstd", 0), ("rstd", 1)
        identf = cf[:, CF_ID:CF_ID + 128]
        KZ = [(actb[:, 0:2048], [("act", 0), ("act", 1)]), (actb[:, 2048:4096], [("act", 2)]),
              (qk[:, 1, :], [("qk", 1, tc_) for tc_ in range(4)]), (mnegt[:, :], [("mnegt", c_) for c_ in range(4)])]

        ones_bf = cb[:, CB_ONES:CB_ONES + 128]
        ident = cb[:, CB_ID:CB_ID + 128]
        tri = cb[:, CB_TRI:CB_TRI + 128]
        maskc = cb[:, CB_MASKC:CB_MASKC + 256]

        def mm(out, lhsT, rhs, start, stop, reads, writes, tp=None):
            kw = {}
            if tp is not None and tp[0] == 96:
                kw["tile_position"] = tp
            return P.add("pe", lambda e: e.matmul(out, lhsT=lhsT, rhs=rhs, start=start, stop=stop, **kw),
                         reads, writes)

        def act(out, in_, func, reads, writes, bias=None, scale=None):
            kw = {}
            if bias is not None:
                kw["bias"] = bias
            if scale is not None:
                kw["scale"] = scale
            return P.add("act", lambda e: e.activation(out, in_, func, **kw), reads, writes)

        def tt(eng, out, in0, in1, op, reads, writes):
            return P.add(eng, lambda e: e.tensor_tensor(out, in0, in1, op), reads, writes)

        def stt(out, in0, scalar, in1, op0, op1, reads, writes):
            return P.add("dve", lambda e: e.scalar_tensor_tensor(out, in0, scalar, in1, op0, op1), reads, writes)

        def ts(eng, out, in0, s1, s2, op0, op1, reads, writes):
            if op1 is None:
                return P.add(eng, lambda e: e.tensor_scalar(out, in0, s1, None, op0), reads, writes)
            return P.add(eng, lambda e: e.tensor_scalar(out, in0, s1, s2, op0, op1), reads, writes)

        def recip(out, in_, reads, writes):
            return P.add("dve", lambda e: e.reciprocal(out, in_), reads, writes)

        def copy(eng, out, in_, reads, writes):
            if eng == "act":
                return P.add("act", lambda e: e.copy(out, in_), reads, writes)
            return P.add(eng, lambda e: e.tensor_copy(out, in_), reads, writes)

        def dma(eng, out, in_, key, reads, writes):
            return P.add(eng, lambda e: e.dma_start(out=out, in_=in_), reads, writes, dma=key)

        def xtok(kc, tc):
            return ("x", kc, tc)

        loads = []

        def colblk(w, l, c0, n):
            return w[l, :, c0:c0 + n].rearrange("(kc p) n -> p kc n", p=128)

        def rowblk(w, l, r0):
            return w[l, r0:r0 + 256, :].rearrange("(rc p) n -> p rc n", p=128)

        def ffn_loads(wg, wu, wd, l):
            for g in range(NG):
                loads.append(("c", colblk(wg, l, g * 256, 256), 256))
                loads.append(("c", colblk(wu, l, g * 256, 256), 256))
                loads.append(("r", rowblk(wd, l, g * 256), 0))

        for l in range(NL):
            ffn_loads(w1g, w1u, w1d, l)
            for p in range(8):
                loads.append(("c", colblk(win, l, p * 384, 256), 256))
                loads.append(("c", colblk(win, l, p * 384 + 256, 128), 128))
                if p % 2 == 1:
                    loads.append(("r", rowblk(wout, l, (p // 2) * 256), 0))
            ffn_loads(w2g, w2u, w2d, l)
        wstate = {"rec": 0, "next": 0}

        def slot_view(i):
            kind, src, n = loads[i]
            s = i % NSLOT
            flat = ring[:, s, :]
            if kind == "c":
                return flat.rearrange("p (kc n) -> p kc n", n=256)
            return flat.rearrange("p (rc n) -> p rc n", n=1024)

        def take(n):
            a = wstate["next"]
            wstate["next"] = a + n
            upto = min(len(loads), a + NSLOT)
            while wstate["rec"] < upto:
                i = wstate["rec"]
                kind, src, ncol = loads[i]
                v = slot_view(i)
                dst = v[:, :, 0:ncol] if kind == "c" else v
                dma("pool", dst, src, ("slot", i % NSLOT), [], [("slot", i % NSLOT)])
                wstate["rec"] += 1
            return [(slot_view(i), ("slot", i % NSLOT)) for i in range(a, a + n)]

        dma("sp", cb[:, :], cbd, "cst0", [], ["c_cb"])
        dma("sp", cf[:, :], cfd, "cst1", [], ["c_cf"])
        dma("sp", par[:, :], prd, "cst2", [], ["c_par"])
        for kc in range(8):
            dma("sp", xT[:, kc, :], xin[kc * 128:(kc + 1) * 128, :], "xin", [], [xtok(kc, tc) for tc in range(4)])
        P.add("pool", lambda e: e.memset(vaug[:, :, :, :], 1.0), [], [("v", t) for t in range(16)])
        P.add("pool", lambda e: e.memset(mneg[:, :, :], 0.0), [], ["mneg"])
        P.add("pool", lambda e: e.memset(kmz[:, :, :], 0.0), [], ["kmz"])

        def rmsnorm(gcol, final=False):
            sq = actb[:, :].rearrange("p (k n) -> p k n", n=512)
            for tc in range(4):
                cs = slice(tc * 512, (tc + 1) * 512)
                bank = 5 + (tc % 2)
                rb = rstd[:, tc % 2, :]
                act(sq, xT[:, :, cs], AF.Square, [xtok(kc, tc) for kc in range(8)], [("act", 0), ("act", 1), ("act", 2)])
                for kc in range(8):
                    mm(ps[:, bank, :], ones_bf, sq[:, kc, :], kc == 0, kc == 7,
                       ["c_cb", ("act", 0), ("act", 1), ("act", 2)], [("ps", bank)])
                act(rb, ps[:, bank, :], AF.Ln, [("ps", bank)], [("rstd", tc % 2)], bias=cf[:, CF_EPS:CF_EPS + 1],
                    scale=1.0 / D)
                act(rb, rb, AF.Exp, [("rstd", tc % 2)], [("rstd", tc % 2)], scale=-0.5)
                for kc in range(8):
                    if final:
                        stt(xT[:, kc, cs], xT[:, kc, cs], par[:, gcol + kc:gcol + kc + 1], rb, ALU.mult, ALU.mult,
                            [xtok(kc, tc), ("rstd", tc % 2), "c_par"], [xtok(kc, tc)])
                    else:
                        stt(hT[:, kc, cs], xT[:, kc, cs], par[:, gcol + kc:gcol + kc + 1], rb, ALU.mult, ALU.mult,
                            [xtok(kc, tc), ("rstd", tc % 2), "c_par"], [("h", kc, tc)])


        def ffn():
            pend = None
            cnt = 0
            for g in range(NG):
                if pend is not None:
                    pend()
                    pend = None
                (wgv, tg), (wuv, tu), (wdv, td) = take(3)
                for tc in range(4):
                    cs = slice(tc * 512, (tc + 1) * 512)
                    ai = cnt % 2
                    av = actb[:, ai * 1024:(ai + 1) * 1024].rearrange("p (f n) -> p f n", n=512)
                    for fi in range(G):
                        bg, bu = 2 * (fi % 2), 2 * (fi % 2) + 1
                        for kc in range(8):
                            mm(ps[:, bg, :], wgv[:, kc, fi * 128:(fi + 1) * 128], hT[:, kc, cs], kc == 0, kc == 7,
                               [tg, ("h", kc, tc)], [("ps", bg)])
                        for kc in range(8):
                            mm(ps[:, bu, :], wuv[:, kc, fi * 128:(fi + 1) * 128], hT[:, kc, cs], kc == 0, kc == 7,
                               [tu, ("h", kc, tc)], [("ps", bu)])
                        act(sg[:, fi % 2, :], ps[:, bg, :], AF.Silu, [("ps", bg)], [("sg", fi % 2)])
                        tt("dve", av[:, fi, :], sg[:, fi % 2, :], ps[:, bu, :], ALU.mult,
                           [("sg", fi % 2), ("ps", bu)], [("act", ai)])
                    if pend is not None:
                        pend()

                    def down(av=av, ai=ai, wdv=wdv, td=td, tc=tc, cs=cs):
                        for dc in range(8):
                            b = 4 + (dc % 4)
                            for fi in range(G):
                                mm(ps[:, b, :], wdv[:, fi, dc * 128:(dc + 1) * 128], av[:, fi, :], fi == 0, fi == G - 1,
                                   [td, ("act", ai)], [("ps", b)])
                            stt(xT[:, dc, cs], ps[:, b, :], 0.5, xT[:, dc, cs], ALU.mult, ALU.add,
                                [("ps", b), xtok(dc, tc)], [xtok(dc, tc)])
                    pend = down
                    cnt += 1
            if pend is not None:
                pend()

        def proj_qk(wv, tw, kind):
            if kind == "B":
                for buf, toks in KZ:
                    P.add("pool", lambda e, buf=buf: e.memset(buf, 0.0), [], toks)
            else:
                P.add("pool", lambda e: e.memset(actb[64:128, 0:2048], 0.0), [], KZ[0][1])
                P.add("pool", lambda e: e.memset(actb[0:64, 2048:4096], 0.0), [], KZ[1][1])
            n = 0
            for j in range(2):
                for tc in range(4):
                    cs = slice(tc * 512, (tc + 1) * 512)
                    b = n % 4
                    n += 1
                    eng = "act" if n % 2 else "dve"
                    for kc in range(8):
                        mm(ps[:, b, :], wv[:, kc, j * 128:(j + 1) * 128], hT[:, kc, cs], kc == 0, kc == 7,
                           [tw, ("h", kc, tc)], [("ps", b)])
                    if j == 0:
                        copy(eng, qk[:, 0, cs], ps[:, b, :], [("ps", b)], [("qk", 0, tc)])
                    elif kind == "B":
                        copy(eng, KZ[0][0][0:32, cs], ps[0:32, b, :], [("ps", b)], KZ[0][1])
                        copy(eng, KZ[1][0][32:64, cs], ps[32:64, b, :], [("ps", b)], KZ[1][1])
                        copy(eng, KZ[2][0][64:96, cs], ps[64:96, b, :], [("ps", b)], KZ[2][1])
                        copy(eng, KZ[3][0][64:128, cs], ps[64:128, b, :], [("ps", b)], KZ[3][1])
                        P.add("pool", lambda e, cs=cs: e.memset(KZ[3][0][64:96, cs], 0.0), [], KZ[3][1])
                    else:
                        copy(eng, KZ[0][0][0:64, cs], ps[0:64, b, :], [("ps", b)], KZ[0][1])
                        copy(eng, KZ[1][0][64:128, cs], ps[64:128, b, :], [("ps", b)], KZ[1][1])
                        if kind == "A":
                            P.add("dve", lambda e, tc=tc, cs=cs: e.reduce_sum(
                                kms[0:64, 2 * tc:2 * tc + 2],
                                KZ[0][0][0:64, cs].rearrange("p (n l) -> p n l", l=256), AX.X),
                                KZ[0][1], ["kms"])
                            P.add("dve", lambda e, tc=tc, cs=cs: e.reduce_sum(
                                kms[64:128, 2 * tc:2 * tc + 2],
                                KZ[1][0][64:128, cs].rearrange("p (n l) -> p n l", l=256), AX.X),
                                KZ[1][1], ["kms"])
            if kind == "A":
                copy("dve", kmz[0:64, 0, :], kms[0:64, :], ["kms", "kmz"], ["kmz"])
                copy("dve", kmz[64:128, 1, :], kms[64:128, :], ["kms", "kmz"], ["kmz"])

        def proj_v(wv, tw, per_t=None):
            for t in range(16):
                b = 4 + (t % 2)
                cs = slice(t * 128, (t + 1) * 128)
                oc = slice(0, 128)
                for kc in range(8):
                    mm(ps[:, b, oc], hT[:, kc, cs], wv[:, kc, 0:128], kc == 0, kc == 7,
                       [tw, ("h", kc, t // 4)], [("ps", b)])
                copy("act" if t % 2 else "dve", vaug[:, t, :, 0:64],
                     ps[:, b, oc].rearrange("p (h d) -> p h d", d=64), [("ps", b)], [("v", t)])
                if per_t is not None:
                    per_t(t)

        ncount = [0]

        def normalize(ob, dst, dtok, h_sink=None):
            buf, tk = (rden, "rden") if ncount[0] % 2 == 0 else (bcsb, "bcsb")
            ncount[0] += 1
            if h_sink is not None:
                srow = bcast_mid(sinkrow[64:128, h_sink * 128:(h_sink + 1) * 128], 4)
                tt("dve", buf[0:64, :].rearrange("p (a b) -> p a b", b=128),
                   ps[64:128, ob, :].rearrange("p (a b) -> p a b", b=128), srow, ALU.add,
                   [("ps", ob), "sinkrow"], [tk])
                if h_sink == 0:
                    recip(buf[0:64, :], buf[0:64, :], [tk], [tk])
                else:
                    act(buf[0:64, :], buf[0:64, :], AF.Ln, [tk], [tk])
            else:
                act(buf[0:64, :], ps[64:128, ob, :], AF.Ln, [("ps", ob)], [tk])
            if h_sink != 0:
                act(buf[0:64, :], buf[0:64, :], AF.Exp, [tk], [tk], scale=-1.0)
            tt("dve", dst, ps[0:64, ob, :], buf[0:64, :], ALU.mult, [("ps", ob), tk], dtok)

        def attn_full(kind, pidx, li):
            nmap = 2 if kind == "B" else 1
            scale = 32 ** -0.5 if kind == "B" else 0.125
            pend = {"p1": None, "p2": None}
            ALLPT = [("pt", i_) for i_ in range(3)] + [("pth", i_) for i_ in range(6)]
            if kind == "B":
                P.add("pool", lambda e: e.memset(gm[:, 0:1], 0.0), ["gm"], ALLPT + ["gm"])

            def after_loop(p1):
                if pend["p2"] is not None:
                    pend["p2"]()
                    pend["p2"] = None
                if pend["p1"] is not None:
                    pend["p2"] = pend["p1"]()
                pend["p1"] = p1

            for hl in range(2):
                h = 2 * pidx + hl
                tbcol = CF_TBAB + (h if kind == "A" else 4 + h) * 16
                for c in range(4):
                    nkt = 4 * c + 4
                    it = hl * 4 + c
                    if kind == "B":
                        obanks = [4, 5] if it % 2 == 0 else [6, 7]
                    else:
                        obanks = [4 + (it % 2)]
                    steps = []
                    for kt in range(nkt):
                        j = kt - 4 * c
                        col0 = max(j, 0) * 128
                        ncols = 512 - col0
                        qs = slice(c * 512 + col0, (c + 1) * 512)
                        ks = slice(kt * 128, (kt + 1) * 128)
                        pi = kt % 3
                        if kind == "A":
                            stb = [kt % 3]
                        else:
                            stb = [2 * (kt % 2), 2 * (kt % 2) + 1]

                        def st_fn(kt=kt, j=j, col0=col0, ncols=ncols, qs=qs, ks=ks, stb=stb):
                            if kind == "A":
                                kzb, kzt = KZ[hl]
                                mm(ps[:, stb[0], 0:ncols], kzb[:, ks], qk[:, 0, qs],
                                   True, False, kzt + [("qk", 0, c)], [("ps", stb[0])])
                                ec = CB_E + hl * 1024 + (kt // 2) * 128
                                mm(ps[:, stb[0], 0:ncols], cb[:, ec:ec + 128], mnegt[:, qs], False, True,
                                   ["c_cb", ("mnegt", c)], [("ps", stb[0])])
                            else:
                                raise AssertionError

                        def ex_fn(kt=kt, j=j, ncols=ncols, stb=stb, pi=pi):
                            bias = cf[:, tbcol + j + 12:tbcol + j + 13]
                            if kind == "A":
                                act(pt[:, pi, 0:ncols], ps[:, stb[0], 0:ncols], AF.Exp, [("ps", stb[0]), "c_cf"],
                                    [("pt", pi)], bias=bias, scale=scale)
                                if j >= 0:
                                    tt("pool", pt[:, pi, 0:128], pt[:, pi, 0:128], tri, ALU.mult,
                                       [("pt", pi), "c_cb"], [("pt", pi)])
                            else:
                                raise AssertionError

                        def pv_fn(kt=kt, col0=col0, ncols=ncols, pi=pi):
                            for m in range(nmap):
                                rhs = pt[:, pi, m * 512:m * 512 + ncols]
                                mm(ps[:, obanks[m], col0:512], vaug[:, kt, hl, :], rhs, kt == 0, kt == nkt - 1,
                                   [("v", kt), ("pt", pi)], [("ps", obanks[m])])
                        if kind == "A":
                            steps.append((st_fn, ex_fn, pv_fn))
                        else:
                            for m in range(2):
                                sidx = 2 * kt + m
                                bnk = sidx % 4
                                hb = sidx % 6
                                pth = pt[:, hb // 2, (hb % 2) * 512:(hb % 2) * 512 + 512]
                                kzb, kzt = KZ[2 * hl + m]

                                def st_b(kzb=kzb, kzt=kzt, bnk=bnk, ncols=ncols, qs=qs, ks=ks):
                                    mm(ps[:, bnk, 0:ncols], kzb[:, ks], qk[:, 0, qs],
                                       True, True, kzt + [("qk", 0, c)], [("ps", bnk)])

                                def ex_b(bnk=bnk, hb=hb, pth=pth, j=j, ncols=ncols):
                                    act(pth[:, 0:ncols], ps[:, bnk, 0:ncols], AF.Exp, [("ps", bnk), "c_cf"],
                                        [("pth", hb)], bias=cf[:, tbcol + j + 12:tbcol + j + 13], scale=scale)
                                    if j >= 0:
                                        tt("pool", pth[:, 0:128], pth[:, 0:128], tri, ALU.mult,
                                           [("pth", hb), "c_cb"], [("pth", hb)])

                                def pv_b(m=m, hb=hb, pth=pth, kt=kt, col0=col0, ncols=ncols):
                                    mm(ps[:, obanks[m], col0:512], vaug[:, kt, hl, :], pth[:, 0:ncols],
                                       kt == 0, kt == nkt - 1, [("v", kt), ("pth", hb)], [("ps", obanks[m])])
                                steps.append((st_b, ex_b, pv_b))
                    LOOK = 2 if kind == "A" else 3
                    ns = len(steps)
                    for i in range(ns + LOOK):
                        if i < ns:
                            steps[i][0]()
                            steps[i][1]()
                        if i - LOOK >= 0:
                            steps[i - LOOK][2]()
                    cs = slice(c * 512, (c + 1) * 512)
                    dst = mix[hl * 64:(hl + 1) * 64, pidx % 2, cs]
                    dtok = [("mix", pidx % 2, c, hl)]
                    if kind == "A":
                        def post1(ob=obanks[0], dst=dst, dtok=dtok):
                            normalize(ob, dst, dtok)
                            return None
                        after_loop(post1)
                    else:
                      def post1(obanks=obanks, dst=dst, dtok=dtok):
                        normalize(obanks[0], o1[0:64, :], [TO1])
                        normalize(obanks[1], o2[0:64, :], [TO2])
                        stt(o1[0:64, :], o2[0:64, :], lamt[0:64, 4:5], o1[0:64, :], ALU.mult, ALU.add,
                            [TO1, TO2, "lamt"], [TO1])
                        tt("dve", osq[0:64, :], o1[0:64, :], o1[0:64, :], ALU.mult, [TO1], ["osq"])

                        def phase2(dst=dst, dtok=dtok):
                            mm(ps[0:64, 3, :], cb[0:64, CB_ONES:CB_ONES + 64], osq[0:64, :], True, True,
                               ["c_cb", "osq"], [("ps", 3)])
                            act(o2[0:64, :], ps[0:64, 3, :], AF.Ln, [("ps", 3)], [TO2],
                                bias=cf[0:64, CF_EPS:CF_EPS + 1], scale=1.0 / 64)
                            act(o2[0:64, :], o2[0:64, :], AF.Exp, [TO2], [TO2], scale=-0.5)
                            stt(dst, o1[0:64, :], lamt[0:64, 5:6], o2[0:64, :], ALU.mult, ALU.mult,
                                [TO1, TO2, "lamt"], dtok)
                        return phase2
                      after_loop(post1)
            after_loop(None)
            after_loop(None)
            if kind == "B":
                P.add("pool", lambda e: e.memset(gm[:, 0:1], 0.0), ["gm"], ALLPT + ["gm"])

        def moba_gate(pidx):
            def step(qt):
                b = qt // 2
                qs = slice(qt * 128, (qt + 1) * 128)
                mm(ps[:, 3, 0:16], qk[:, 0, qs], kmz[:, :, :].rearrange("p h n -> p (h n)"),
                   True, True, [("qk", 0, qt // 4), "kmz"], [("ps", 3)])
                tt("dve", gm[:, :], ps[:, 3, 0:16], cf[:, CF_PAST + b * 16:CF_PAST + b * 16 + 16], ALU.add,
                   [("ps", 3), "c_cf"], ["gm"])
                for hl in range(2):
                    P.add("dve", lambda e, hl=hl: e.max(top[:, hl * 8:hl * 8 + 8], gm[:, hl * 8:hl * 8 + 8]),
                          ["gm"], [("top", hl)])
                for hl in range(2):
                    stt(sel[:, hl * 8:hl * 8 + 8], gm[:, hl * 8:hl * 8 + 8], top[:, hl * 8 + 2:hl * 8 + 3],
                        cf[:, CF_A30 + b * 16 + hl * 8:CF_A30 + b * 16 + hl * 8 + 8], ALU.is_ge, ALU.mult,
                        ["gm", ("top", hl), "c_cf"], [("sel", hl)])
                mi = qt % 2
                tt("dve", mneg[:, mi, :].rearrange("p (h n) -> p h n", n=64)[:, :, 0:8],
                   sel[:, :].rearrange("p (h n) -> p h n", n=8),
                   cf[:, CF_BC + b * 16:CF_BC + b * 16 + 16].rearrange("p (h n) -> p h n", n=8), ALU.add,
                   [("sel", 0), ("sel", 1), "c_cf", "mneg"], [("mneg", mi)])
                if qt > 0:
                    xpose(qt - 1)
                if qt == 15:
                    xpose(15)

            def xpose(qt):
                mi = qt % 2
                pc = (qt % 4) * 128
                P.add("pe", lambda e, mi=mi, pc=pc: e.transpose(ps[:, 7, pc:pc + 128], mneg[:, mi, :], identf),
                      [("mneg", mi), "c_cf"], [("ps", 7)])
                if qt % 4 == 3:
                    c = qt // 4
                    copy("act", mnegt[:, c * 512:(c + 1) * 512], ps[:, 7, :], [("ps", 7)], [("mnegt", c)])
            return step

        def swa(pidx, li):
            cp = pidx - 4
            pendn = [None]
            for hl in range(2):
                h = 2 * cp + hl
                kzb, kzt = KZ[hl]
                for c in range(4):
                    ob = 4 + ((hl * 4 + c) % 2)
                    steps = []
                    for qi in range(4):
                        qt = 4 * c + qi
                        qs = slice(qt * 128, (qt + 1) * 128)
                        sbk = qt % 4
                        pi = qt % 3

                        def st_fn(qt=qt, qs=qs, sbk=sbk):
                            mm(ps[:, sbk, 128:256], kzb[:, qs], qk[:, 0, qs], True, True,
                               kzt + [("qk", 0, qt // 4)], [("ps", sbk)])
                            if qt > 0:
                                ks = slice((qt - 1) * 128, qt * 128)
                                mm(ps[:, sbk, 0:128], kzb[:, ks], qk[:, 0, qs], True, True,
                                   kzt + [("qk", 0, qt // 4)], [("ps", sbk)])

                        def ex_fn(qt=qt, sbk=sbk, pi=pi):
                            act(pt[:, pi, 128:256], ps[:, sbk, 128:256], AF.Exp, [("ps", sbk), "c_cf"], [("pt", pi)],
                                bias=cf[:, CF_TBC + h * 2 + 1:CF_TBC + h * 2 + 2], scale=0.125)
                            if qt > 0:
                                act(pt[:, pi, 0:128], ps[:, sbk, 0:128], AF.Exp, [("ps", sbk), "c_cf"], [("pt", pi)],
                                    bias=cf[:, CF_TBC + h * 2:CF_TBC + h * 2 + 1], scale=0.125)
                                tt("pool", pt[:, pi, 0:256], pt[:, pi, 0:256], maskc, ALU.mult,
                                   [("pt", pi), "c_cb"], [("pt", pi)])
                            else:
                                tt("pool", pt[:, pi, 128:256], pt[:, pi, 128:256], tri, ALU.mult,
                                   [("pt", pi), "c_cb"], [("pt", pi)])

                        def pv_fn(qt=qt, qi=qi, pi=pi):
                            oc = slice(qi * 128, (qi + 1) * 128)
                            if qt > 0:
                                mm(ps[:, ob, oc], vaug[:, qt - 1, hl, :], pt[:, pi, 0:128], True, False,
                                   [("v", qt - 1), ("pt", pi)], [("ps", ob)])
                                mm(ps[:, ob, oc], vaug[:, qt, hl, :], pt[:, pi, 128:256], False, True,
                                   [("v", qt), ("pt", pi)], [("ps", ob)])
                            else:
                                mm(ps[:, ob, oc], vaug[:, qt, hl, :], pt[:, pi, 128:256], True, True,
                                   [("v", qt), ("pt", pi)], [("ps", ob)])
                        steps.append((st_fn, ex_fn, pv_fn))
                    LOOK = 2
                    for i in range(4 + LOOK):
                        if i < 4:
                            steps[i][0]()
                            steps[i][1]()
                        if i - LOOK >= 0:
                            steps[i - LOOK][2]()
                    cs = slice(c * 512, (c + 1) * 512)
                    if pendn[0] is not None:
                        pendn[0]()

                    def postn(ob=ob, hl=hl, cs=cs, c=c, h=h):
                        normalize(ob, mix[hl * 64:(hl + 1) * 64, pidx % 2, cs], [("mix", pidx % 2, c, hl)], h_sink=h)
                    pendn[0] = postn
            if pendn[0] is not None:
                pendn[0]()

        def w_out(wv, tw):
            n = 0
            for tc in range(4):
                cs = slice(tc * 512, (tc + 1) * 512)
                for dc in range(8):
                    b = n % 4
                    n += 1
                    for rc in range(2):
                        mm(ps[:, b, :], wv[:, rc, dc * 128:(dc + 1) * 128], mix[:, rc, cs], rc == 0, rc == 1,
                           [tw, ("mix", rc, tc, 0), ("mix", rc, tc, 1)], [("ps", b)])
                    tt("dve", xT[:, dc, cs], ps[:, b, :], xT[:, dc, cs], ALU.add, [("ps", b), xtok(dc, tc)],
                       [xtok(dc, tc)])

        def layer_params(li, ltrue):
            lam_init = 0.8 - 0.6 * math.exp(-0.3 * ltrue)
            base = PR_LAM + li * 128
            tt("dve", lamp[:, 0, :], par[:, base:base + 32], par[:, base + 32:base + 64], ALU.mult, ["c_par", "lamp"], ["lamp"])
            tt("dve", lamp[:, 1, :], par[:, base + 64:base + 96], par[:, base + 96:base + 128], ALU.mult, ["c_par", "lamp"], ["lamp"])
            P.add("dve", lambda e: e.reduce_sum(lamt[:, 0:2], lamp[:, :, :], AX.X), ["lamp", "lamt"], ["lamt"])
            act(lamt[:, 2:4], lamt[:, 0:2], AF.Exp, ["lamt"], ["lamt"])
            tt("dve", lamt[:, 4:5], lamt[:, 3:4], lamt[:, 2:3], ALU.subtract, ["lamt"], ["lamt"])
            ts("dve", lamt[:, 4:5], lamt[:, 4:5], -lam_init, None, ALU.add, None, ["lamt"], ["lamt"])
            ts("dve", lamt[:, 5:6], par[:, PR_SUB + li:PR_SUB + li + 1], 1.0 - lam_init, None, ALU.mult, None,
               ["c_par", "lamt"], ["lamt"])
            dma("sp", sinkrow[:, :], posd, "sink", ["sinkrow"], ["sinkrow"])
            for h in range(8):
                act(sinkrow[:, h * 128:(h + 1) * 128], sinkrow[:, h * 128:(h + 1) * 128], AF.Exp,
                    ["sinkrow", "c_par"], ["sinkrow"], bias=par[:, PR_SINK + li * 8 + h:PR_SINK + li * 8 + h + 1],
                    scale=1.0)

        for li, ltrue in enumerate(layer_ids):
            P.epoch = li
            if stop_after == "pro":
                break
            layer_params(li, ltrue)
            if stop_after == "params":
                break
            rmsnorm(PR_G + (li * 3 + 0) * 8)
            if stop_after == "norm":
                break
            ffn()
            if stop_after == "ffn1":
                break
            rmsnorm(PR_G + (li * 3 + 1) * 8)
            for pidx in range(8):
                kind = "A" if pidx < 2 else ("B" if pidx < 4 else "C")
                nl = 3 if pidx % 2 == 1 else 2
                got = take(nl)
                (wqk, tqk), (wvv, tvv) = got[0], got[1]
                proj_qk(wqk, tqk, kind)
                proj_v(wvv, tvv, moba_gate(pidx) if kind == "A" else None)
                if kind == "A":
                    attn_full("A", pidx, li)
                elif kind == "B":
                    attn_full("B", pidx - 2, li)
                else:
                    swa(pidx, li)
                if pidx % 2 == 1:
                    w_out(got[2][0], got[2][1])
                if stop_after == ("pair", pidx):
                    break
            if stop_after is not None:
                break
            rmsnorm(PR_G + (li * 3 + 2) * 8)
            ffn()
        if do_final and stop_after is None:
            rmsnorm(PR_G + 96, final=True)
        outs = []
        for kc in range(8):
            outs.append(dma("sp", outd[kc * 128:(kc + 1) * 128, :], xT[:, kc, :], "out",
                            [xtok(kc, tc) for tc in range(4)], [("outdone", kc)]))
        P.add("sp", lambda e: e.nop(), [("outdone", kc) for kc in range(8)] + [("slot", s_) for s_ in range(NSLOT)], [])
        stats = P.emit(nc, st)
    return nc, stats


def _consts():
    cbm = np.zeros((128, CB_N), np.float32)
    k = np.arange(128)[:, None]
    q = np.arange(128)[None, :]
    cbm[:, CB_MASKC:CB_MASKC + 128] = (q < k)
    cbm[:, CB_TRI:CB_TRI + 128] = (q >= k)
    cbm[:, CB_ID:CB_ID + 128] = np.eye(128)
    E = np.zeros((128, 2, 8, 128), np.float32)
    for hl in range(2):
        for n in range(8):
            E[hl * 64 + n, hl, n, :] = 1.0
    cbm[:, CB_E:CB_E + 2048] = E.reshape(128, 2048)
    cbm[:, CB_ONES:CB_ONES + 128] = 1.0
    cf = np.zeros((128, CF_N), np.float64)
    p = np.arange(128, dtype=np.float64)
    ab = np.concatenate([SL_A, SL_B])
    for s in range(8):
        for d in range(-12, 4):
            cf[:, CF_TBAB + s * 16 + d + 12] = ab[s] * (p + 128.0 * d)
    for h in range(8):
        cf[:, CF_TBC + h * 2] = SL_C[h] * (p - 192.0)
        cf[:, CF_TBC + h * 2 + 1] = SL_C[h] * (p - 64.0)
    for b in range(8):
        for hl in range(2):
            for n in range(8):
                cf[:, CF_PAST + b * 16 + hl * 8 + n] = 0.0 if n < b else -1e30
                cf[:, CF_A30 + b * 16 + hl * 8 + n] = 30000.0 if n < b else 0.0
                cf[:, CF_BC + b * 16 + hl * 8 + n] = 0.0 if n == b else -30000.0
    cf[:, CF_ONES:CF_ONES + 64] = 1.0
    cf[:, CF_EPS] = EPS
    cf[:, CF_ID:CF_ID + 128] = np.eye(128)
    pos = np.zeros((128, 1024), np.float64)
    for h in range(8):
        pos[:, h * 128:(h + 1) * 128] = (SL_C[h] * (np.arange(128) - 64.0))[None, :]
    return cbm.astype(ml_dtypes.bfloat16), cf.astype(np.float32), pos.astype(np.float32)


def _perm_win():
    cols = []
    for j in range(2):
        cols += list(range(128 * j, 128 * j + 128)) + list(range(256 + 128 * j, 256 + 128 * j + 128)) + \
            list(range(512 + 128 * j, 512 + 128 * j + 128))
    for j in range(2):
        cols += list(range(768 + 128 * j, 768 + 128 * j + 128)) + list(range(1024 + 128 * j, 1024 + 128 * j + 128)) + \
            list(range(1280 + 128 * j, 1280 + 128 * j + 128))
    for j in range(4):
        kv = j // 2
        kc = list(range(2048 + 64 * kv, 2048 + 64 * kv + 64))
        vc = list(range(2176 + 64 * kv, 2176 + 64 * kv + 64))
        cols += list(range(1536 + 128 * j, 1536 + 128 * j + 128)) + kc + kc + vc + vc
    return np.asarray(cols, np.int64)


def _params(inp, layer_ids):
    par = np.zeros((128, PR_N), np.float32)

    def gl(v):
        return np.ascontiguousarray(np.asarray(v, np.float32).reshape(8, 128).T)
    for li, l in enumerate(layer_ids):
        par[:, PR_G + (li * 3 + 0) * 8:PR_G + (li * 3 + 0) * 8 + 8] = gl(inp["norm_ffn1"][l])
        par[:, PR_G + (li * 3 + 1) * 8:PR_G + (li * 3 + 1) * 8 + 8] = gl(inp["norm_mix"][l])
        par[:, PR_G + (li * 3 + 2) * 8:PR_G + (li * 3 + 2) * 8 + 8] = gl(inp["norm_ffn2"][l])
        par[:, PR_SUB + li] = np.tile(np.asarray(inp["diff_subln"][l], np.float32), 2)
        for j, nm in enumerate(("lam_q1", "lam_k1", "lam_q2", "lam_k2")):
            par[:, PR_LAM + li * 128 + j * 32:PR_LAM + li * 128 + j * 32 + 32] = np.asarray(inp[nm][l], np.float32)[None, :]
        par[:, PR_SINK + li * 8:PR_SINK + li * 8 + 8] = np.asarray(inp["sinks"][l], np.float32)[None, :]
    par[:, PR_G + 96:PR_G + 104] = gl(inp["final_norm"])
    return par


_CACHE = {}
_RUN_KW = {}
_LAST = []


def _get_nc(layer_ids, do_final, stop_after=None):
    key = (tuple(layer_ids), do_final, stop_after)
    if key not in _CACHE:
        _CACHE[key] = build(list(layer_ids), do_final, stop_after)[0]
    return _CACHE[key]


def run_layers(xT_list, inp, layer_ids, do_final, stop_after=None, core_ids=None):
    cbm, cf, pos = _consts()
    perm = _perm_win()
    ls = list(layer_ids)
    f32 = lambda a: np.ascontiguousarray(np.asarray(a, np.float32))
    shared = {
        "w1g": f32(inp["w1_gate"][ls]), "w1u": f32(inp["w1_up"][ls]), "w1d": f32(inp["w1_down"][ls]),
        "w2g": f32(inp["w2_gate"][ls]), "w2u": f32(inp["w2_up"][ls]), "w2d": f32(inp["w2_down"][ls]),
        "win": f32(np.asarray(inp["w_in"], np.float32)[ls][:, :, perm]), "wout": f32(inp["w_out"][ls]),
        "cb": cbm, "cf": cf, "par": _params(inp, ls), "posrow": pos,
    }
    nc = _get_nc(ls, do_final, stop_after)
    n = len(xT_list)
    in_maps = [dict(shared, xT=np.ascontiguousarray(x)) for x in xT_list]
    res = run_bass_kernel_spmd(nc, in_maps, core_ids=list(range(n)) if core_ids is None else core_ids, **_RUN_KW)
    _LAST.clear(); _LAST.append(res)
    return [r["outT"] for r in res.results]


def kernel(**inputs):
    inp = {k: np.asarray(v) for k, v in inputs.items()}
    x = np.asarray(inp["x"], np.float32)
    B = x.shape[0]
    xs = [np.ascontiguousarray(x[b].T) for b in range(B)]
    if FUSED:
        outs = run_layers(xs, inp, range(DEPTH), True)
    else:
        outs = xs
        for l in range(DEPTH):
            outs = run_layers(outs, inp, [l], l == DEPTH - 1)
    return np.stack([np.ascontiguousarray(o.T) for o in outs], axis=0).astype(np.float32)
```

```python
import math
from contextlib import ExitStack
import numpy as np
import ml_dtypes
import concourse.bass as bass
import concourse.mybir as mybir
from concourse.bass_utils import run_bass_kernel_spmd

F32 = mybir.dt.float32
BF16 = mybir.dt.bfloat16
AF = mybir.ActivationFunctionType
ALU = mybir.AluOpType
AX = mybir.AxisListType

D = 1024
T = 2048
DFF = 2816
NF = DFF // 128
G = 2
NG = NF // G
DEPTH = 4
EPS = 1e-6
NSLOT = 6
FUSED = True
SAME_ENGINE_SYNC = True

SLOPES = 2.0 ** (-8.0 * (np.arange(16, dtype=np.float64) + 1.0) / 16.0)
SL_C = SLOPES[0:8]
SL_B = SLOPES[8:12]
SL_A = SLOPES[12:16]

CB_MASKC = 0
CB_TRI = 128
CB_ID = 256
CB_E = 384
CB_ONES = 2432
CB_N = 2560
CF_TBAB = 0
CF_TBC = 128
CF_PAST = 144
CF_A30 = 272
CF_BC = 400
CF_ONES = 528
CF_EPS = 592
CF_ID = 600
CF_N = 728
PR_G = 0
PR_SUB = 104
PR_LAM = 108
PR_SINK = 620
PR_N = 652


class Op:
    __slots__ = ("eng", "fn", "deps", "needed", "sem", "val", "dma", "epoch")


class Prog:
    ENGS = ("pe", "act", "dve", "pool", "sp")

    def __init__(self):
        self.ops = {e: [] for e in self.ENGS}
        self.lastw = {}
        self.readers = {}
        self.epoch = 0

    def add(self, eng, fn, reads=(), writes=(), dma=None):
        op = Op()
        op.eng, op.fn, op.dma, op.epoch = eng, fn, dma, self.epoch
        op.needed = False
        op.sem = None
        op.val = 0
        deps = {}
        for t in reads:
            w = self.lastw.get(t)
            if w is not None:
                deps[id(w)] = w
        for t in writes:
            w = self.lastw.get(t)
            if w is not None:
                deps[id(w)] = w
            for r in self.readers.get(t, ()):
                deps[id(r)] = r
        out = []
        for d in deps.values():
            if d is op:
                continue
            if d.eng == eng and d.dma is None:
                if eng == "pe" or eng == "sp" or not SAME_ENGINE_SYNC:
                    continue
            out.append(d)
        op.deps = out
        for t in reads:
            if isinstance(t, str) and t.startswith("c_"):
                continue
            self.readers.setdefault(t, []).append(op)
        for t in writes:
            self.lastw[t] = op
            self.readers[t] = []
        self.ops[eng].append(op)
        return op

    def emit(self, nc, stack):
        sems = {}

        def getsem(key):
            if key not in sems:
                sems[key] = stack.enter_context(nc.semaphore("s%d" % len(sems)))
            return sems[key]

        for e in self.ENGS:
            for op in self.ops[e]:
                for d in op.deps:
                    d.needed = True
        cnt = {}
        for e in self.ENGS:
            for op in self.ops[e]:
                if op.dma is not None:
                    key = ("dma", op.dma)
                    cnt[key] = cnt.get(key, 0) + 16
                    op.sem, op.val = getsem(key), cnt[key]
                elif op.needed:
                    key = (e, op.epoch)
                    cnt[key] = cnt.get(key, 0) + 1
                    op.sem, op.val = getsem(key), cnt[key]
        block = stack.enter_context(nc.Block())
        stats = {}

        def run(eng_name, eng):
            waited = {}
            nw = 0
            for op in self.ops[eng_name]:
                need = {}
                for d in op.deps:
                    k = id(d.sem)
                    if waited.get(k, 0) >= d.val:
                        continue
                    if k not in need or need[k][1] < d.val:
                        need[k] = (d.sem, d.val)
                for k, (s, v) in need.items():
                    eng.wait_ge(s, v)
                    waited[k] = v
                    nw += 1
                inst = op.fn(eng)
                if op.dma is not None:
                    inst.then_inc(op.sem, 16)
                elif op.needed:
                    inst.then_inc(op.sem, 1)
            stats[eng_name] = (len(self.ops[eng_name]), nw)

        @block.tensor
        def _(e):
            run("pe", e)

        @block.scalar
        def _(e):
            run("act", e)

        @block.vector
        def _(e):
            run("dve", e)

        @block.gpsimd
        def _(e):
            run("pool", e)

        @block.sync
        def _(e):
            run("sp", e)

        return stats


def bcast_mid(ap, n):
    l = [list(x) for x in ap.ap]
    return bass.AP(ap.tensor, ap.offset, [l[0], [0, n]] + l[1:])


def build(layer_ids, do_final, stop_after=None):
    NL = len(layer_ids)
    nc = bass.Bass("TRN2", target_bir_lowering=False)
    dr = {}

    def din(name, shape, dt=F32):
        dr[name] = nc.dram_tensor(name, list(shape), dt, kind="ExternalInput").ap()
        return dr[name]

    xin = din("xT", [D, T])
    w1g = din("w1g", [NL, D, DFF]); w1u = din("w1u", [NL, D, DFF]); w1d = din("w1d", [NL, DFF, D])
    w2g = din("w2g", [NL, D, DFF]); w2u = din("w2u", [NL, D, DFF]); w2d = din("w2d", [NL, DFF, D])
    win = din("win", [NL, D, 8 * 384]); wout = din("wout", [NL, D, D])
    cbd = din("cb", [128, CB_N], BF16); cfd = din("cf", [128, CF_N]); prd = din("par", [128, PR_N])
    posd = din("posrow", [128, 1024])
    outd = nc.dram_tensor("outT", [D, T], F32, kind="ExternalOutput").ap()

    P = Prog()
    st = ExitStack()
    with st:
        def sb(name, shape, dt):
            return st.enter_context(nc.sbuf_tensor(name, list(shape), dt))

        xT = sb("xT_sb", [128, 8, T], F32)
        hT = sb("hT", [128, 8, T], BF16)
        ring = sb("ring", [128, NSLOT, 2048], BF16)
        qk = sb("qk", [128, 2, T], BF16)
        vaug = sb("vaug", [128, 16, 2, 128], BF16)
        mix = sb("mix", [128, 2, T], BF16)
        actb = sb("actb", [128, 4096], BF16)
        pt = sb("pt", [128, 3, 1024], BF16)
        mnegt = sb("mnegt", [128, T], BF16)
        mneg = sb("mneg", [128, 2, 128], F32)
        gm = sb("gm", [128, 16], F32)
        top = sb("top", [128, 16], F32)
        sel = sb("sel", [128, 16], F32)
        kms = sb("kms", [128, 8], F32)
        kmz = sb("kmz", [128, 2, 8], BF16)
        rstd = sb("rstd", [128, 2, 512], F32)
        sg = sb("sg", [128, 2, 512], BF16)
        bcsb = sb("bcsb", [128, 512], F32)
        osq = sb("osq", [128, 512], BF16)
        rden = sb("rden", [128, 512], F32)
        sinkrow = sb("sinkrow", [128, 1024], F32)
        cb = sb("cb_sb", [128, CB_N], BF16)
        cf = sb("cf_sb", [128, CF_N], F32)
        par = sb("par_sb", [128, PR_N], F32)
        lamt = sb("lamt", [128, 16], F32)
        lamp = sb("lamp", [128, 2, 32], F32)
        ps = st.enter_context(nc.psum_tensor("ps", [128, 8, 512], F32))
        o1 = rstd[:, 0, :]
        o2 = rstd[:, 1, :]
        TO1, TO2 = ("rstd", 0), ("rstd", 1)
        identf = cf[:, CF_ID:CF_ID + 128]
        KZ = [(actb[:, 0:2048], [("act", 0), ("act", 1)]), (actb[:, 2048:4096], [("act", 2)]),
              (qk[:, 1, :], [("qk", 1, tc_) for tc_ in range(4)]), (mnegt[:, :], [("mnegt", c_) for c_ in range(4)])]

        ones_bf = cb[:, CB_ONES:CB_ONES + 128]
        ident = cb[:, CB_ID:CB_ID + 128]
        tri = cb[:, CB_TRI:CB_TRI + 128]
        maskc = cb[:, CB_MASKC:CB_MASKC + 256]

        def mm(out, lhsT, rhs, start, stop, reads, writes, tp=None):
            kw = {}
            if tp is not None and tp[0] == 96:
                kw["tile_position"] = tp
            return P.add("pe", lambda e: e.matmul(out, lhsT=lhsT, rhs=rhs, start=start, stop=stop, **kw),
                         reads, writes)

        def act(out, in_, func, reads, writes, bias=None, scale=None):
            kw = {}
            if bias is not None:
                kw["bias"] = bias
            if scale is not None:
                kw["scale"] = scale
            return P.add("act", lambda e: e.activation(out, in_, func, **kw), reads, writes)

        def tt(eng, out, in0, in1, op, reads, writes):
            return P.add(eng, lambda e: e.tensor_tensor(out, in0, in1, op), reads, writes)

        def stt(out, in0, scalar, in1, op0, op1, reads, writes):
            return P.add("dve", lambda e: e.scalar_tensor_tensor(out, in0, scalar, in1, op0, op1), reads, writes)

        def ts(eng, out, in0, s1, s2, op0, op1, reads, writes):
            if op1 is None:
                return P.add(eng, lambda e: e.tensor_scalar(out, in0, s1, None, op0), reads, writes)
            return P.add(eng, lambda e: e.tensor_scalar(out, in0, s1, s2, op0, op1), reads, writes)

        def recip(out, in_, reads, writes):
            return P.add("dve", lambda e: e.reciprocal(out, in_), reads, writes)

        def copy(eng, out, in_, reads, writes):
            if eng == "act":
                return P.add("act", lambda e: e.copy(out, in_), reads, writes)
            return P.add(eng, lambda e: e.tensor_copy(out, in_), reads, writes)

        def dma(eng, out, in_, key, reads, writes):
            return P.add(eng, lambda e: e.dma_start(out=out, in_=in_), reads, writes, dma=key)

        def xtok(kc, tc):
            return ("x", kc, tc)

        loads = []

        def colblk(w, l, c0, n):
            return w[l, :, c0:c0 + n].rearrange("(kc p) n -> p kc n", p=128)

        def rowblk(w, l, r0):
            return w[l, r0:r0 + 256, :].rearrange("(rc p) n -> p rc n", p=128)

        def ffn_loads(wg, wu, wd, l):
            for g in range(NG):
                loads.append(("c", colblk(wg, l, g * 256, 256), 256))
                loads.append(("c", colblk(wu, l, g * 256, 256), 256))
                loads.append(("r", rowblk(wd, l, g * 256), 0))

        for l in range(NL):
            ffn_loads(w1g, w1u, w1d, l)
            for p in range(8):
                loads.append(("c", colblk(win, l, p * 384, 256), 256))
                loads.append(("c", colblk(win, l, p * 384 + 256, 128), 128))
                if p % 2 == 1:
                    loads.append(("r", rowblk(wout, l, (p // 2) * 256), 0))
            ffn_loads(w2g, w2u, w2d, l)
        wstate = {"rec": 0, "next": 0}

        def slot_view(i):
            kind, src, n = loads[i]
            s = i % NSLOT
            flat = ring[:, s, :]
            if kind == "c":
                return flat.rearrange("p (kc n) -> p kc n", n=256)
            return flat.rearrange("p (rc n) -> p rc n", n=1024)

        def take(n):
            a = wstate["next"]
            wstate["next"] = a + n
            upto = min(len(loads), a + NSLOT)
            while wstate["rec"] < upto:
                i = wstate["rec"]
                kind, src, ncol = loads[i]
                v = slot_view(i)
                dst = v[:, :, 0:ncol] if kind == "c" else v
                dma("pool", dst, src, ("slot", i % NSLOT), [], [("slot", i % NSLOT)])
                wstate["rec"] += 1
            return [(slot_view(i), ("slot", i % NSLOT)) for i in range(a, a + n)]

        dma("sp", cb[:, :], cbd, "cst0", [], ["c_cb"])
        dma("sp", cf[:, :], cfd, "cst1", [], ["c_cf"])
        dma("sp", par[:, :], prd, "cst2", [], ["c_par"])
        for kc in range(8):
            dma("sp", xT[:, kc, :], xin[kc * 128:(kc + 1) * 128, :], "xin", [], [xtok(kc, tc) for tc in range(4)])
        P.add("pool", lambda e: e.memset(vaug[:, :, :, :], 1.0), [], [("v", t) for t in range(16)])
        P.add("pool", lambda e: e.memset(mneg[:, :, :], 0.0), [], ["mneg"])
        P.add("pool", lambda e: e.memset(kmz[:, :, :], 0.0), [], ["kmz"])

        def rmsnorm(gcol, final=False):
            sq = actb[:, :].rearrange("p (k n) -> p k n", n=512)
            for tc in range(4):
                cs = slice(tc * 512, (tc + 1) * 512)
                bank = 5 + (tc % 2)
                rb = rstd[:, tc % 2, :]
                act(sq, xT[:, :, cs], AF.Square, [xtok(kc, tc) for kc in range(8)], [("act", 0), ("act", 1), ("act", 2)])
                for kc in range(8):
                    mm(ps[:, bank, :], ones_bf, sq[:, kc, :], kc == 0, kc == 7,
                       ["c_cb", ("act", 0), ("act", 1), ("act", 2)], [("ps", bank)])
                act(rb, ps[:, bank, :], AF.Ln, [("ps", bank)], [("rstd", tc % 2)], bias=cf[:, CF_EPS:CF_EPS + 1],
                    scale=1.0 / D)
                act(rb, rb, AF.Exp, [("rstd", tc % 2)], [("rstd", tc % 2)], scale=-0.5)
                for kc in range(8):
                    if final:
                        stt(xT[:, kc, cs], xT[:, kc, cs], par[:, gcol + kc:gcol + kc + 1], rb, ALU.mult, ALU.mult,
                            [xtok(kc, tc), ("rstd", tc % 2), "c_par"], [xtok(kc, tc)])
                    else:
                        stt(hT[:, kc, cs], xT[:, kc, cs], par[:, gcol + kc:gcol + kc + 1], rb, ALU.mult, ALU.mult,
                            [xtok(kc, tc), ("rstd", tc % 2), "c_par"], [("h", kc, tc)])


        def ffn():
            pend = None
            cnt = 0
            for g in range(NG):
                if pend is not None:
                    pend()
                    pend = None
                (wgv, tg), (wuv, tu), (wdv, td) = take(3)
                for tc in range(4):
                    cs = slice(tc * 512, (tc + 1) * 512)
                    ai = cnt % 2
                    av = actb[:, ai * 1024:(ai + 1) * 1024].rearrange("p (f n) -> p f n", n=512)
                    for fi in range(G):
                        bg, bu = 2 * (fi % 2), 2 * (fi % 2) + 1
                        for kc in range(8):
                            mm(ps[:, bg, :], wgv[:, kc, fi * 128:(fi + 1) * 128], hT[:, kc, cs], kc == 0, kc == 7,
                               [tg, ("h", kc, tc)], [("ps", bg)])
                        for kc in range(8):
                            mm(ps[:, bu, :], wuv[:, kc, fi * 128:(fi + 1) * 128], hT[:, kc, cs], kc == 0, kc == 7,
                               [tu, ("h", kc, tc)], [("ps", bu)])
                        act(sg[:, fi % 2, :], ps[:, bg, :], AF.Silu, [("ps", bg)], [("sg", fi % 2)])
                        tt("dve", av[:, fi, :], sg[:, fi % 2, :], ps[:, bu, :], ALU.mult,
                           [("sg", fi % 2), ("ps", bu)], [("act", ai)])
                    if pend is not None:
                        pend()

                    def down(av=av, ai=ai, wdv=wdv, td=td, tc=tc, cs=cs):
                        for dc in range(8):
                            b = 4 + (dc % 4)
                            for fi in range(G):
                                mm(ps[:, b, :], wdv[:, fi, dc * 128:(dc + 1) * 128], av[:, fi, :], fi == 0, fi == G - 1,
                                   [td, ("act", ai)], [("ps", b)])
                            stt(xT[:, dc, cs], ps[:, b, :], 0.5, xT[:, dc, cs], ALU.mult, ALU.add,
                                [("ps", b), xtok(dc, tc)], [xtok(dc, tc)])
                    pend = down
                    cnt += 1
            if pend is not None:
                pend()

        def proj_qk(wv, tw, kind):
            if kind == "B":
                for buf, toks in KZ:
                    P.add("pool", lambda e, buf=buf: e.memset(buf, 0.0), [], toks)
            else:
                P.add("pool", lambda e: e.memset(actb[64:128, 0:2048], 0.0), [], KZ[0][1])
                P.add("pool", lambda e: e.memset(actb[0:64, 2048:4096], 0.0), [], KZ[1][1])
            n = 0
            for j in range(2):
                for tc in range(4):
                    cs = slice(tc * 512, (tc + 1) * 512)
                    b = n % 4
                    n += 1
                    eng = "act" if n % 2 else "dve"
                    for kc in range(8):
                        mm(ps[:, b, :], wv[:, kc, j * 128:(j + 1) * 128], hT[:, kc, cs], kc == 0, kc == 7,
                           [tw, ("h", kc, tc)], [("ps", b)])
                    if j == 0:
                        copy(eng, qk[:, 0, cs], ps[:, b, :], [("ps", b)], [("qk", 0, tc)])
                    elif kind == "B":
                        copy(eng, KZ[0][0][0:32, cs], ps[0:32, b, :], [("ps", b)], KZ[0][1])
                        copy(eng, KZ[1][0][32:64, cs], ps[32:64, b, :], [("ps", b)], KZ[1][1])
                        copy(eng, KZ[2][0][64:96, cs], ps[64:96, b, :], [("ps", b)], KZ[2][1])
                        copy(eng, KZ[3][0][64:128, cs], ps[64:128, b, :], [("ps", b)], KZ[3][1])
                        P.add("pool", lambda e, cs=cs: e.memset(KZ[3][0][64:96, cs], 0.0), [], KZ[3][1])
                    else:
                        copy(eng, KZ[0][0][0:64, cs], ps[0:64, b, :], [("ps", b)], KZ[0][1])
                        copy(eng, KZ[1][0][64:128, cs], ps[64:128, b, :], [("ps", b)], KZ[1][1])
                        if kind == "A":
                            P.add("dve", lambda e, tc=tc, cs=cs: e.reduce_sum(
                                kms[0:64, 2 * tc:2 * tc + 2],
                                KZ[0][0][0:64, cs].rearrange("p (n l) -> p n l", l=256), AX.X),
                                KZ[0][1], ["kms"])
                            P.add("dve", lambda e, tc=tc, cs=cs: e.reduce_sum(
                                kms[64:128, 2 * tc:2 * tc + 2],
                                KZ[1][0][64:128, cs].rearrange("p (n l) -> p n l", l=256), AX.X),
                                KZ[1][1], ["kms"])
            if kind == "A":
                copy("dve", kmz[0:64, 0, :], kms[0:64, :], ["kms", "kmz"], ["kmz"])
                copy("dve", kmz[64:128, 1, :], kms[64:128, :], ["kms", "kmz"], ["kmz"])

        def proj_v(wv, tw, per_t=None):
            for t in range(16):
                b = 4 + (t % 2)
                cs = slice(t * 128, (t + 1) * 128)
                oc = slice(0, 128)
                for kc in range(8):
                    mm(ps[:, b, oc], hT[:, kc, cs], wv[:, kc, 0:128], kc == 0, kc == 7,
                       [tw, ("h", kc, t // 4)], [("ps", b)])
                copy("act" if t % 2 else "dve", vaug[:, t, :, 0:64],
                     ps[:, b, oc].rearrange("p (h d) -> p h d", d=64), [("ps", b)], [("v", t)])
                if per_t is not None:
                    per_t(t)

        ncount = [0]

        def normalize(ob, dst, dtok, h_sink=None):
            buf, tk = (rden, "rden") if ncount[0] % 2 == 0 else (bcsb, "bcsb")
            ncount[0] += 1
            if h_sink is not None:
                srow = bcast_mid(sinkrow[64:128, h_sink * 128:(h_sink + 1) * 128], 4)
                tt("dve", buf[0:64, :].rearrange("p (a b) -> p a b", b=128),
                   ps[64:128, ob, :].rearrange("p (a b) -> p a b", b=128), srow, ALU.add,
                   [("ps", ob), "sinkrow"], [tk])
                if h_sink == 0:
                    recip(buf[0:64, :], buf[0:64, :], [tk], [tk])
                else:
                    act(buf[0:64, :], buf[0:64, :], AF.Ln, [tk], [tk])
            else:
                act(buf[0:64, :], ps[64:128, ob, :], AF.Ln, [("ps", ob)], [tk])
            if h_sink != 0:
                act(buf[0:64, :], buf[0:64, :], AF.Exp, [tk], [tk], scale=-1.0)
            tt("dve", dst, ps[0:64, ob, :], buf[0:64, :], ALU.mult, [("ps", ob), tk], dtok)

        def run_pipeline(steps, look):
            n = len(steps)
            for i in range(n + look):
                if i < n:
                    steps[i][0]()
                    steps[i][1]()
                j = i - look
                if j >= 0:
                    steps[j][2]()
                    if steps[j][3] is not None:
                        steps[j][3]()

        def pth_of(hb):
            return pt[:, hb // 2, (hb % 2) * 512:(hb % 2) * 512 + 512]

        def attn_full(kind, pidx, li):
            scale = 32 ** -0.5 if kind == "B" else 0.125
            pend = {"p1": None, "p2": None}

            def after_loop(p1):
                if pend["p2"] is not None:
                    pend["p2"]()
                    pend["p2"] = None
                if pend["p1"] is not None:
                    pend["p2"] = pend["p1"]()
                pend["p1"] = p1

            steps = []
            sc = 0
            for hl in range(2):
                h = 2 * pidx + hl
                tbcol = CF_TBAB + (h if kind == "A" else 4 + h) * 16
                for c in range(4):
                    nkt = 4 * c + 4
                    it = hl * 4 + c
                    obanks = ([4, 5] if it % 2 == 0 else [6, 7]) if kind == "B" else [4 + (it % 2)]
                    cs = slice(c * 512, (c + 1) * 512)
                    dst = mix[hl * 64:(hl + 1) * 64, pidx % 2, cs]
                    dtok = [("mix", pidx % 2, c, hl)]
                    if kind == "A":
                        def post1(ob=obanks[0], dst=dst, dtok=dtok):
                            normalize(ob, dst, dtok)
                            return None
                    else:
                        def post1(obanks=obanks, dst=dst, dtok=dtok):
                            normalize(obanks[0], o1[0:64, :], [TO1])
                            normalize(obanks[1], o2[0:64, :], [TO2])
                            stt(o1[0:64, :], o2[0:64, :], lamt[0:64, 4:5], o1[0:64, :], ALU.mult, ALU.add,
                                [TO1, TO2, "lamt"], [TO1])
                            tt("dve", osq[0:64, :], o1[0:64, :], o1[0:64, :], ALU.mult, [TO1], ["osq"])

                            def phase2(dst=dst, dtok=dtok):
                                mm(ps[0:64, 3, :], cb[0:64, CB_ONES:CB_ONES + 64], osq[0:64, :], True, True,
                                   ["c_cb", "osq"], [("ps", 3)])
                                act(o2[0:64, :], ps[0:64, 3, :], AF.Ln, [("ps", 3)], [TO2],
                                    bias=cf[0:64, CF_EPS:CF_EPS + 1], scale=1.0 / 64)
                                act(o2[0:64, :], o2[0:64, :], AF.Exp, [TO2], [TO2], scale=-0.5)
                                stt(dst, o1[0:64, :], lamt[0:64, 5:6], o2[0:64, :], ALU.mult, ALU.mult,
                                    [TO1, TO2, "lamt"], dtok)
                            return phase2
                    for kt in range(nkt):
                        j = kt - 4 * c
                        col0 = max(j, 0) * 128
                        ncols = 512 - col0
                        qs = slice(c * 512 + col0, (c + 1) * 512)
                        ks = slice(kt * 128, (kt + 1) * 128)
                        for m in range(2 if kind == "B" else 1):
                            bnk = sc % 4
                            hb = sc % 6
                            sc += 1
                            pth = pth_of(hb)
                            kzb, kzt = KZ[2 * hl + m] if kind == "B" else KZ[hl]
                            bias = cf[:, tbcol + j + 12:tbcol + j + 13]
                            last = (kt == nkt - 1) and (m == (1 if kind == "B" else 0))

                            def st_f(kzb=kzb, kzt=kzt, bnk=bnk, ncols=ncols, qs=qs, ks=ks, c=c, hl=hl, kt=kt):
                                if kind == "A":
                                    mm(ps[:, bnk, 0:ncols], kzb[:, ks], qk[:, 0, qs],
                                       True, False, kzt + [("qk", 0, c)], [("ps", bnk)])
                                    ec = CB_E + hl * 1024 + (kt // 2) * 128
                                    mm(ps[:, bnk, 0:ncols], cb[:, ec:ec + 128], mnegt[:, qs], False, True,
                                       ["c_cb", ("mnegt", c)], [("ps", bnk)])
                                else:
                                    mm(ps[:, bnk, 0:ncols], kzb[:, ks], qk[:, 0, qs],
                                       True, True, kzt + [("qk", 0, c)], [("ps", bnk)])

                            def ex_f(bnk=bnk, hb=hb, pth=pth, j=j, ncols=ncols, bias=bias):
                                act(pth[:, 0:ncols], ps[:, bnk, 0:ncols], AF.Exp, [("ps", bnk), "c_cf"],
                                    [("pth", hb)], bias=bias, scale=scale)
                                if j >= 0:
                                    tt("pool", pth[:, 0:128], pth[:, 0:128], tri, ALU.mult,
                                       [("pth", hb), "c_cb"], [("pth", hb)])

                            def pv_f(ob=obanks[m], hb=hb, pth=pth, kt=kt, col0=col0, ncols=ncols, hl=hl, nkt=nkt):
                                mm(ps[:, ob, col0:512], vaug[:, kt, hl, :], pth[:, 0:ncols],
                                   kt == 0, kt == nkt - 1, [("v", kt), ("pth", hb)], [("ps", ob)])

                            end_f = (lambda post1=post1: after_loop(post1)) if last else None
                            steps.append((st_f, ex_f, pv_f, end_f))
            run_pipeline(steps, 3)
            after_loop(None)
            after_loop(None)

        def moba_gate(pidx):
            def step(qt):
                b = qt // 2
                qs = slice(qt * 128, (qt + 1) * 128)
                mm(ps[:, 3, 0:16], qk[:, 0, qs], kmz[:, :, :].rearrange("p h n -> p (h n)"),
                   True, True, [("qk", 0, qt // 4), "kmz"], [("ps", 3)])
                tt("dve", gm[:, :], ps[:, 3, 0:16], cf[:, CF_PAST + b * 16:CF_PAST + b * 16 + 16], ALU.add,
                   [("ps", 3), "c_cf"], ["gm"])
                for hl in range(2):
                    P.add("dve", lambda e, hl=hl: e.max(top[:, hl * 8:hl * 8 + 8], gm[:, hl * 8:hl * 8 + 8]),
                          ["gm"], [("top", hl)])
                for hl in range(2):
                    stt(sel[:, hl * 8:hl * 8 + 8], gm[:, hl * 8:hl * 8 + 8], top[:, hl * 8 + 2:hl * 8 + 3],
                        cf[:, CF_A30 + b * 16 + hl * 8:CF_A30 + b * 16 + hl * 8 + 8], ALU.is_ge, ALU.mult,
                        ["gm", ("top", hl), "c_cf"], [("sel", hl)])
                mi = qt % 2
                tt("dve", mneg[:, mi, :].rearrange("p (h n) -> p h n", n=64)[:, :, 0:8],
                   sel[:, :].rearrange("p (h n) -> p h n", n=8),
                   cf[:, CF_BC + b * 16:CF_BC + b * 16 + 16].rearrange("p (h n) -> p h n", n=8), ALU.add,
                   [("sel", 0), ("sel", 1), "c_cf", "mneg"], [("mneg", mi)])
                if qt > 0:
                    xpose(qt - 1)
                if qt == 15:
                    xpose(15)

            def xpose(qt):
                mi = qt % 2
                pc = (qt % 4) * 128
                P.add("pe", lambda e, mi=mi, pc=pc: e.transpose(ps[:, 7, pc:pc + 128], mneg[:, mi, :], identf),
                      [("mneg", mi), "c_cf"], [("ps", 7)])
                if qt % 4 == 3:
                    c = qt // 4
                    copy("act", mnegt[:, c * 512:(c + 1) * 512], ps[:, 7, :], [("ps", 7)], [("mnegt", c)])
            return step

        def swa(pidx, li):
            cp = pidx - 4
            pendn = [None]

            def chunk_done(p):
                if pendn[0] is not None:
                    pendn[0]()
                pendn[0] = p

            steps = []
            sc = 0
            for hl in range(2):
                h = 2 * cp + hl
                kzb, kzt = KZ[hl]
                for c in range(4):
                    ob = 4 + ((hl * 4 + c) % 2)
                    cs = slice(c * 512, (c + 1) * 512)

                    def postn(ob=ob, hl=hl, cs=cs, c=c, h=h):
                        normalize(ob, mix[hl * 64:(hl + 1) * 64, pidx % 2, cs], [("mix", pidx % 2, c, hl)], h_sink=h)
                    for qi in range(4):
                        qt = 4 * c + qi
                        qs = slice(qt * 128, (qt + 1) * 128)
                        sbk = sc % 4
                        hb = sc % 6
                        sc += 1
                        pth = pth_of(hb)
                        ptk = ("pth", hb)

                        def st_f(qt=qt, qs=qs, sbk=sbk, kzb=kzb, kzt=kzt):
                            mm(ps[:, sbk, 128:256], kzb[:, qs], qk[:, 0, qs], True, True,
                               kzt + [("qk", 0, qt // 4)], [("ps", sbk)])
                            if qt > 0:
                                ks = slice((qt - 1) * 128, qt * 128)
                                mm(ps[:, sbk, 0:128], kzb[:, ks], qk[:, 0, qs], True, True,
                                   kzt + [("qk", 0, qt // 4)], [("ps", sbk)])

                        def ex_f(qt=qt, sbk=sbk, pth=pth, ptk=ptk, h=h):
                            act(pth[:, 128:256], ps[:, sbk, 128:256], AF.Exp, [("ps", sbk), "c_cf"], [ptk],
                                bias=cf[:, CF_TBC + h * 2 + 1:CF_TBC + h * 2 + 2], scale=0.125)
                            if qt > 0:
                                act(pth[:, 0:128], ps[:, sbk, 0:128], AF.Exp, [("ps", sbk), "c_cf"], [ptk],
                                    bias=cf[:, CF_TBC + h * 2:CF_TBC + h * 2 + 1], scale=0.125)
                                tt("pool", pth[:, 0:256], pth[:, 0:256], maskc, ALU.mult, [ptk, "c_cb"], [ptk])
                            else:
                                tt("pool", pth[:, 128:256], pth[:, 128:256], tri, ALU.mult, [ptk, "c_cb"], [ptk])

                        def pv_f(qt=qt, qi=qi, pth=pth, ptk=ptk, ob=ob, hl=hl):
                            oc = slice(qi * 128, (qi + 1) * 128)
                            if qt > 0:
                                mm(ps[:, ob, oc], vaug[:, qt - 1, hl, :], pth[:, 0:128], True, False,
                                   [("v", qt - 1), ptk], [("ps", ob)])
                                mm(ps[:, ob, oc], vaug[:, qt, hl, :], pth[:, 128:256], False, True,
                                   [("v", qt), ptk], [("ps", ob)])
                            else:
                                mm(ps[:, ob, oc], vaug[:, qt, hl, :], pth[:, 128:256], True, True,
                                   [("v", qt), ptk], [("ps", ob)])

                        end_f = (lambda postn=postn: chunk_done(postn)) if qi == 3 else None
                        steps.append((st_f, ex_f, pv_f, end_f))
            run_pipeline(steps, 3)
            chunk_done(None)

        def w_out(wv, tw):
            n = 0
            for tc in range(4):
                cs = slice(tc * 512, (tc + 1) * 512)
                for dc in range(8):
                    b = n % 4
                    n += 1
                    for rc in range(2):
                        mm(ps[:, b, :], wv[:, rc, dc * 128:(dc + 1) * 128], mix[:, rc, cs], rc == 0, rc == 1,
                           [tw, ("mix", rc, tc, 0), ("mix", rc, tc, 1)], [("ps", b)])
                    tt("dve", xT[:, dc, cs], ps[:, b, :], xT[:, dc, cs], ALU.add, [("ps", b), xtok(dc, tc)],
                       [xtok(dc, tc)])

        def layer_params(li, ltrue):
            lam_init = 0.8 - 0.6 * math.exp(-0.3 * ltrue)
            base = PR_LAM + li * 128
            tt("dve", lamp[:, 0, :], par[:, base:base + 32], par[:, base + 32:base + 64], ALU.mult, ["c_par", "lamp"], ["lamp"])
            tt("dve", lamp[:, 1, :], par[:, base + 64:base + 96], par[:, base + 96:base + 128], ALU.mult, ["c_par", "lamp"], ["lamp"])
            P.add("dve", lambda e: e.reduce_sum(lamt[:, 0:2], lamp[:, :, :], AX.X), ["lamp", "lamt"], ["lamt"])
            act(lamt[:, 2:4], lamt[:, 0:2], AF.Exp, ["lamt"], ["lamt"])
            tt("dve", lamt[:, 4:5], lamt[:, 3:4], lamt[:, 2:3], ALU.subtract, ["lamt"], ["lamt"])
            ts("dve", lamt[:, 4:5], lamt[:, 4:5], -lam_init, None, ALU.add, None, ["lamt"], ["lamt"])
            ts("dve", lamt[:, 5:6], par[:, PR_SUB + li:PR_SUB + li + 1], 1.0 - lam_init, None, ALU.mult, None,
               ["c_par", "lamt"], ["lamt"])
            dma("sp", sinkrow[:, :], posd, "sink", ["sinkrow"], ["sinkrow"])
            for h in range(8):
                act(sinkrow[:, h * 128:(h + 1) * 128], sinkrow[:, h * 128:(h + 1) * 128], AF.Exp,
                    ["sinkrow", "c_par"], ["sinkrow"], bias=par[:, PR_SINK + li * 8 + h:PR_SINK + li * 8 + h + 1],
                    scale=1.0)

        for li, ltrue in enumerate(layer_ids):
            P.epoch = li
            if stop_after == "pro":
                break
            layer_params(li, ltrue)
            if stop_after == "params":
                break
            rmsnorm(PR_G + (li * 3 + 0) * 8)
            if stop_after == "norm":
                break
            ffn()
            if stop_after == "ffn1":
                break
            rmsnorm(PR_G + (li * 3 + 1) * 8)
            for pidx in range(8):
                kind = "A" if pidx < 2 else ("B" if pidx < 4 else "C")
                nl = 3 if pidx % 2 == 1 else 2
                got = take(nl)
                (wqk, tqk), (wvv, tvv) = got[0], got[1]
                proj_qk(wqk, tqk, kind)
                proj_v(wvv, tvv, moba_gate(pidx) if kind == "A" else None)
                if kind == "A":
                    attn_full("A", pidx, li)
                elif kind == "B":
                    attn_full("B", pidx - 2, li)
                else:
                    swa(pidx, li)
                if pidx % 2 == 1:
                    w_out(got[2][0], got[2][1])
                if stop_after == ("pair", pidx):
                    break
            if stop_after is not None:
                break
            rmsnorm(PR_G + (li * 3 + 2) * 8)
            ffn()
        if do_final and stop_after is None:
            rmsnorm(PR_G + 96, final=True)
        outs = []
        for kc in range(8):
            outs.append(dma("sp", outd[kc * 128:(kc + 1) * 128, :], xT[:, kc, :], "out",
                            [xtok(kc, tc) for tc in range(4)], [("outdone", kc)]))
        P.add("sp", lambda e: e.nop(), [("outdone", kc) for kc in range(8)] + [("slot", s_) for s_ in range(NSLOT)], [])
        stats = P.emit(nc, st)
    return nc, stats


def _consts():
    cbm = np.zeros((128, CB_N), np.float32)
    k = np.arange(128)[:, None]
    q = np.arange(128)[None, :]
    cbm[:, CB_MASKC:CB_MASKC + 128] = (q < k)
    cbm[:, CB_TRI:CB_TRI + 128] = (q >= k)
    cbm[:, CB_ID:CB_ID + 128] = np.eye(128)
    E = np.zeros((128, 2, 8, 128), np.float32)
    for hl in range(2):
        for n in range(8):
            E[hl * 64 + n, hl, n, :] = 1.0
    cbm[:, CB_E:CB_E + 2048] = E.reshape(128, 2048)
    cbm[:, CB_ONES:CB_ONES + 128] = 1.0
    cf = np.zeros((128, CF_N), np.float64)
    p = np.arange(128, dtype=np.float64)
    ab = np.concatenate([SL_A, SL_B])
    for s in range(8):
        for d in range(-12, 4):
            cf[:, CF_TBAB + s * 16 + d + 12] = ab[s] * (p + 128.0 * d)
    for h in range(8):
        cf[:, CF_TBC + h * 2] = SL_C[h] * (p - 192.0)
        cf[:, CF_TBC + h * 2 + 1] = SL_C[h] * (p - 64.0)
    for b in range(8):
        for hl in range(2):
            for n in range(8):
                cf[:, CF_PAST + b * 16 + hl * 8 + n] = 0.0 if n < b else -1e30
                cf[:, CF_A30 + b * 16 + hl * 8 + n] = 30000.0 if n < b else 0.0
                cf[:, CF_BC + b * 16 + hl * 8 + n] = 0.0 if n == b else -30000.0
    cf[:, CF_ONES:CF_ONES + 64] = 1.0
    cf[:, CF_EPS] = EPS
    cf[:, CF_ID:CF_ID + 128] = np.eye(128)
    pos = np.zeros((128, 1024), np.float64)
    for h in range(8):
        pos[:, h * 128:(h + 1) * 128] = (SL_C[h] * (np.arange(128) - 64.0))[None, :]
    return cbm.astype(ml_dtypes.bfloat16), cf.astype(np.float32), pos.astype(np.float32)


def _perm_win():
    cols = []
    for j in range(2):
        cols += list(range(128 * j, 128 * j + 128)) + list(range(256 + 128 * j, 256 + 128 * j + 128)) + \
            list(range(512 + 128 * j, 512 + 128 * j + 128))
    for j in range(2):
        cols += list(range(768 + 128 * j, 768 + 128 * j + 128)) + list(range(1024 + 128 * j, 1024 + 128 * j + 128)) + \
            list(range(1280 + 128 * j, 1280 + 128 * j + 128))
    for j in range(4):
        kv = j // 2
        kc = list(range(2048 + 64 * kv, 2048 + 64 * kv + 64))
        vc = list(range(2176 + 64 * kv, 2176 + 64 * kv + 64))
        cols += list(range(1536 + 128 * j, 1536 + 128 * j + 128)) + kc + kc + vc + vc
    return np.asarray(cols, np.int64)


def _params(inp, layer_ids):
    par = np.zeros((128, PR_N), np.float32)

    def gl(v):
        return np.ascontiguousarray(np.asarray(v, np.float32).reshape(8, 128).T)
    for li, l in enumerate(layer_ids):
        par[:, PR_G + (li * 3 + 0) * 8:PR_G + (li * 3 + 0) * 8 + 8] = gl(inp["norm_ffn1"][l])
        par[:, PR_G + (li * 3 + 1) * 8:PR_G + (li * 3 + 1) * 8 + 8] = gl(inp["norm_mix"][l])
        par[:, PR_G + (li * 3 + 2) * 8:PR_G + (li * 3 + 2) * 8 + 8] = gl(inp["norm_ffn2"][l])
        par[:, PR_SUB + li] = np.tile(np.asarray(inp["diff_subln"][l], np.float32), 2)
        for j, nm in enumerate(("lam_q1", "lam_k1", "lam_q2", "lam_k2")):
            par[:, PR_LAM + li * 128 + j * 32:PR_LAM + li * 128 + j * 32 + 32] = np.asarray(inp[nm][l], np.float32)[None, :]
        par[:, PR_SINK + li * 8:PR_SINK + li * 8 + 8] = np.asarray(inp["sinks"][l], np.float32)[None, :]
    par[:, PR_G + 96:PR_G + 104] = gl(inp["final_norm"])
    return par


_CACHE = {}
_RUN_KW = {}
_LAST = []


def _get_nc(layer_ids, do_final, stop_after=None):
    key = (tuple(layer_ids), do_final, stop_after)
    if key not in _CACHE:
        _CACHE[key] = build(list(layer_ids), do_final, stop_after)[0]
    return _CACHE[key]


def run_layers(xT_list, inp, layer_ids, do_final, stop_after=None, core_ids=None):
    cbm, cf, pos = _consts()
    perm = _perm_win()
    ls = list(layer_ids)
    f32 = lambda a: np.ascontiguousarray(np.asarray(a, np.float32))
    shared = {
        "w1g": f32(inp["w1_gate"][ls]), "w1u": f32(inp["w1_up"][ls]), "w1d": f32(inp["w1_down"][ls]),
        "w2g": f32(inp["w2_gate"][ls]), "w2u": f32(inp["w2_up"][ls]), "w2d": f32(inp["w2_down"][ls]),
        "win": f32(np.asarray(inp["w_in"], np.float32)[ls][:, :, perm]), "wout": f32(inp["w_out"][ls]),
        "cb": cbm, "cf": cf, "par": _params(inp, ls), "posrow": pos,
    }
    nc = _get_nc(ls, do_final, stop_after)
    n = len(xT_list)
    in_maps = [dict(shared, xT=np.ascontiguousarray(x)) for x in xT_list]
    res = run_bass_kernel_spmd(nc, in_maps, core_ids=list(range(n)) if core_ids is None else core_ids, **_RUN_KW)
    _LAST.clear(); _LAST.append(res)
    return [r["outT"] for r in res.results]


def kernel(**inputs):
    inp = {k: np.asarray(v) for k, v in inputs.items()}
    x = np.asarray(inp["x"], np.float32)
    B = x.shape[0]
    xs = [np.ascontiguousarray(x[b].T) for b in range(B)]
    if FUSED:
        outs = run_layers(xs, inp, range(DEPTH), True)
    else:
        outs = xs
        for l in range(DEPTH):
            outs = run_layers(outs, inp, [l], l == DEPTH - 1)
    return np.stack([np.ascontiguousarray(o.T) for o in outs], axis=0).astype(np.float32)
```
